# Optimizing a Trainium2 kernel written in Bass

```python
import math
import jax, jax.numpy as jnp
from jax import lax
import numpy as np

D_MODEL = 1024
BATCH = 8
SEQ = 8192
DEPTH = 2

N_HEADS = 16
HEAD_DIM = 64
N_KV_HEADS = 2
Q_GROUP = N_HEADS // N_KV_HEADS
ATTN_WIDTH = N_HEADS * HEAD_DIM
KV_WIDTH = N_KV_HEADS * HEAD_DIM
WINDOW = 128
BLOCK = 128
N_META = 16
META_PAD = BLOCK - N_META
POOL_WINDOWS = (2, 4, 8, 16)
N_POOL_GROUPS = 4
POOL_GROUP_DIM = 128
POOL_WIDTH = N_POOL_GROUPS * POOL_GROUP_DIM
POOL_GROUP_OUT = D_MODEL // N_POOL_GROUPS
N_BRANCHES = 2
IN_WIDTH = ATTN_WIDTH + 2 * KV_WIDTH + POOL_WIDTH + N_BRANCHES * D_MODEL
D_FF = 2816
N_EXPERTS = 8
TOP_K = 2
D_FF_EXPERT = 3584
N_DENSE = (DEPTH + 1) // 2
N_MOE = DEPTH // 2
RMS_EPS = 1e-5
NEG_INF = -1e30

kernel_name = "hybrid_swa_sink_pool_gated_moe"


def rms_norm(x, gain):
    xf = x.astype(jnp.float32)
    y = xf * lax.rsqrt(jnp.mean(xf * xf, axis=-1, keepdims=True) + RMS_EPS)
    return (y * gain.astype(jnp.float32)).astype(x.dtype)


def alibi_slopes():
    return jnp.asarray(np.array([2.0 ** (-8.0 * (h + 1) / N_HEADS) for h in range(N_HEADS)], dtype=np.float32))


def swa_sink_attention(q, k, v, sinks, slopes):
    B, L = q.shape[0], q.shape[1]
    P = L + META_PAD
    nblk = P // BLOCK

    def pad_front(t, n):
        return jnp.pad(t, ((0, 0), (n, 0), (0, 0), (0, 0)))

    qb = pad_front(q, META_PAD).reshape(B, nblk, BLOCK, N_KV_HEADS, Q_GROUP, HEAD_DIM)

    def band(t):
        tp = pad_front(t, META_PAD)
        prev = pad_front(tp, BLOCK)[:, :P]
        return jnp.concatenate([prev.reshape(B, nblk, BLOCK, N_KV_HEADS, HEAD_DIM),
                                tp.reshape(B, nblk, BLOCK, N_KV_HEADS, HEAD_DIM)], axis=2)

    kb, vb = band(k), band(v)
    km, vm = k[:, :N_META], v[:, :N_META]
    scale = HEAD_DIM ** -0.5
    s_band = jnp.einsum('bnqkgd,bnskd->bkgnqs', qb, kb, preferred_element_type=jnp.float32) * scale
    s_meta = jnp.einsum('bnqkgd,bmkd->bkgnqm', qb, km, preferred_element_type=jnp.float32) * scale

    blk = jnp.arange(nblk)[:, None] * BLOCK
    t_q = blk + jnp.arange(BLOCK)[None, :]
    t_band = blk - BLOCK + jnp.arange(2 * BLOCK)[None, :]
    t_meta = META_PAD + jnp.arange(N_META)
    d_band = t_q[:, :, None] - t_band[:, None, :]
    d_meta = t_q[:, :, None] - t_meta[None, None, :]
    ok_band = (d_band >= 0) & (d_band < WINDOW) & (t_band[:, None, :] >= BLOCK)
    ok_meta = d_meta >= 0
    m = slopes.reshape(N_KV_HEADS, Q_GROUP, 1, 1, 1)
    l_band = jnp.where(ok_band, s_band - m * d_band.astype(jnp.float32), NEG_INF)
    l_meta = jnp.where(ok_meta, s_meta - m * d_meta.astype(jnp.float32), NEG_INF)
    l_sink = jnp.broadcast_to(sinks.astype(jnp.float32).reshape(1, N_KV_HEADS, Q_GROUP, 1, 1, 1),
                              l_band.shape[:-1] + (1,))
    probs = jax.nn.softmax(jnp.concatenate([l_band, l_meta, l_sink], axis=-1), axis=-1)
    p_band = probs[..., :2 * BLOCK].astype(v.dtype)
    p_meta = probs[..., 2 * BLOCK:2 * BLOCK + N_META].astype(v.dtype)
    out = (jnp.einsum('bkgnqs,bnskd->bnqkgd', p_band, vb)
           + jnp.einsum('bkgnqm,bmkd->bnqkgd', p_meta, vm))
    return out.reshape(B, P, ATTN_WIDTH)[:, META_PAD:]


def causal_mean_minus_self(u, w):
    L = u.shape[1]
    cs = jnp.cumsum(u, axis=1)
    lagged = jnp.pad(cs[:, :L - w], ((0, 0), (w, 0), (0, 0)))
    count = jnp.minimum(jnp.arange(1, L + 1), w).astype(jnp.float32)[None, :, None]
    return (cs - lagged) / count - u


def multiscale_pool(u):
    uf = u.astype(jnp.float32)
    groups = [causal_mean_minus_self(uf[..., g * POOL_GROUP_DIM:(g + 1) * POOL_GROUP_DIM], POOL_WINDOWS[g])
              for g in range(N_POOL_GROUPS)]
    return jnp.concatenate(groups, axis=-1).astype(u.dtype)


def hybrid_mixer(h, gain, w_in, sinks, w_attn_br, w_pool_grp, pool_scale, w_out, slopes):
    B, L = h.shape[0], h.shape[1]
    xn = rms_norm(h, gain)
    proj = xn @ w_in
    i0 = ATTN_WIDTH
    i1 = i0 + KV_WIDTH
    i2 = i1 + KV_WIDTH
    i3 = i2 + POOL_WIDTH
    q = proj[..., :i0].reshape(B, L, N_HEADS, HEAD_DIM)
    k = proj[..., i0:i1].reshape(B, L, N_KV_HEADS, HEAD_DIM)
    v = proj[..., i1:i2].reshape(B, L, N_KV_HEADS, HEAD_DIM)
    u = proj[..., i2:i3]
    gates = jax.nn.sigmoid(proj[..., i3:].astype(jnp.float32)).astype(h.dtype).reshape(B, L, N_BRANCHES, D_MODEL)
    a_branch = swa_sink_attention(q, k, v, sinks, slopes) @ w_attn_br
    pooled = multiscale_pool(u).reshape(B, L, N_POOL_GROUPS, POOL_GROUP_DIM)
    p_branch = jnp.einsum('blgc,gcd->blgd', pooled, w_pool_grp).reshape(B, L, D_MODEL) * pool_scale
    merged = gates[..., 0, :] * a_branch + gates[..., 1, :] * p_branch
    return merged @ w_out


def swiglu(x, wg, wu, wd):
    return (jax.nn.silu(x @ wg) * (x @ wu)) @ wd


def moe_swiglu(xn, router, wg, wu, wd):
    logits = (xn @ router).astype(jnp.float32)
    top_vals, top_idx = lax.top_k(logits, TOP_K)
    weights = jax.nn.softmax(top_vals, axis=-1)
    combine = jnp.sum(jax.nn.one_hot(top_idx, N_EXPERTS, dtype=jnp.float32) * weights[..., None], axis=-2)
    combine = combine.astype(xn.dtype)
    y = jnp.zeros_like(xn)
    for e in range(N_EXPERTS):
        y = y + combine[..., e:e + 1] * swiglu(xn, wg[e], wu[e], wd[e])
    return y


def setup_inputs(seed: int = 0) -> dict:
    key = jax.random.key(seed)
    ks = jax.random.split(key, 18)
    f32 = jnp.float32

    def nrm(k, shape, fan_in):
        return jax.random.normal(k, shape, f32) * (fan_in ** -0.5)

    return {
        "x": jax.random.normal(ks[0], (BATCH, SEQ, D_MODEL), f32),
        "meta_tokens": jax.random.normal(ks[1], (N_META, D_MODEL), f32),
        "norm_mix": 1.0 + 0.02 * jax.random.normal(ks[2], (DEPTH, D_MODEL), f32),
        "w_in": nrm(ks[3], (DEPTH, D_MODEL, IN_WIDTH), D_MODEL),
        "attn_sinks": 0.5 * jax.random.normal(ks[4], (DEPTH, N_HEADS), f32),
        "w_attn_br": nrm(ks[5], (DEPTH, ATTN_WIDTH, D_MODEL), ATTN_WIDTH),
        "w_pool_grp": nrm(ks[6], (DEPTH, N_POOL_GROUPS, POOL_GROUP_DIM, POOL_GROUP_OUT), POOL_GROUP_DIM),
        "pool_scale": 1.0 + 0.1 * jax.random.normal(ks[7], (DEPTH, D_MODEL), f32),
        "w_out": nrm(ks[8], (DEPTH, D_MODEL, D_MODEL), D_MODEL),
        "norm_ffn": 1.0 + 0.02 * jax.random.normal(ks[9], (DEPTH, D_MODEL), f32),
        "dense_w_gate": nrm(ks[10], (N_DENSE, D_MODEL, D_FF), D_MODEL),
        "dense_w_up": nrm(ks[11], (N_DENSE, D_MODEL, D_FF), D_MODEL),
        "dense_w_down": nrm(ks[12], (N_DENSE, D_FF, D_MODEL), D_FF),
        "moe_router": nrm(ks[13], (N_MOE, D_MODEL, N_EXPERTS), D_MODEL),
        "moe_w_gate": nrm(ks[14], (N_MOE, N_EXPERTS, D_MODEL, D_FF_EXPERT), D_MODEL),
        "moe_w_up": nrm(ks[15], (N_MOE, N_EXPERTS, D_MODEL, D_FF_EXPERT), D_MODEL),
        "moe_w_down": nrm(ks[16], (N_MOE, N_EXPERTS, D_FF_EXPERT, D_MODEL), D_FF_EXPERT),
        "norm_final": 1.0 + 0.02 * jax.random.normal(ks[17], (D_MODEL,), f32),
    }


def reference(x, meta_tokens, norm_mix, w_in, attn_sinks, w_attn_br, w_pool_grp, pool_scale, w_out,
              norm_ffn, dense_w_gate, dense_w_up, dense_w_down, moe_router, moe_w_gate, moe_w_up,
              moe_w_down, norm_final):
    B = x.shape[0]
    slopes = alibi_slopes()
    meta = jnp.broadcast_to(meta_tokens.astype(x.dtype)[None], (B, N_META, D_MODEL))
    h = jnp.concatenate([meta, x], axis=1)
    for layer in range(DEPTH):
        h = h + hybrid_mixer(h, norm_mix[layer], w_in[layer], attn_sinks[layer], w_attn_br[layer],
                             w_pool_grp[layer], pool_scale[layer], w_out[layer], slopes)
        hn = rms_norm(h, norm_ffn[layer])
        if layer % 2 == 0:
            j = layer // 2
            h = h + swiglu(hn, dense_w_gate[j], dense_w_up[j], dense_w_down[j])
        else:
            j = layer // 2
            h = h + moe_swiglu(hn, moe_router[j], moe_w_gate[j], moe_w_up[j], moe_w_down[j])
    return rms_norm(h, norm_final)[:, N_META:]
```

```python
import numpy as np
from contextlib import ExitStack
import ml_dtypes
import concourse.bass as bass
import concourse.mybir as mybir
from concourse.bass_utils import run_bass_kernel_spmd

F32 = mybir.dt.float32
BF16 = mybir.dt.bfloat16
AF = mybir.ActivationFunctionType
ALU = mybir.AluOpType
AX = mybir.AxisListType
bf = ml_dtypes.bfloat16

D = 1024
NTILES = 65
NEXP = 8
DFF = 2816
DFE = 3584
IN_W = 3840
EPS = 1e-5
NEG = -30000.0


class Prog:
    def __init__(self, nc, es):
        self.nc = nc
        self.es = es
        self.eng = {"pe": nc.tensor, "act": nc.scalar, "dve": nc.vector,
                    "pool": nc.gpsimd, "sp": nc.sync}
        self.ops = []
        self.last_w = {}
        self.readers = {}
        self.sems = {}
        self.cnt = {}
        self.waited = {}
        self.ticket = []
        self.emitted = 0
        self.dma_issued = {}
        self.RING = {"w": 16, "ld": 24, "st": 8}

    def _sem(self, name):
        if name not in self.sems:
            self.sems[name] = self.es.enter_context(self.nc.semaphore(name))
        return self.sems[name]

    def op(self, eng, fn, reads=(), writes=(), dma=None):
        idx = len(self.ops)
        deps = set()
        for r in reads:
            if r in self.last_w:
                deps.add(self.last_w[r])
        for w in writes:
            if w in self.last_w:
                deps.add(self.last_w[w])
            for rd in self.readers.get(w, {}).values():
                deps.add(rd)
        deps.discard(idx)
        self.ops.append(dict(eng=eng, fn=fn, deps=deps, dma=dma, reads=tuple(reads), writes=tuple(writes)))
        rkey = ("d", dma) if dma is not None else ("e", eng)
        for r in reads:
            self.readers.setdefault(r, {})[rkey] = idx
        for w in writes:
            self.last_w[w] = idx
            self.readers[w] = {}
        return idx

    def emit_phase(self):
        ops = self.ops
        lo, hi = self.emitted, len(ops)
        need = [False] * (hi - lo)
        last_of_eng = {}
        for i in range(lo, hi):
            o = ops[i]
            if o["dma"] is None:
                last_of_eng[o["eng"]] = i
            for d in o["deps"]:
                if d < lo:
                    continue
                p = ops[d]
                if p["dma"] is not None:
                    continue
                if p["eng"] == o["eng"] and o["dma"] is None:
                    if p["eng"] == "pe":
                        continue
                    if not (set(p["writes"]) & set(o["reads"])):
                        continue
                need[d - lo] = True
        for e, i in last_of_eng.items():
            need[i - lo] = True
        self.ticket.extend([None] * (hi - lo))
        for i in range(lo, hi):
            o = ops[i]
            if o["dma"] is not None:
                ring = self.RING.get(o["dma"], 8)
                k = self.dma_issued.get(o["dma"], 0)
                self.dma_issued[o["dma"]] = k + 1
                key = "d_%s_%d" % (o["dma"], k % ring)
                self.cnt[key] = self.cnt.get(key, 0) + 16
                self.ticket[i] = (key, self.cnt[key])
            elif need[i - lo]:
                key = "e_" + o["eng"]
                self.cnt[key] = self.cnt.get(key, 0) + 1
                self.ticket[i] = (key, self.cnt[key])
        for i in range(lo, hi):
            o = ops[i]
            e = self.eng[o["eng"]]
            req = {}
            for d in o["deps"]:
                if d < lo:
                    continue
                t = self.ticket[d]
                if t is None:
                    continue
                key, val = t
                if req.get(key, 0) < val:
                    req[key] = val
            for key, val in req.items():
                if self.waited.get((o["eng"], key), 0) >= val:
                    continue
                e.wait_ge(self._sem(key), val)
                self.waited[(o["eng"], key)] = val
            ins = o["fn"]()
            if self.ticket[i] is not None:
                key, val = self.ticket[i]
                ins.then_inc(self._sem(key), 16 if o["dma"] is not None else 1)
        for en, e in self.eng.items():
            for key, val in self.cnt.items():
                if self.waited.get((en, key), 0) >= val:
                    continue
                e.wait_ge(self._sem(key), val)
                self.waited[(en, key)] = val
        self.emitted = hi


def _slopes():
    return np.array([2.0 ** (-8.0 * (h + 1) / 16) for h in range(16)], dtype=np.float64)


def _head_of(g, col):
    par, jj = col // 4, col % 4
    return 8 * g + 2 * jj + par


def make_consts():
    sl = _slopes()
    key = np.arange(128)[:, None]
    q = np.arange(128)[None, :]
    bias_prev = np.zeros((128, 2, 8, 128), np.float32)
    bias_cur = np.zeros((128, 2, 8, 128), np.float32)
    for g in range(2):
        for c in range(8):
            s = sl[_head_of(g, c)]
            d_cur = q - key
            bias_cur[:, g, c, :] = np.where(d_cur >= 0, -s * d_cur, NEG)
            d_prev = q + 128 - key
            bias_prev[:, g, c, :] = np.where(d_prev < 128, -s * d_prev, NEG)
    m = np.arange(16)[:, None]
    bias_meta = np.zeros((16, 2, 8, 128), np.float32)
    bias_meta0 = np.zeros((16, 2, 8, 128), np.float32)
    off = np.zeros((16, NTILES, 2, 8), np.float32)
    for g in range(2):
        for c in range(8):
            s = sl[_head_of(g, c)]
            bias_meta[:, g, c, :] = -s * (16 + q - m)
            d0 = q - 112 - m
            bias_meta0[:, g, c, :] = np.where(d0 >= 0, -s * d0, NEG)
            for n in range(1, NTILES):
                off[:, n, g, c] = -s * 128.0 * (n - 1)
    W = (2, 4, 8, 16)
    pc = np.zeros((128, 4, 128), np.float32)
    pp = np.zeros((128, 4, 128), np.float32)
    pc0 = np.zeros((128, 4, 128), np.float32)
    for gi, w in enumerate(W):
        for t in range(128):
            for s_ in range(max(0, t - w + 1), t + 1):
                pc[s_, gi, t] += 1.0 / w
            pc[t, gi, t] -= 1.0
            for s_ in range(t + 128 - w + 1, 128):
                pp[s_, gi, t] += 1.0 / w
            if t >= 112:
                cnt = min(t - 111, w)
                for s_ in range(max(112, t - w + 1), t + 1):
                    pc0[s_, gi, t] += 1.0 / cnt
                pc0[t, gi, t] -= 1.0
    return {
        "c_identb": np.eye(128, dtype=np.float32).astype(bf),
        "c_identf": np.eye(128, dtype=np.float32),
        "c_bias_prev": bias_prev.reshape(128, 2048),
        "c_bias_cur": bias_cur.reshape(128, 2048),
        "c_bias_meta": bias_meta.reshape(16, 2048),
        "c_bias_meta0": bias_meta0.reshape(16, 2048),
        "c_off": off.reshape(16, NTILES * 16),
        "c_pool_cur": pc.reshape(128, 512).astype(bf),
        "c_pool_prev": pp.reshape(128, 512).astype(bf),
        "c_pool_cur0": pc0.reshape(128, 512).astype(bf),
    }


def build_program(ntiles=NTILES, moe_pass_tiles=16, debug=False, stop_after=4):
    nc = bass.Bass("TRN2", target_bir_lowering=False)
    nreal = ntiles - 1

    def din(name, shape, dt=F32):
        return nc.dram_tensor(name, list(shape), dt, kind="ExternalInput").ap()

    x = din("x", [8192, D])
    meta = din("meta_tokens", [16, D])
    norm_mix = din("norm_mix", [2, D])
    w_in = din("w_in", [2, D, IN_W])
    sinks = din("attn_sinks", [2, 16])
    w_br = din("w_attn_br", [2, D, D])
    w_pool = din("w_pool_grp", [2, 4, 128, 256])
    pool_scale = din("pool_scale", [2, D])
    w_out = din("w_out", [2, D, D])
    norm_ffn = din("norm_ffn", [2, D])
    dwg = din("dense_w_gate", [1, D, DFF])
    dwu = din("dense_w_up", [1, D, DFF])
    dwd = din("dense_w_down", [1, DFF, D])
    router = din("moe_router", [1, D, NEXP])
    mwg = din("moe_w_gate", [1, NEXP, D, DFE])
    mwu = din("moe_w_up", [1, NEXP, D, DFE])
    mwd = din("moe_w_down", [1, NEXP, DFE, D])
    norm_final = din("norm_final", [1, D])
    c_identb = din("c_identb", [128, 128], BF16)
    c_identf = din("c_identf", [128, 128])
    c_bias_prev = din("c_bias_prev", [128, 2048])
    c_bias_cur = din("c_bias_cur", [128, 2048])
    c_bias_meta = din("c_bias_meta", [16, 2048])
    c_bias_meta0 = din("c_bias_meta0", [16, 2048])
    c_off = din("c_off", [16, NTILES * 16])
    c_pool_cur = din("c_pool_cur", [128, 512], BF16)
    c_pool_prev = din("c_pool_prev", [128, 512], BF16)
    c_pool_cur0 = din("c_pool_cur0", [128, 512], BF16)
    out = nc.dram_tensor("out", [8192, D], F32, kind="ExternalOutput").ap()
    hbuf = nc.dram_tensor("hbuf", [NTILES * 128, D], F32, kind="Internal").ap()
    dbg = None
    if debug:
        dbg = nc.dram_tensor("dbg", [3, NTILES * 128, D], F32, kind="ExternalOutput").ap()

    with ExitStack() as top:
        P = Prog(nc, top)

        def sbt(es, name, shape, dt):
            return es.enter_context(nc.sbuf_tensor(name, list(shape), dt))

        ssqtab = sbt(top, "ssqtab", [128, NTILES], F32)
        rstdtab = sbt(top, "rstdtab", [128, NTILES], F32)
        identb = sbt(top, "identb", [128, 128], BF16)
        identf = sbt(top, "identf", [128, 128], F32)
        pg = [top.enter_context(nc.psum_tensor(f"pg{i}", [128, 512], F32)) for i in range(3)]
        psc = top.enter_context(nc.psum_tensor("psc", [128, 1024], F32))
        ppv = top.enter_context(nc.psum_tensor("ppv", [128, 1536], F32))
        pgi = [0]

        def next_pg():
            i = pgi[0] % 3
            pgi[0] += 1
            return pg[i], f"pg{i}"

        P.op("sp", lambda: nc.sync.dma_start(out=identb[:], in_=c_identb[:, :]), writes=["identb"], dma="ld")
        P.op("sp", lambda: nc.sync.dma_start(out=identf[:], in_=c_identf[:, :]), writes=["identf"], dma="ld")

        def rstd_from_ssq(n_lo, n_hi):
            P.op("act", lambda: nc.scalar.activation(out=rstdtab[:, n_lo:n_hi], in_=ssqtab[:, n_lo:n_hi], func=AF.Sqrt,
                                                     bias=epsb[:, 0:1], scale=1.0 / D),
                 reads=["ssqtab", "epsb"], writes=["rstdtab"])
            P.op("dve", lambda: nc.vector.reciprocal(out=rstdtab[:, n_lo:n_hi], in_=rstdtab[:, n_lo:n_hi]),
                 reads=["rstdtab"], writes=["rstdtab"])

        epsb = sbt(top, "epsb", [128, 1], F32)
        P.op("dve", lambda: nc.vector.memset(epsb[:], EPS), writes=["epsb"])

        def tile_src(layer, n):
            if layer == 0 and n >= 1:
                return x[(n - 1) * 128:n * 128, :]
            return hbuf[n * 128:(n + 1) * 128, :]

        with ExitStack() as es:
            hz = sbt(es, "p0_h", [128, 2, D], F32)
            junk = sbt(es, "p0_junk", [128, D], BF16)
            P.op("dve", lambda: nc.vector.memset(hz[:, 0, :], 0.0), writes=["p0h0"])
            P.op("sp", lambda: nc.sync.dma_start(out=hz[112:128, 0, :], in_=meta[:, :]), reads=[], writes=["p0h0"], dma="ld")
            P.op("sp", lambda: nc.sync.dma_start(out=hbuf[0:128, :], in_=hz[:, 0, :]), reads=["p0h0"], writes=["hbuf0"], dma="st")
            P.op("act", lambda: nc.scalar.activation(out=junk[:], in_=hz[:, 0, :], func=AF.Square, accum_out=ssqtab[:, 0:1]),
                 reads=["p0h0"], writes=["p0junk", "ssqtab"])
            for n in range(1, ntiles):
                b = n % 2
                P.op("sp", (lambda n=n, b=b: nc.sync.dma_start(out=hz[:, b, :], in_=x[(n - 1) * 128:n * 128, :])),
                     writes=[f"p0h{b}"], dma="ld")
                P.op("act", (lambda n=n, b=b: nc.scalar.activation(out=junk[:], in_=hz[:, b, :], func=AF.Square,
                                                                   accum_out=ssqtab[:, n:n + 1])),
                     reads=[f"p0h{b}"], writes=["p0junk", "ssqtab"])
            rstd_from_ssq(0, ntiles)
            P.emit_phase()

        def mixer_phase(l):
            with ExitStack() as es:
                Win = sbt(es, f"Win_{l}", [128, 8, IN_W], BF16)
                WkS = sbt(es, f"WkS_{l}", [128, 8, 128], BF16)
                Wbr = sbt(es, f"Wbr_{l}", [128, 8, D], BF16)
                Wout = sbt(es, f"Wout_{l}", [128, 8, D], BF16)
                Wpool = sbt(es, f"Wpool_{l}", [128, 4, 256], BF16)
                gain = sbt(es, f"gain_{l}", [128, D], F32)
                bprev = sbt(es, f"bprev_{l}", [128, 2048], F32)
                bcur = sbt(es, f"bcur_{l}", [128, 2048], F32)
                bmeta = sbt(es, f"bmeta_{l}", [16, 2048], F32)
                offt = sbt(es, f"offt_{l}", [16, NTILES * 16], F32)
                pcur = sbt(es, f"pcur_{l}", [128, 512], BF16)
                pprev = sbt(es, f"pprev_{l}", [128, 512], BF16)
                pcur0 = sbt(es, f"pcur0_{l}", [128, 512], BF16)
                sk_raw = sbt(es, f"sk_raw_{l}", [128, 16], F32)
                sinkexp = sbt(es, f"sinkexp_{l}", [128, 16], F32)
                h_sb = sbt(es, f"h_sb_{l}", [128, 3, D], F32)
                xn = sbt(es, f"xn_{l}", [128, D], BF16)
                xnT = sbt(es, f"xnT_{l}", [128, 2, 8, 128], BF16)
                qT = sbt(es, f"qT_{l}", [128, 2, 8, 128], BF16)
                kTa = sbt(es, f"kTa_{l}", [128, 2, 128], BF16)
                kTb = sbt(es, f"kTb_{l}", [128, 2, 128], BF16)
                kTma = sbt(es, f"kTma_{l}", [128, 16], BF16)
                kTmb = sbt(es, f"kTmb_{l}", [128, 16], BF16)
                vE = sbt(es, f"vE_{l}", [128, 2, 2, 66], BF16)
                vEm = sbt(es, f"vEm_{l}", [16, 2, 66], BF16)
                u_sb = sbt(es, f"u_sb_{l}", [128, 2, 512], BF16)
                gT = sbt(es, f"gT_{l}", [128, 2, 2048], BF16)
                t_sb = sbt(es, f"t_sb_{l}", [128, 2, 1024], F32)
                pTp = sbt(es, f"pTp_{l}", [128, 2048], BF16)
                pTc = sbt(es, f"pTc_{l}", [128, 2048], BF16)
                pTm = sbt(es, f"pTm_{l}", [16, 2048], BF16)
                den = sbt(es, f"den_{l}", [128, 16], F32)
                attn = sbt(es, f"attn_{l}", [128, D], BF16)
                attnT = sbt(es, f"attnT_{l}", [128, 8, 128], BF16)
                m1 = sbt(es, f"m1_{l}", [128, D], F32)
                m2 = sbt(es, f"m2_{l}", [128, 256], F32)
                mergedTM = sbt(es, f"mergedTM_{l}", [128, D], BF16)
                pooledT = sbt(es, f"pooledT_{l}", [128, 4, 128], BF16)
                mergedT = sbt(es, f"mergedT_{l}", [128, 8, 128], BF16)
                hnew = sbt(es, f"hnew_{l}", [128, D], F32)

                P.op("pool", lambda: nc.gpsimd.dma_start(out=Win[:, :, :], in_=w_in[l, :, :].rearrange("(kc p) n -> p kc n", p=128)),
                     writes=["Win"], dma="w")
                P.op("pool", lambda: nc.gpsimd.dma_start(out=WkS[:, :, 0:64], in_=w_in[l, :, 1088:1152].rearrange("(kc p) n -> p kc n", p=128)),
                     writes=["WkS"], dma="w")
                P.op("pool", lambda: nc.gpsimd.dma_start(out=WkS[:, :, 64:128], in_=w_in[l, :, 1024:1088].rearrange("(kc p) n -> p kc n", p=128)),
                     writes=["WkS"], dma="w")
                P.op("pool", lambda: nc.gpsimd.dma_start(out=Wbr[:, :, :], in_=w_br[l, :, :].rearrange("(kc p) n -> p kc n", p=128)),
                     writes=["Wbr"], dma="w")
                P.op("pool", lambda: nc.gpsimd.dma_start(out=Wout[:, :, :], in_=w_out[l, :, :].rearrange("(kc p) n -> p kc n", p=128)),
                     writes=["Wout"], dma="w")
                P.op("pool", lambda: nc.gpsimd.dma_start(out=Wpool[:, :, :], in_=w_pool[l, :, :, :].rearrange("g c d -> c g d")),
                     writes=["Wpool"], dma="w")
                P.op("sp", lambda: nc.sync.dma_start(out=gain[:], in_=norm_mix[l:l + 1, :].to_broadcast([128, D])), writes=["gain"], dma="ld")
                P.op("sp", lambda: nc.sync.dma_start(out=bprev[:], in_=c_bias_prev[:, :]), writes=["bprev"], dma="ld")
                P.op("sp", lambda: nc.sync.dma_start(out=bcur[:], in_=c_bias_cur[:, :]), writes=["bcur"], dma="ld")
                P.op("sp", lambda: nc.sync.dma_start(out=bmeta[:], in_=c_bias_meta0[:, :]), writes=["bmeta"], dma="ld")
                P.op("sp", lambda: nc.sync.dma_start(out=offt[:], in_=c_off[:, :]), writes=["offt"], dma="ld")
                P.op("sp", lambda: nc.sync.dma_start(out=pcur[:], in_=c_pool_cur[:, :]), writes=["pcur"], dma="ld")
                P.op("sp", lambda: nc.sync.dma_start(out=pprev[:], in_=c_pool_prev[:, :]), writes=["pprev"], dma="ld")
                P.op("sp", lambda: nc.sync.dma_start(out=pcur0[:], in_=c_pool_cur0[:, :]), writes=["pcur0"], dma="ld")
                P.op("sp", lambda: nc.sync.dma_start(out=sk_raw[:], in_=sinks[l:l + 1, :].to_broadcast([128, 16])), writes=["sk_raw"], dma="ld")
                P.op("sp", lambda: nc.sync.dma_start(out=hnew[:], in_=pool_scale[l:l + 1, :].to_broadcast([128, D])), writes=["hnew"], dma="ld")
                for g in range(4):
                    P.op("dve", (lambda g=g: nc.vector.tensor_tensor(out=Wpool[:, g, :], in0=Wpool[:, g, :], in1=hnew[:, g * 256:(g + 1) * 256], op=ALU.mult)),
                         reads=["Wpool", "hnew"], writes=["Wpool"])
                P.op("act", lambda: nc.scalar.activation(out=sinkexp[:], in_=sk_raw[:], func=AF.Exp), reads=["sk_raw"], writes=["sinkexp"])
                P.op("dve", lambda: nc.vector.memset(vE[:], 1.0), writes=["vE0", "vE1"])
                P.op("dve", lambda: nc.vector.memset(vEm[:], 1.0), writes=["vEm"])

                def proj_fm(n, wsel, evac_eng, evac_fn, wres, writes=()):
                    b_ = n % 2
                    pt, pk2 = next_pg()
                    for kc in range(8):
                        P.op("pe", (lambda kc=kc, pt=pt, b_=b_: nc.tensor.matmul(pt[:, 0:128], wsel(kc), xnT[:, b_, kc, :], start=(kc == 0), stop=(kc == 7))),
                             reads=[wres, f"xnT{b_}"], writes=[pk2])
                    P.op(evac_eng, (lambda pt=pt: evac_fn(pt[:, 0:128])), reads=[pk2], writes=list(writes))

                def stA(n):
                    hb = n % 3
                    P.op("sp", (lambda: nc.sync.dma_start(out=h_sb[:, hb, :], in_=tile_src(l, n))),
                         reads=[f"hbuf{n}"], writes=[f"h{hb}"], dma="ld")
                    P.op("dve", (lambda: nc.vector.scalar_tensor_tensor(out=xn[:], in0=h_sb[:, hb, :], scalar=rstdtab[:, n:n + 1],
                                                                        in1=gain[:], op0=ALU.mult, op1=ALU.mult)),
                         reads=[f"h{hb}", "rstdtab", "gain"], writes=["xn"])
                    pgt, pk = next_pg()
                    pgb = pgt[:].bitcast(BF16)
                    for kc in range(8):
                        P.op("pe", (lambda kc=kc: nc.tensor.transpose(pgb[:, kc * 128:(kc + 1) * 128], xn[:, kc * 128:(kc + 1) * 128], identb[:])),
                             reads=["xn", "identb"], writes=[pk])
                    P.op("act", (lambda: nc.scalar.copy(out=xnT[:, n % 2, :, :].rearrange("p a b -> p (a b)"), in_=pgb)),
                         reads=[pk], writes=[f"xnT{n % 2}"])

                def b1_chunks(n):
                    b_ = n % 2
                    sl = n % 2
                    jobs = []
                    for j in range(8):
                        jobs.append(lambda j=j: proj_fm(n, (lambda kc, j=j: Win[:, kc, j * 128:(j + 1) * 128]), "dve",
                                                        (lambda p_, j=j: nc.vector.tensor_scalar(qT[:, b_, j, :], p_, 0.125, None, ALU.mult)),
                                                        "Win", writes=[f"qT{b_}"]))
                    jobs.append(lambda: proj_fm(n, (lambda kc: Win[:, kc, 1024:1152]), "act",
                                                (lambda p_: nc.scalar.copy(out=kTa[:, sl, :], in_=p_)), "Win", writes=[f"kTa{sl}"]))
                    jobs.append(lambda: proj_fm(n, (lambda kc: WkS[:, kc, :]), "act",
                                                (lambda p_: nc.scalar.copy(out=kTb[:, sl, :], in_=p_)), "WkS", writes=[f"kTb{sl}"]))
                    return jobs

                def stB1(n):
                    for jb in b1_chunks(n):
                        jb()

                def stB2(n, q_lo, q_hi):
                    b_ = n % 2
                    for qd in range(q_lo, q_hi):
                        pt, pk2 = next_pg()
                        for kc in range(8):
                            P.op("pe", (lambda kc=kc, pt=pt, qd=qd: nc.tensor.matmul(pt[:, 0:512], xnT[:, b_, kc, :], Win[:, kc, 1792 + qd * 512:1792 + (qd + 1) * 512], start=(kc == 0), stop=(kc == 7))),
                                 reads=[f"xnT{b_}", "Win"], writes=[pk2])
                        P.op("act", (lambda pt=pt, qd=qd: nc.scalar.activation(out=gT[:, b_, qd * 512:(qd + 1) * 512], in_=pt[:, 0:512], func=AF.Tanh, scale=0.5)),
                             reads=[pk2], writes=[f"gT{b_}"])

                def stB3(n):
                    b_ = n % 2
                    sl = n % 2
                    pv1, pk1 = next_pg()
                    pv2, pk2_ = next_pg()
                    for kc in range(8):
                        P.op("pe", (lambda kc=kc: nc.tensor.matmul(pv1[:, 0:512], xnT[:, b_, kc, :], Win[:, kc, 1152:1664], start=(kc == 0), stop=(kc == 7))),
                             reads=[f"xnT{b_}", "Win"], writes=[pk1])
                    for kc in range(8):
                        P.op("pe", (lambda kc=kc: nc.tensor.matmul(pv2[:, 0:128], xnT[:, b_, kc, :], Win[:, kc, 1664:1792], start=(kc == 0), stop=(kc == 7))),
                             reads=[f"xnT{b_}", "Win"], writes=[pk2_])
                    for kv in range(2):
                        P.op("act", (lambda kv=kv: nc.scalar.copy(out=vE[:, sl, kv, 0:64], in_=pv1[:, kv * 64:(kv + 1) * 64])),
                             reads=[pk1], writes=[f"vE{sl}"])
                    P.op("act", (lambda: nc.scalar.copy(out=u_sb[:, sl, 0:384], in_=pv1[:, 128:512])),
                         reads=[pk1], writes=[f"u{sl}"])
                    P.op("act", (lambda: nc.scalar.copy(out=u_sb[:, sl, 384:512], in_=pv2[:, 0:128])),
                         reads=[pk2_], writes=[f"u{sl}"])
                    if n == 0:
                        P.op("act", lambda: nc.scalar.copy(out=kTma[:], in_=kTa[:, 0, 112:128]), reads=["kTa0"], writes=["kTma"])
                        P.op("act", lambda: nc.scalar.copy(out=kTmb[:], in_=kTb[:, 0, 112:128]), reads=["kTb0"], writes=["kTmb"])
                        pm, pkm = next_pg()
                        for kc in range(8):
                            P.op("pe", (lambda kc=kc: nc.tensor.matmul(pm[0:16, 0:128], xnT[:, 0, kc, 112:128], Win[:, kc, 1152:1280], start=(kc == 0), stop=(kc == 7))),
                                 reads=["xnT0", "Win"], writes=[pkm])
                        for kv in range(2):
                            P.op("act", (lambda kv=kv: nc.scalar.copy(out=vEm[:, kv, 0:64], in_=pm[0:16, kv * 64:(kv + 1) * 64])),
                                 reads=[pkm], writes=["vEm"])

                def blocks_of(n):
                    blocks = []
                    if n >= 2:
                        blocks.append(("prev", (n - 1) % 2))
                    if n >= 1:
                        blocks.append(("cur", n % 2))
                    blocks.append(("meta", None))
                    return blocks

                def stC(n, fillers=()):
                    b_ = n % 2
                    fillers = list(fillers)
                    blocks = blocks_of(n)
                    nsteps = 2 * len(blocks)
                    step = 0
                    for (bname, bs) in blocks:
                        for g in range(2):
                            tb = step % 2
                            tk = f"t_sb{tb}"
                            for par in range(2):
                                base = par * 64
                                use_a = (g == par)
                                if bname == "meta":
                                    kt = (kTma if use_a else kTmb)[base:base + 64, :]
                                    kres = "kTma" if use_a else "kTmb"
                                    M = 16
                                else:
                                    kt = (kTa if use_a else kTb)[base:base + 64, bs, :]
                                    kres = (f"kTa{bs}" if use_a else f"kTb{bs}")
                                    M = 128
                                P.op("pe", (lambda kt=kt, M=M, par=par, g=g, base=base: nc.tensor.matmul(
                                    psc[0:M, par * 512:(par + 1) * 512], kt, qT[base:base + 64, b_, 4 * g:4 * g + 4, :], start=True, stop=True)),
                                    reads=[kres, f"qT{b_}"], writes=["psc"])
                            if bname == "meta":
                                P.op("dve", (lambda g=g, tb=tb: nc.vector.tensor_tensor(out=t_sb[0:16, tb, :], in0=psc[0:16, :], in1=bmeta[:, g * 1024:(g + 1) * 1024], op=ALU.add)),
                                     reads=["psc", "bmeta"], writes=[tk])
                                if n >= 2:
                                    P.op("dve", (lambda g=g, tb=tb: nc.vector.tensor_tensor(
                                        out=t_sb[0:16, tb, :].rearrange("p (c q) -> p c q", c=8),
                                        in0=t_sb[0:16, tb, :].rearrange("p (c q) -> p c q", c=8),
                                        in1=offt[:, n * 16 + g * 8:n * 16 + g * 8 + 8].unsqueeze(2).to_broadcast([16, 8, 128]), op=ALU.add)),
                                        reads=[tk, "offt"], writes=[tk])
                                P.op("act", (lambda g=g, tb=tb: nc.scalar.activation(out=pTm[:, g * 1024:(g + 1) * 1024], in_=t_sb[0:16, tb, :], func=AF.Exp)),
                                     reads=[tk], writes=["pTm"])
                            else:
                                btab = bprev if bname == "prev" else bcur
                                pdst = pTp if bname == "prev" else pTc
                                P.op("dve", (lambda g=g, btab=btab, tb=tb: nc.vector.tensor_tensor(out=t_sb[:, tb, :], in0=psc[:], in1=btab[:, g * 1024:(g + 1) * 1024], op=ALU.add)),
                                     reads=["psc", "bprev" if bname == "prev" else "bcur"], writes=[tk])
                                P.op("act", (lambda g=g, pdst=pdst, tb=tb: nc.scalar.activation(out=pdst[:, g * 1024:(g + 1) * 1024], in_=t_sb[:, tb, :], func=AF.Exp)),
                                     reads=[tk], writes=["pTp" if bname == "prev" else "pTc"])
                            step += 1
                            if fillers:
                                k = -(-len(fillers) // (nsteps - step + 1))
                                for _ in range(k):
                                    fillers.pop(0)()
                    while fillers:
                        fillers.pop(0)()
                    if n == 0:
                        P.op("sp", lambda: nc.sync.dma_start(out=bmeta[:], in_=c_bias_meta[:, :]), writes=["bmeta"], dma="ld")

                def stD(n):
                    blocks = blocks_of(n)
                    for h in range(16):
                        g = h // 8
                        par = h % 2
                        jj = (h % 8) // 2
                        col = g * 1024 + par * 512 + jj * 128
                        bank, hh = h // 7, h % 7
                        o = ppv[:, bank * 512 + hh * 65:bank * 512 + hh * 65 + 65]
                        seq = []
                        for (bname, bs) in blocks:
                            if bname == "meta":
                                seq.append((pTm[:, col:col + 128], vEm[:, g, 0:65], "pTm", "vEm"))
                            elif bname == "prev":
                                seq.append((pTp[:, col:col + 128], vE[:, bs, g, 0:65], "pTp", f"vE{bs}"))
                            else:
                                seq.append((pTc[:, col:col + 128], vE[:, bs, g, 0:65], "pTc", f"vE{bs}"))
                        for i, (lt, rt, r1, r2) in enumerate(seq):
                            P.op("pe", (lambda o=o, lt=lt, rt=rt, i=i, L=len(seq): nc.tensor.matmul(o, lt, rt, start=(i == 0), stop=(i == L - 1))),
                                 reads=[r1, r2], writes=["ppv"])
                    for bank, nh in ((0, 7), (1, 7), (2, 2)):
                        P.op("dve", (lambda bank=bank, nh=nh: nc.vector.tensor_tensor(
                            out=den[:, bank * 7:bank * 7 + nh],
                            in0=ppv[:, bank * 512:bank * 512 + nh * 65].rearrange("p (h e) -> p h e", e=65)[:, :, 64],
                            in1=sinkexp[:, bank * 7:bank * 7 + nh], op=ALU.add)),
                            reads=["ppv", "sinkexp"], writes=["den"])
                    P.op("dve", lambda: nc.vector.reciprocal(out=den[:], in_=den[:]), reads=["den"], writes=["den"])
                    for bank, nh in ((0, 7), (1, 7), (2, 2)):
                        P.op("dve", (lambda bank=bank, nh=nh: nc.vector.tensor_tensor(
                            out=attn[:, bank * 448:bank * 448 + nh * 64].rearrange("p (h e) -> p h e", e=64),
                            in0=ppv[:, bank * 512:bank * 512 + nh * 65].rearrange("p (h e) -> p h e", e=65)[:, :, 0:64],
                            in1=den[:, bank * 7:bank * 7 + nh].unsqueeze(2).to_broadcast([128, nh, 64]), op=ALU.mult)),
                            reads=["ppv", "den"], writes=["attn"])
                    pgt, pk = next_pg()
                    pgb = pgt[:].bitcast(BF16)
                    for kc in range(8):
                        P.op("pe", (lambda kc=kc: nc.tensor.transpose(pgb[:, kc * 128:(kc + 1) * 128], attn[:, kc * 128:(kc + 1) * 128], identb[:])),
                             reads=["attn", "identb"], writes=[pk])
                    P.op("act", (lambda: nc.scalar.copy(out=attnT[:].rearrange("p a b -> p (a b)"), in_=pgb)),
                         reads=[pk], writes=["attnT"])

                def stE(n):
                    b_ = n % 2
                    for half in range(2):
                        pt, pk2 = next_pg()
                        for kc in range(8):
                            P.op("pe", (lambda kc=kc, pt=pt, half=half: nc.tensor.matmul(pt[:, 0:512], attnT[:, kc, :], Wbr[:, kc, half * 512:(half + 1) * 512], start=(kc == 0), stop=(kc == 7))),
                                 reads=["Wbr", "attnT"], writes=[pk2])
                        P.op("dve", (lambda pt=pt, half=half: nc.vector.scalar_tensor_tensor(out=m1[:, half * 512:(half + 1) * 512], in0=gT[:, b_, half * 512:(half + 1) * 512], scalar=1.0,
                                                                                            in1=pt[:, 0:512], op0=ALU.add, op1=ALU.mult)),
                             reads=[pk2, f"gT{b_}"], writes=["m1"])

                def stF1(n):
                    sl = n % 2
                    slp = (n - 1) % 2
                    pt, pk2 = next_pg()
                    for g in range(4):
                        pm_cur = (pcur0 if n == 0 else pcur)
                        two = (n >= 1)
                        P.op("pe", (lambda g=g, pm_cur=pm_cur, two=two: nc.tensor.matmul(
                            pt[:, g * 128:(g + 1) * 128], u_sb[:, sl, g * 128:(g + 1) * 128], pm_cur[:, g * 128:(g + 1) * 128], start=True, stop=(not two))),
                            reads=[f"u{sl}", "pcur0" if n == 0 else "pcur"], writes=[pk2])
                        if two:
                            P.op("pe", (lambda g=g: nc.tensor.matmul(
                                pt[:, g * 128:(g + 1) * 128], u_sb[:, slp, g * 128:(g + 1) * 128], pprev[:, g * 128:(g + 1) * 128], start=False, stop=True)),
                                reads=[f"u{slp}", "pprev"], writes=[pk2])
                    P.op("act", (lambda: nc.scalar.copy(out=pooledT[:].rearrange("p a b -> p (a b)"), in_=pt[:, 0:512])),
                         reads=[pk2], writes=["pooledT"])

                def stF2(n):
                    b_ = n % 2
                    for g in range(4):
                        pt, pk2 = next_pg()
                        P.op("pe", (lambda pt=pt, g=g: nc.tensor.matmul(pt[:, 0:256], pooledT[:, g, :], Wpool[:, g, :], start=True, stop=True)),
                             reads=["Wpool", "pooledT"], writes=[pk2])
                        P.op("dve", (lambda pt=pt, g=g: nc.vector.scalar_tensor_tensor(out=m2[:], in0=gT[:, b_, 1024 + g * 256:1024 + (g + 1) * 256], scalar=1.0, in1=pt[:, 0:256], op0=ALU.add, op1=ALU.mult)),
                             reads=[pk2, f"gT{b_}"], writes=["m2"])
                        P.op("dve", (lambda g=g: nc.vector.tensor_tensor(out=mergedTM[:, g * 256:(g + 1) * 256], in0=m2[:], in1=m1[:, g * 256:(g + 1) * 256], op=ALU.add)),
                             reads=["m2", "m1"], writes=["mergedTM"])
                    pgt, pk = next_pg()
                    pgb = pgt[:].bitcast(BF16)
                    for kc in range(8):
                        P.op("pe", (lambda kc=kc: nc.tensor.transpose(pgb[:, kc * 128:(kc + 1) * 128], mergedTM[:, kc * 128:(kc + 1) * 128], identb[:])),
                             reads=["mergedTM", "identb"], writes=[pk])
                    P.op("act", (lambda: nc.scalar.copy(out=mergedT[:].rearrange("p a b -> p (a b)"), in_=pgb)),
                         reads=[pk], writes=["mergedT"])

                def stG(n):
                    hb = n % 3
                    for half in range(2):
                        pt, pk2 = next_pg()
                        for kc in range(8):
                            P.op("pe", (lambda kc=kc, pt=pt, half=half: nc.tensor.matmul(pt[:, 0:512], mergedT[:, kc, :], Wout[:, kc, half * 512:(half + 1) * 512], start=(kc == 0), stop=(kc == 7))),
                                 reads=["mergedT", "Wout"], writes=[pk2])
                        P.op("dve", (lambda pt=pt, half=half: nc.vector.scalar_tensor_tensor(out=hnew[:, half * 512:(half + 1) * 512], in0=pt[:, 0:512], scalar=0.5,
                                                                                            in1=h_sb[:, hb, half * 512:(half + 1) * 512], op0=ALU.mult, op1=ALU.add)),
                             reads=[pk2, f"h{hb}"], writes=["hnew"])
                    P.op("act", (lambda: nc.scalar.activation(out=mergedTM[:], in_=hnew[:], func=AF.Square, accum_out=ssqtab[:, n:n + 1])),
                         reads=["hnew"], writes=["mergedTM", "ssqtab"])
                    P.op("sp", (lambda: nc.sync.dma_start(out=hbuf[n * 128:(n + 1) * 128, :], in_=hnew[:])),
                         reads=["hnew"], writes=[f"hbuf{n}"], dma="st")
                    if dbg is not None:
                        P.op("sp", (lambda: nc.sync.dma_start(out=dbg[l, n * 128:(n + 1) * 128, :], in_=hnew[:])),
                             reads=["hnew"], writes=[], dma="st")

                stA(0); stB1(0); stB2(0, 0, 4); stB3(0)
                if ntiles > 1:
                    stA(1)
                for n in range(ntiles):
                    nxt = n + 1 if n + 1 < ntiles else None
                    stC(n, b1_chunks(nxt) if nxt is not None else ())
                    stD(n)
                    stF1(n)
                    if nxt is not None:
                        stB2(nxt, 0, 2)
                    stE(n)
                    if nxt is not None:
                        stB2(nxt, 2, 4)
                        stB3(nxt)
                    if n + 2 < ntiles:
                        stA(n + 2)
                    stF2(n)
                    stG(n)
                rstd_from_ssq(0, ntiles)
                P.emit_phase()

        def ffn_phase():
            NF = DFF // 128
            with ExitStack() as es:
                Wg = sbt(es, "Wg", [128, 8, DFF], BF16)
                Wu = sbt(es, "Wu", [128, 8, DFF], BF16)
                Wd = sbt(es, "Wd", [128, NF, D], BF16)
                gain = sbt(es, "fgain", [128, D], F32)
                h_sb = sbt(es, "fh", [128, 4, D], F32)
                xn = sbt(es, "fxn", [128, D], BF16)
                xnT = sbt(es, "fxnT", [128, 8, 512], BF16)
                hT = sbt(es, "fhT", [128, NF, 512], BF16)
                sg = sbt(es, "fsg", [128, 2, 512], F32)
                junk = sbt(es, "fjunk", [128, D], BF16)
                P.op("pool", lambda: nc.gpsimd.dma_start(out=Wg[:, :, :], in_=dwg[0, :, :].rearrange("(kc p) n -> p kc n", p=128)), writes=["Wg"], dma="w")
                P.op("pool", lambda: nc.gpsimd.dma_start(out=Wu[:, :, :], in_=dwu[0, :, :].rearrange("(kc p) n -> p kc n", p=128)), writes=["Wu"], dma="w")
                P.op("pool", lambda: nc.gpsimd.dma_start(out=Wd[:, :, :], in_=dwd[0, :, :].rearrange("(f p) n -> p f n", p=128)), writes=["Wd"], dma="w")
                P.op("sp", lambda: nc.sync.dma_start(out=gain[:], in_=norm_ffn[0:1, :].to_broadcast([128, D])), writes=["fgain"], dma="ld")
                groups = [[0]] + [list(range(s, min(s + 4, ntiles))) for s in range(1, ntiles, 4)]
                for tiles in groups:
                    nt = len(tiles)
                    N = nt * 128
                    for i, n in enumerate(tiles):
                        P.op("sp", (lambda n=n, i=i: nc.sync.dma_start(out=h_sb[:, i, :], in_=hbuf[n * 128:(n + 1) * 128, :])),
                             reads=[f"hbuf{n}"], writes=[f"fh{i}"], dma="ld")
                        P.op("dve", (lambda n=n, i=i: nc.vector.scalar_tensor_tensor(out=xn[:], in0=h_sb[:, i, :], scalar=rstdtab[:, n:n + 1], in1=gain[:], op0=ALU.mult, op1=ALU.mult)),
                             reads=[f"fh{i}", "rstdtab", "fgain"], writes=["fxn"])
                        pgt, pk = next_pg()
                        pgb = pgt[:].bitcast(BF16)
                        for kc in range(8):
                            P.op("pe", (lambda kc=kc, pgb=pgb: nc.tensor.transpose(pgb[:, kc * 128:(kc + 1) * 128], xn[:, kc * 128:(kc + 1) * 128], identb[:])),
                                 reads=["fxn", "identb"], writes=[pk])
                        P.op("act", (lambda pgb=pgb, i=i: nc.scalar.copy(out=xnT[:, :, i * 128:(i + 1) * 128], in_=pgb.rearrange("p (a b) -> p a b", a=8))),
                             reads=[pk], writes=["fxnT"])
                    for f in range(NF):
                        sb_ = f % 2
                        for kc in range(8):
                            P.op("pe", (lambda kc=kc, f=f, N=N: nc.tensor.matmul(psc[:, 0:N], Wg[:, kc, f * 128:(f + 1) * 128], xnT[:, kc, 0:N], start=(kc == 0), stop=(kc == 7))),
                                 reads=["Wg", "fxnT"], writes=["pscA"])
                        for kc in range(8):
                            P.op("pe", (lambda kc=kc, f=f, N=N: nc.tensor.matmul(psc[:, 512:512 + N], Wu[:, kc, f * 128:(f + 1) * 128], xnT[:, kc, 0:N], start=(kc == 0), stop=(kc == 7))),
                                 reads=["Wu", "fxnT"], writes=["pscB"])
                        P.op("act", (lambda sb_=sb_, N=N: nc.scalar.activation(out=sg[:, sb_, 0:N], in_=psc[:, 0:N], func=AF.Silu)),
                             reads=["pscA"], writes=[f"fsg{sb_}"])
                        P.op("dve", (lambda sb_=sb_, f=f, N=N: nc.vector.tensor_tensor(out=hT[:, f, 0:N], in0=psc[:, 512:512 + N], in1=sg[:, sb_, 0:N], op=ALU.mult)),
                             reads=["pscB", f"fsg{sb_}"], writes=["fhT"])
                    for i, n in enumerate(tiles):
                        for half in range(2):
                            pt, pk2 = next_pg()
                            for f in range(NF):
                                P.op("pe", (lambda f=f, pt=pt, half=half, i=i: nc.tensor.matmul(pt[:, 0:512], hT[:, f, i * 128:(i + 1) * 128], Wd[:, f, half * 512:(half + 1) * 512], start=(f == 0), stop=(f == NF - 1))),
                                     reads=["fhT", "Wd"], writes=[pk2])
                            P.op("dve", (lambda pt=pt, half=half, i=i: nc.vector.tensor_tensor(out=h_sb[:, i, half * 512:(half + 1) * 512], in0=pt[:, 0:512], in1=h_sb[:, i, half * 512:(half + 1) * 512], op=ALU.add)),
                                 reads=[pk2, f"fh{i}"], writes=[f"fh{i}"])
                        P.op("act", (lambda n=n, i=i: nc.scalar.activation(out=junk[:], in_=h_sb[:, i, :], func=AF.Square, accum_out=ssqtab[:, n:n + 1])),
                             reads=[f"fh{i}"], writes=["fjunk", "ssqtab"])
                        P.op("sp", (lambda n=n, i=i: nc.sync.dma_start(out=hbuf[n * 128:(n + 1) * 128, :], in_=h_sb[:, i, :])),
                             reads=[f"fh{i}"], writes=[f"hbuf{n}"], dma="st")
                        if dbg is not None:
                            P.op("sp", (lambda n=n, i=i: nc.sync.dma_start(out=dbg[2, n * 128:(n + 1) * 128, :], in_=h_sb[:, i, :])),
                                 reads=[f"fh{i}"], writes=[], dma="st")
                rstd_from_ssq(0, ntiles)
                P.emit_phase()

        def moe_phase():
            NG = 7
            PT = moe_pass_tiles
            with ExitStack() as es:
                Wg = sbt(es, "eWg", [128, 2, 8, 512], BF16)
                Wu = sbt(es, "eWu", [128, 2, 8, 512], BF16)
                Wd = sbt(es, "eWd", [128, 2, 4, D], BF16)
                gain = sbt(es, "egain", [128, D], F32)
                gainf = sbt(es, "egainf", [128, D], F32)
                Rt = sbt(es, "eR", [128, 8, NEXP], F32)
                h_sb = sbt(es, "eh", [128, 2, D], F32)
                xnf = sbt(es, "exnf", [128, D], F32)
                xnTf = sbt(es, "exnTf", [128, 8, 128], F32)
                xnT = sbt(es, "exnT", [128, 8, PT * 128], BF16)
                yacc = sbt(es, "eyacc", [128, PT, D], F32)
                hT = sbt(es, "ehT", [128, 2, 4, 512], BF16)
                sg = sbt(es, "esg", [128, 2, 512], F32)
                lg = sbt(es, "elg", [128, PT, NEXP], F32)
                comb = sbt(es, "ecomb", [128, PT, NEXP], F32)
                tmp8 = sbt(es, "etmp8", [128, PT, NEXP], F32)
                v1 = sbt(es, "ev1", [128, PT], F32)
                v2 = sbt(es, "ev2", [128, PT], F32)
                fssq = sbt(es, "efssq", [128, PT], F32)
                frstd = sbt(es, "efrstd", [128, PT], F32)
                junk = sbt(es, "ejunk", [128, D], BF16)
                ob = sbt(es, "eob", [128, 2, D], F32)
                P.op("sp", lambda: nc.sync.dma_start(out=gain[:], in_=norm_ffn[1:2, :].to_broadcast([128, D])), writes=["egain"], dma="ld")
                P.op("sp", lambda: nc.sync.dma_start(out=gainf[:], in_=norm_final[0:1, :].to_broadcast([128, D])), writes=["egainf"], dma="ld")
                with nc.allow_non_contiguous_dma(reason="tiny router"):
                    P.op("sp", lambda: nc.sync.dma_start(out=Rt[:], in_=router[0, :, :].rearrange("(c p) e -> p c e", p=128)), writes=["eR"], dma="ld")
                npass = (nreal + PT - 1) // PT
                wctr = [0]
                for ps_i in range(npass):
                    tiles = list(range(1 + ps_i * PT, min(1 + (ps_i + 1) * PT, ntiles)))
                    ntl = len(tiles)
                    for i, n in enumerate(tiles):
                        hb = i % 2
                        P.op("sp", (lambda n=n, hb=hb: nc.sync.dma_start(out=h_sb[:, hb, :], in_=hbuf[n * 128:(n + 1) * 128, :])),
                             reads=[f"hbuf{n}"], writes=[f"eh{hb}"], dma="ld")
                        P.op("dve", (lambda n=n, hb=hb: nc.vector.scalar_tensor_tensor(out=xnf[:], in0=h_sb[:, hb, :], scalar=rstdtab[:, n:n + 1], in1=gain[:], op0=ALU.mult, op1=ALU.mult)),
                             reads=[f"eh{hb}", "rstdtab", "egain"], writes=["exnf"])
                        for kc in range(8):
                            P.op("pe", (lambda kc=kc: nc.tensor.transpose(psc[:, kc * 128:(kc + 1) * 128], xnf[:, kc * 128:(kc + 1) * 128], identf[:])),
                                 reads=["exnf", "identf"], writes=["pscA", "pscB"])
                        P.op("act", (lambda i=i: nc.scalar.copy(out=xnT[:, :, i * 128:(i + 1) * 128], in_=psc[:].rearrange("p (a b) -> p a b", a=8))),
                             reads=["pscA", "pscB"], writes=["exnT"])
                        P.op("act", lambda: nc.scalar.copy(out=xnTf[:].rearrange("p a b -> p (a b)"), in_=psc[:]),
                             reads=["pscA", "pscB"], writes=["exnTf"])
                        pt, pk2 = next_pg()
                        for kc in range(8):
                            P.op("pe", (lambda kc=kc, pt=pt: nc.tensor.matmul(pt[:, 0:NEXP], xnTf[:, kc, :], Rt[:, kc, :], start=(kc == 0), stop=(kc == 7))),
                                 reads=["exnTf", "eR"], writes=[pk2])
                        P.op("act", (lambda pt=pt, i=i: nc.scalar.copy(out=lg[:, i, :], in_=pt[:, 0:NEXP])), reads=[pk2], writes=["elg"])
                    L = lg[:, 0:ntl, :]
                    C = comb[:, 0:ntl, :]
                    T8 = tmp8[:, 0:ntl, :]
                    V1 = v1[:, 0:ntl]
                    V2 = v2[:, 0:ntl]
                    bc = (lambda v, ntl=ntl: v.unsqueeze(2).to_broadcast([128, ntl, NEXP]))
                    P.op("dve", lambda L=L, C=C, T8=T8, V1=V1, V2=V2, bc=bc: nc.vector.tensor_reduce(out=V1, in_=L, axis=AX.X, op=ALU.max), reads=["elg"], writes=["ev1"])
                    P.op("dve", lambda L=L, C=C, T8=T8, V1=V1, V2=V2, bc=bc: nc.vector.tensor_tensor(out=T8, in0=L, in1=bc(V1), op=ALU.is_equal), reads=["elg", "ev1"], writes=["etmp8"])
                    P.op("dve", lambda L=L, C=C, T8=T8, V1=V1, V2=V2, bc=bc: nc.vector.scalar_tensor_tensor(out=T8, in0=T8, scalar=-1e30, in1=L, op0=ALU.mult, op1=ALU.add), reads=["etmp8", "elg"], writes=["etmp8"])
                    P.op("dve", lambda L=L, C=C, T8=T8, V1=V1, V2=V2, bc=bc: nc.vector.tensor_reduce(out=V2, in_=T8, axis=AX.X, op=ALU.max), reads=["etmp8"], writes=["ev2"])
                    P.op("dve", lambda L=L, C=C, T8=T8, V1=V1, V2=V2, bc=bc: nc.vector.tensor_tensor(out=T8, in0=L, in1=bc(V2), op=ALU.is_ge), reads=["elg", "ev2", "etmp8"], writes=["etmp8"])
                    P.op("dve", lambda L=L, C=C, T8=T8, V1=V1, V2=V2, bc=bc: nc.vector.tensor_tensor(out=C, in0=L, in1=bc(V1), op=ALU.subtract), reads=["elg", "ev1"], writes=["ecomb"])
                    P.op("act", lambda L=L, C=C, T8=T8, V1=V1, V2=V2, bc=bc: nc.scalar.activation(out=C, in_=C, func=AF.Exp), reads=["ecomb"], writes=["ecomb"])
                    P.op("dve", lambda L=L, C=C, T8=T8, V1=V1, V2=V2, bc=bc: nc.vector.tensor_tensor(out=C, in0=C, in1=T8, op=ALU.mult), reads=["ecomb", "etmp8"], writes=["ecomb"])
                    P.op("dve", lambda L=L, C=C, T8=T8, V1=V1, V2=V2, bc=bc: nc.vector.tensor_reduce(out=V1, in_=C, axis=AX.X, op=ALU.add), reads=["ecomb"], writes=["ev1"])
                    P.op("dve", lambda L=L, C=C, T8=T8, V1=V1, V2=V2, bc=bc: nc.vector.reciprocal(out=V1, in_=V1), reads=["ev1"], writes=["ev1"])
                    P.op("dve", lambda L=L, C=C, T8=T8, V1=V1, V2=V2, bc=bc: nc.vector.tensor_tensor(out=C, in0=C, in1=bc(V1), op=ALU.mult), reads=["ecomb", "ev1"], writes=["ecomb"])
                    first = True
                    for e in range(NEXP):
                        for gq in range(NG):
                            wb = wctr[0] % 2
                            wctr[0] += 1
                            c0 = gq * 512
                            P.op("pool", (lambda wb=wb, e=e, c0=c0: nc.gpsimd.dma_start(out=Wg[:, wb, :, :], in_=mwg[0, e, :, c0:c0 + 512].rearrange("(kc p) n -> p kc n", p=128))),
                                 writes=[f"eWg{wb}"], dma="w")
                            P.op("pool", (lambda wb=wb, e=e, c0=c0: nc.gpsimd.dma_start(out=Wu[:, wb, :, :], in_=mwu[0, e, :, c0:c0 + 512].rearrange("(kc p) n -> p kc n", p=128))),
                                 writes=[f"eWu{wb}"], dma="w")
                            P.op("pool", (lambda wb=wb, e=e, c0=c0: nc.gpsimd.dma_start(out=Wd[:, wb, :, :], in_=mwd[0, e, c0:c0 + 512, :].rearrange("(f p) n -> p f n", p=128))),
                                 writes=[f"eWd{wb}"], dma="w")
                            for s0 in range(0, ntl, 4):
                                nt = min(4, ntl - s0)
                                N = nt * 128
                                hb2 = (s0 // 4) % 2
                                for f in range(4):
                                    sb_ = f % 2
                                    for kc in range(8):
                                        P.op("pe", (lambda kc=kc, f=f, wb=wb, s0=s0, N=N: nc.tensor.matmul(psc[:, 0:N], Wg[:, wb, kc, f * 128:(f + 1) * 128], xnT[:, kc, s0 * 128:s0 * 128 + N], start=(kc == 0), stop=(kc == 7))),
                                             reads=[f"eWg{wb}", "exnT"], writes=["pscA"])
                                    for kc in range(8):
                                        P.op("pe", (lambda kc=kc, f=f, wb=wb, s0=s0, N=N: nc.tensor.matmul(psc[:, 512:512 + N], Wu[:, wb, kc, f * 128:(f + 1) * 128], xnT[:, kc, s0 * 128:s0 * 128 + N], start=(kc == 0), stop=(kc == 7))),
                                             reads=[f"eWu{wb}", "exnT"], writes=["pscB"])
                                    P.op("act", (lambda sb_=sb_, N=N: nc.scalar.activation(out=sg[:, sb_, 0:N], in_=psc[:, 0:N], func=AF.Silu)),
                                         reads=["pscA"], writes=[f"esg{sb_}"])
                                    P.op("dve", (lambda sb_=sb_, f=f, N=N, hb2=hb2: nc.vector.tensor_tensor(out=hT[:, hb2, f, 0:N], in0=psc[:, 512:512 + N], in1=sg[:, sb_, 0:N], op=ALU.mult)),
                                         reads=["pscB", f"esg{sb_}"], writes=[f"ehT{hb2}"])
                                for i in range(nt):
                                    ti = s0 + i
                                    for half in range(2):
                                        pt, pk2 = next_pg()
                                        for f in range(4):
                                            P.op("pe", (lambda f=f, pt=pt, half=half, i=i, wb=wb, hb2=hb2: nc.tensor.matmul(pt[:, 0:512], hT[:, hb2, f, i * 128:(i + 1) * 128], Wd[:, wb, f, half * 512:(half + 1) * 512], start=(f == 0), stop=(f == 3))),
                                                 reads=[f"ehT{hb2}", f"eWd{wb}"], writes=[pk2])
                                        if first:
                                            P.op("dve", (lambda pt=pt, half=half, ti=ti, e=e: nc.vector.tensor_scalar(yacc[:, ti, half * 512:(half + 1) * 512], pt[:, 0:512], comb[:, ti, e:e + 1], None, ALU.mult)),
                                                 reads=[pk2, "ecomb"], writes=[f"ey{ti}"])
                                        else:
                                            P.op("dve", (lambda pt=pt, half=half, ti=ti, e=e: nc.vector.scalar_tensor_tensor(out=yacc[:, ti, half * 512:(half + 1) * 512], in0=pt[:, 0:512], scalar=comb[:, ti, e:e + 1],
                                                                                                                            in1=yacc[:, ti, half * 512:(half + 1) * 512], op0=ALU.mult, op1=ALU.add)),
                                                 reads=[pk2, "ecomb", f"ey{ti}"], writes=[f"ey{ti}"])
                            first = False
                    for i, n in enumerate(tiles):
                        hb = i % 2
                        P.op("sp", (lambda n=n, hb=hb: nc.sync.dma_start(out=h_sb[:, hb, :], in_=hbuf[n * 128:(n + 1) * 128, :])),
                             reads=[f"hbuf{n}"], writes=[f"eh{hb}"], dma="ld")
                        P.op("dve", (lambda i=i, hb=hb: nc.vector.tensor_tensor(out=yacc[:, i, :], in0=yacc[:, i, :], in1=h_sb[:, hb, :], op=ALU.add)),
                             reads=[f"ey{i}", f"eh{hb}"], writes=[f"ey{i}"])
                        P.op("act", (lambda i=i: nc.scalar.activation(out=junk[:], in_=yacc[:, i, :], func=AF.Square, accum_out=fssq[:, i:i + 1])),
                             reads=[f"ey{i}"], writes=["ejunk", "efssq"])
                    P.op("act", lambda ntl=ntl: nc.scalar.activation(out=frstd[:, 0:ntl], in_=fssq[:, 0:ntl], func=AF.Sqrt, bias=epsb[:, 0:1], scale=1.0 / D),
                         reads=["efssq", "epsb"], writes=["efrstd"])
                    P.op("dve", lambda ntl=ntl: nc.vector.reciprocal(out=frstd[:, 0:ntl], in_=frstd[:, 0:ntl]), reads=["efrstd"], writes=["efrstd"])
                    for i, n in enumerate(tiles):
                        ob_i = i % 2
                        P.op("dve", (lambda i=i, ob_i=ob_i: nc.vector.scalar_tensor_tensor(out=ob[:, ob_i, :], in0=yacc[:, i, :], scalar=frstd[:, i:i + 1], in1=gainf[:], op0=ALU.mult, op1=ALU.mult)),
                             reads=[f"ey{i}", "efrstd", "egainf"], writes=[f"eob{ob_i}"])
                        P.op("sp", (lambda n=n, ob_i=ob_i: nc.sync.dma_start(out=out[(n - 1) * 128:n * 128, :], in_=ob[:, ob_i, :])),
                             reads=[f"eob{ob_i}"], writes=[], dma="st")
                P.emit_phase()

        if stop_after >= 1:
            mixer_phase(0)
        if stop_after >= 2:
            ffn_phase()
        if stop_after >= 3:
            mixer_phase(1)
        if stop_after >= 4:
            moe_phase()
    return nc


_CONSTS = None


def kernel(**inputs):
    global _CONSTS
    if _CONSTS is None:
        _CONSTS = make_consts()
    nc = build_program()
    shared = {k: np.ascontiguousarray(np.asarray(v, dtype=np.float32)) for k, v in inputs.items() if k != "x"}
    shared["norm_final"] = shared["norm_final"].reshape(1, D)
    shared.update(_CONSTS)
    x = np.asarray(inputs["x"], dtype=np.float32)
    in_maps = []
    for b in range(8):
        m = dict(shared)
        m["x"] = np.ascontiguousarray(x[b])
        in_maps.append(m)
    res = run_bass_kernel_spmd(nc, in_maps, core_ids=list(range(8)))
    return np.stack([np.asarray(r["out"]) for r in res.results], axis=0).astype(np.float32)
```

```python
import numpy as np
from contextlib import ExitStack
import ml_dtypes
import concourse.bass as bass
import concourse.mybir as mybir
from concourse.bass_utils import run_bass_kernel_spmd

F32 = mybir.dt.float32
BF16 = mybir.dt.bfloat16
AF = mybir.ActivationFunctionType
ALU = mybir.AluOpType
AX = mybir.AxisListType
bf = ml_dtypes.bfloat16

D = 1024
NTILES = 65
NEXP = 8
DFF = 2816
DFE = 3584
IN_W = 3840
EPS = 1e-5
NEG = -30000.0


class Prog:
    def __init__(self, nc, es):
        self.nc = nc
        self.es = es
        self.eng = {"pe": nc.tensor, "act": nc.scalar, "dve": nc.vector,
                    "pool": nc.gpsimd, "sp": nc.sync}
        self.ops = []
        self.last_w = {}
        self.readers = {}
        self.sems = {}
        self.cnt = {}
        self.waited = {}
        self.ticket = []
        self.emitted = 0
        self.dma_issued = {}
        self.RING = {"w": 16, "ld": 24, "st": 8}

    def _sem(self, name):
        if name not in self.sems:
            self.sems[name] = self.es.enter_context(self.nc.semaphore(name))
        return self.sems[name]

    def op(self, eng, fn, reads=(), writes=(), dma=None):
        idx = len(self.ops)
        deps = set()
        for r in reads:
            if r in self.last_w:
                deps.add(self.last_w[r])
        for w in writes:
            if w in self.last_w:
                deps.add(self.last_w[w])
            for rd in self.readers.get(w, {}).values():
                deps.add(rd)
        deps.discard(idx)
        self.ops.append(dict(eng=eng, fn=fn, deps=deps, dma=dma, reads=tuple(reads), writes=tuple(writes)))
        rkey = ("d", dma) if dma is not None else ("e", eng)
        for r in reads:
            self.readers.setdefault(r, {})[rkey] = idx
        for w in writes:
            self.last_w[w] = idx
            self.readers[w] = {}
        return idx

    def emit_phase(self):
        ops = self.ops
        lo, hi = self.emitted, len(ops)
        need = [False] * (hi - lo)
        last_of_eng = {}
        for i in range(lo, hi):
            o = ops[i]
            if o["dma"] is None:
                last_of_eng[o["eng"]] = i
            for d in o["deps"]:
                if d < lo:
                    continue
                p = ops[d]
                if p["dma"] is not None:
                    continue
                if p["eng"] == o["eng"] and o["dma"] is None:
                    if p["eng"] == "pe":
                        continue
                    if not (set(p["writes"]) & set(o["reads"])):
                        continue
                need[d - lo] = True
        for e, i in last_of_eng.items():
            need[i - lo] = True
        self.ticket.extend([None] * (hi - lo))
        for i in range(lo, hi):
            o = ops[i]
            if o["dma"] is not None:
                ring = self.RING.get(o["dma"], 8)
                k = self.dma_issued.get(o["dma"], 0)
                self.dma_issued[o["dma"]] = k + 1
                key = "d_%s_%d" % (o["dma"], k % ring)
                self.cnt[key] = self.cnt.get(key, 0) + 16
                self.ticket[i] = (key, self.cnt[key])
            elif need[i - lo]:
                key = "e_" + o["eng"]
                self.cnt[key] = self.cnt.get(key, 0) + 1
                self.ticket[i] = (key, self.cnt[key])
        for i in range(lo, hi):
            o = ops[i]
            e = self.eng[o["eng"]]
            req = {}
            for d in o["deps"]:
                if d < lo:
                    continue
                t = self.ticket[d]
                if t is None:
                    continue
                key, val = t
                if req.get(key, 0) < val:
                    req[key] = val
            for key, val in req.items():
                if self.waited.get((o["eng"], key), 0) >= val:
                    continue
                e.wait_ge(self._sem(key), val)
                self.waited[(o["eng"], key)] = val
            ins = o["fn"]()
            if self.ticket[i] is not None:
                key, val = self.ticket[i]
                ins.then_inc(self._sem(key), 16 if o["dma"] is not None else 1)
        for en, e in self.eng.items():
            for key, val in self.cnt.items():
                if self.waited.get((en, key), 0) >= val:
                    continue
                e.wait_ge(self._sem(key), val)
                self.waited[(en, key)] = val
        self.emitted = hi


def _slopes():
    return np.array([2.0 ** (-8.0 * (h + 1) / 16) for h in range(16)], dtype=np.float64)


def _head_of(g, col):
    par, jj = col // 4, col % 4
    return 8 * g + 2 * jj + par


def make_consts():
    sl = _slopes()
    key = np.arange(128)[:, None]
    q = np.arange(128)[None, :]
    bias_prev = np.zeros((128, 2, 8, 128), np.float32)
    bias_cur = np.zeros((128, 2, 8, 128), np.float32)
    for g in range(2):
        for c in range(8):
            s = sl[_head_of(g, c)]
            d_cur = q - key
            bias_cur[:, g, c, :] = np.where(d_cur >= 0, -s * d_cur, NEG)
            d_prev = q + 128 - key
            bias_prev[:, g, c, :] = np.where(d_prev < 128, -s * d_prev, NEG)
    m = np.arange(16)[:, None]
    bias_meta = np.zeros((16, 2, 8, 128), np.float32)
    bias_meta0 = np.zeros((16, 2, 8, 128), np.float32)
    off = np.zeros((16, NTILES, 2, 8), np.float32)
    for g in range(2):
        for c in range(8):
            s = sl[_head_of(g, c)]
            bias_meta[:, g, c, :] = -s * (16 + q - m)
            d0 = q - 112 - m
            bias_meta0[:, g, c, :] = np.where(d0 >= 0, -s * d0, NEG)
            for n in range(1, NTILES):
                off[:, n, g, c] = -s * 128.0 * (n - 1)
    W = (2, 4, 8, 16)
    pc = np.zeros((128, 4, 128), np.float32)
    pp = np.zeros((128, 4, 128), np.float32)
    pc0 = np.zeros((128, 4, 128), np.float32)
    for gi, w in enumerate(W):
        for t in range(128):
            for s_ in range(max(0, t - w + 1), t + 1):
                pc[s_, gi, t] += 1.0 / w
            pc[t, gi, t] -= 1.0
            for s_ in range(t + 128 - w + 1, 128):
                pp[s_, gi, t] += 1.0 / w
            if t >= 112:
                cnt = min(t - 111, w)
                for s_ in range(max(112, t - w + 1), t + 1):
                    pc0[s_, gi, t] += 1.0 / cnt
                pc0[t, gi, t] -= 1.0
    return {
        "c_identb": np.eye(128, dtype=np.float32).astype(bf),
        "c_identf": np.eye(128, dtype=np.float32),
        "c_bias_prev": bias_prev.reshape(128, 2048),
        "c_bias_cur": bias_cur.reshape(128, 2048),
        "c_bias_meta": bias_meta.reshape(16, 2048),
        "c_bias_meta0": bias_meta0.reshape(16, 2048),
        "c_off": off.reshape(16, NTILES * 16),
        "c_pool_cur": pc.reshape(128, 512).astype(bf),
        "c_pool_prev": pp.reshape(128, 512).astype(bf),
        "c_pool_cur0": pc0.reshape(128, 512).astype(bf),
    }


def build_program(ntiles=NTILES, moe_pass_tiles=16, debug=False, stop_after=4):
    nc = bass.Bass("TRN2", target_bir_lowering=False)
    nreal = ntiles - 1

    def din(name, shape, dt=F32):
        return nc.dram_tensor(name, list(shape), dt, kind="ExternalInput").ap()

    x = din("x", [8192, D])
    meta = din("meta_tokens", [16, D])
    norm_mix = din("norm_mix", [2, D])
    w_in = din("w_in", [2, D, IN_W])
    sinks = din("attn_sinks", [2, 16])
    w_br = din("w_attn_br", [2, D, D])
    w_pool = din("w_pool_grp", [2, 4, 128, 256])
    pool_scale = din("pool_scale", [2, D])
    w_out = din("w_out", [2, D, D])
    norm_ffn = din("norm_ffn", [2, D])
    dwg = din("dense_w_gate", [1, D, DFF])
    dwu = din("dense_w_up", [1, D, DFF])
    dwd = din("dense_w_down", [1, DFF, D])
    router = din("moe_router", [1, D, NEXP])
    mwg = din("moe_w_gate", [1, NEXP, D, DFE])
    mwu = din("moe_w_up", [1, NEXP, D, DFE])
    mwd = din("moe_w_down", [1, NEXP, DFE, D])
    norm_final = din("norm_final", [1, D])
    c_identb = din("c_identb", [128, 128], BF16)
    c_identf = din("c_identf", [128, 128])
    c_bias_prev = din("c_bias_prev", [128, 2048])
    c_bias_cur = din("c_bias_cur", [128, 2048])
    c_bias_meta = din("c_bias_meta", [16, 2048])
    c_bias_meta0 = din("c_bias_meta0", [16, 2048])
    c_off = din("c_off", [16, NTILES * 16])
    c_pool_cur = din("c_pool_cur", [128, 512], BF16)
    c_pool_prev = din("c_pool_prev", [128, 512], BF16)
    c_pool_cur0 = din("c_pool_cur0", [128, 512], BF16)
    out = nc.dram_tensor("out", [8192, D], F32, kind="ExternalOutput").ap()
    hbuf = nc.dram_tensor("hbuf", [NTILES * 128, D], F32, kind="Internal").ap()
    dbg = None
    if debug:
        dbg = nc.dram_tensor("dbg", [3, NTILES * 128, D], F32, kind="ExternalOutput").ap()

    with ExitStack() as top:
        P = Prog(nc, top)

        def sbt(es, name, shape, dt):
            return es.enter_context(nc.sbuf_tensor(name, list(shape), dt))

        ssqtab = sbt(top, "ssqtab", [128, NTILES], F32)
        rstdtab = sbt(top, "rstdtab", [128, NTILES], F32)
        identb = sbt(top, "identb", [128, 128], BF16)
        identf = sbt(top, "identf", [128, 128], F32)
        pg = [top.enter_context(nc.psum_tensor(f"pg{i}", [128, 512], F32)) for i in range(3)]
        psc = top.enter_context(nc.psum_tensor("psc", [128, 1024], F32))
        ppv = top.enter_context(nc.psum_tensor("ppv", [128, 1536], F32))
        pgi = [0]

        def next_pg():
            i = pgi[0] % 3
            pgi[0] += 1
            return pg[i], f"pg{i}"

        P.op("sp", lambda: nc.sync.dma_start(out=identb[:], in_=c_identb[:, :]), writes=["identb"], dma="ld")
        P.op("sp", lambda: nc.sync.dma_start(out=identf[:], in_=c_identf[:, :]), writes=["identf"], dma="ld")

        def rstd_from_ssq(n_lo, n_hi):
            P.op("act", lambda: nc.scalar.activation(out=rstdtab[:, n_lo:n_hi], in_=ssqtab[:, n_lo:n_hi], func=AF.Sqrt,
                                                     bias=epsb[:, 0:1], scale=1.0 / D),
                 reads=["ssqtab", "epsb"], writes=["rstdtab"])
            P.op("dve", lambda: nc.vector.reciprocal(out=rstdtab[:, n_lo:n_hi], in_=rstdtab[:, n_lo:n_hi]),
                 reads=["rstdtab"], writes=["rstdtab"])

        epsb = sbt(top, "epsb", [128, 1], F32)
        P.op("dve", lambda: nc.vector.memset(epsb[:], EPS), writes=["epsb"])

        def tile_src(layer, n):
            if layer == 0 and n >= 1:
                return x[(n - 1) * 128:n * 128, :]
            return hbuf[n * 128:(n + 1) * 128, :]

        with ExitStack() as es:
            hz = sbt(es, "p0_h", [128, 2, D], F32)
            junk = sbt(es, "p0_junk", [128, D], BF16)
            P.op("dve", lambda: nc.vector.memset(hz[:, 0, :], 0.0), writes=["p0h0"])
            P.op("sp", lambda: nc.sync.dma_start(out=hz[112:128, 0, :], in_=meta[:, :]), reads=[], writes=["p0h0"], dma="ld")
            P.op("sp", lambda: nc.sync.dma_start(out=hbuf[0:128, :], in_=hz[:, 0, :]), reads=["p0h0"], writes=["hbuf0"], dma="st")
            P.op("act", lambda: nc.scalar.activation(out=junk[:], in_=hz[:, 0, :], func=AF.Square, accum_out=ssqtab[:, 0:1]),
                 reads=["p0h0"], writes=["p0junk", "ssqtab"])
            for n in range(1, ntiles):
                b = n % 2
                P.op("sp", (lambda n=n, b=b: nc.sync.dma_start(out=hz[:, b, :], in_=x[(n - 1) * 128:n * 128, :])),
                     writes=[f"p0h{b}"], dma="ld")
                P.op("act", (lambda n=n, b=b: nc.scalar.activation(out=junk[:], in_=hz[:, b, :], func=AF.Square,
                                                                   accum_out=ssqtab[:, n:n + 1])),
                     reads=[f"p0h{b}"], writes=["p0junk", "ssqtab"])
            rstd_from_ssq(0, ntiles)
            P.emit_phase()

        def mixer_phase(l):
            with ExitStack() as es:
                Win = sbt(es, f"Win_{l}", [128, 8, IN_W], BF16)
                WkS = sbt(es, f"WkS_{l}", [128, 8, 128], BF16)
                Wbr = sbt(es, f"Wbr_{l}", [128, 8, D], BF16)
                Wout = sbt(es, f"Wout_{l}", [128, 8, D], BF16)
                Wpool = sbt(es, f"Wpool_{l}", [128, 4, 256], BF16)
                gain = sbt(es, f"gain_{l}", [128, D], F32)
                bprev = sbt(es, f"bprev_{l}", [128, 2048], F32)
                bcur = sbt(es, f"bcur_{l}", [128, 2048], F32)
                bmeta = sbt(es, f"bmeta_{l}", [16, 2048], F32)
                offt = sbt(es, f"offt_{l}", [16, NTILES * 16], F32)
                pcur = sbt(es, f"pcur_{l}", [128, 512], BF16)
                pprev = sbt(es, f"pprev_{l}", [128, 512], BF16)
                pcur0 = sbt(es, f"pcur0_{l}", [128, 512], BF16)
                sk_raw = sbt(es, f"sk_raw_{l}", [128, 16], F32)
                sinkexp = sbt(es, f"sinkexp_{l}", [128, 16], F32)
                h_sb = sbt(es, f"h_sb_{l}", [128, 3, D], F32)
                xn = sbt(es, f"xn_{l}", [128, D], BF16)
                xnT = sbt(es, f"xnT_{l}", [128, 2, 8, 128], BF16)
                qT = sbt(es, f"qT_{l}", [128, 2, 8, 128], BF16)
                kTa = sbt(es, f"kTa_{l}", [128, 2, 128], BF16)
                kTb = sbt(es, f"kTb_{l}", [128, 2, 128], BF16)
                kTma = sbt(es, f"kTma_{l}", [128, 16], BF16)
                kTmb = sbt(es, f"kTmb_{l}", [128, 16], BF16)
                vE = sbt(es, f"vE_{l}", [128, 2, 2, 66], BF16)
                vEm = sbt(es, f"vEm_{l}", [16, 2, 66], BF16)
                u_sb = sbt(es, f"u_sb_{l}", [128, 2, 512], BF16)
                gT = sbt(es, f"gT_{l}", [128, 2, 2048], BF16)
                t_sb = sbt(es, f"t_sb_{l}", [128, 2, 1024], F32)
                pTp = sbt(es, f"pTp_{l}", [128, 2048], BF16)
                pTc = sbt(es, f"pTc_{l}", [128, 2048], BF16)
                pTm = sbt(es, f"pTm_{l}", [16, 2048], BF16)
                den = sbt(es, f"den_{l}", [128, 16], F32)
                attn = sbt(es, f"attn_{l}", [128, D], BF16)
                attnT = sbt(es, f"attnT_{l}", [128, 8, 128], BF16)
                m1 = sbt(es, f"m1_{l}", [128, D], F32)
                m2 = sbt(es, f"m2_{l}", [128, 256], F32)
                mergedTM = sbt(es, f"mergedTM_{l}", [128, D], BF16)
                pooledT = sbt(es, f"pooledT_{l}", [128, 4, 128], BF16)
                mergedT = sbt(es, f"mergedT_{l}", [128, 8, 128], BF16)
                hnew = sbt(es, f"hnew_{l}", [128, D], F32)

                P.op("pool", lambda: nc.gpsimd.dma_start(out=Win[:, :, :], in_=w_in[l, :, :].rearrange("(kc p) n -> p kc n", p=128)),
                     writes=["Win"], dma="w")
                P.op("pool", lambda: nc.gpsimd.dma_start(out=WkS[:, :, 0:64], in_=w_in[l, :, 1088:1152].rearrange("(kc p) n -> p kc n", p=128)),
                     writes=["WkS"], dma="w")
                P.op("pool", lambda: nc.gpsimd.dma_start(out=WkS[:, :, 64:128], in_=w_in[l, :, 1024:1088].rearrange("(kc p) n -> p kc n", p=128)),
                     writes=["WkS"], dma="w")
                P.op("pool", lambda: nc.gpsimd.dma_start(out=Wbr[:, :, :], in_=w_br[l, :, :].rearrange("(kc p) n -> p kc n", p=128)),
                     writes=["Wbr"], dma="w")
                P.op("pool", lambda: nc.gpsimd.dma_start(out=Wout[:, :, :], in_=w_out[l, :, :].rearrange("(kc p) n -> p kc n", p=128)),
                     writes=["Wout"], dma="w")
                P.op("pool", lambda: nc.gpsimd.dma_start(out=Wpool[:, :, :], in_=w_pool[l, :, :, :].rearrange("g c d -> c g d")),
                     writes=["Wpool"], dma="w")
                P.op("sp", lambda: nc.sync.dma_start(out=gain[:], in_=norm_mix[l:l + 1, :].to_broadcast([128, D])), writes=["gain"], dma="ld")
                P.op("sp", lambda: nc.sync.dma_start(out=bprev[:], in_=c_bias_prev[:, :]), writes=["bprev"], dma="ld")
                P.op("sp", lambda: nc.sync.dma_start(out=bcur[:], in_=c_bias_cur[:, :]), writes=["bcur"], dma="ld")
                P.op("sp", lambda: nc.sync.dma_start(out=bmeta[:], in_=c_bias_meta0[:, :]), writes=["bmeta"], dma="ld")
                P.op("sp", lambda: nc.sync.dma_start(out=offt[:], in_=c_off[:, :]), writes=["offt"], dma="ld")
                P.op("sp", lambda: nc.sync.dma_start(out=pcur[:], in_=c_pool_cur[:, :]), writes=["pcur"], dma="ld")
                P.op("sp", lambda: nc.sync.dma_start(out=pprev[:], in_=c_pool_prev[:, :]), writes=["pprev"], dma="ld")
                P.op("sp", lambda: nc.sync.dma_start(out=pcur0[:], in_=c_pool_cur0[:, :]), writes=["pcur0"], dma="ld")
                P.op("sp", lambda: nc.sync.dma_start(out=sk_raw[:], in_=sinks[l:l + 1, :].to_broadcast([128, 16])), writes=["sk_raw"], dma="ld")
                P.op("sp", lambda: nc.sync.dma_start(out=hnew[:], in_=pool_scale[l:l + 1, :].to_broadcast([128, D])), writes=["hnew"], dma="ld")
                for g in range(4):
                    P.op("dve", (lambda g=g: nc.vector.tensor_tensor(out=Wpool[:, g, :], in0=Wpool[:, g, :], in1=hnew[:, g * 256:(g + 1) * 256], op=ALU.mult)),
                         reads=["Wpool", "hnew"], writes=["Wpool"])
                P.op("act", lambda: nc.scalar.activation(out=sinkexp[:], in_=sk_raw[:], func=AF.Exp), reads=["sk_raw"], writes=["sinkexp"])
                P.op("dve", lambda: nc.vector.memset(vE[:], 1.0), writes=["vE0", "vE1"])
                P.op("dve", lambda: nc.vector.memset(vEm[:], 1.0), writes=["vEm"])

                def proj_fm(n, wsel, evac_eng, evac_fn, wres, writes=()):
                    b_ = n % 2
                    pt, pk2 = next_pg()
                    for kc in range(8):
                        P.op("pe", (lambda kc=kc, pt=pt, b_=b_: nc.tensor.matmul(pt[:, 0:128], wsel(kc), xnT[:, b_, kc, :], start=(kc == 0), stop=(kc == 7))),
                             reads=[wres, f"xnT{b_}"], writes=[pk2])
                    P.op(evac_eng, (lambda pt=pt: evac_fn(pt[:, 0:128])), reads=[pk2], writes=list(writes))

                def stA(n):
                    hb = n % 3
                    P.op("sp", (lambda: nc.sync.dma_start(out=h_sb[:, hb, :], in_=tile_src(l, n))),
                         reads=[f"hbuf{n}"], writes=[f"h{hb}"], dma="ld")
                    P.op("dve", (lambda: nc.vector.scalar_tensor_tensor(out=xn[:], in0=h_sb[:, hb, :], scalar=rstdtab[:, n:n + 1],
                                                                        in1=gain[:], op0=ALU.mult, op1=ALU.mult)),
                         reads=[f"h{hb}", "rstdtab", "gain"], writes=["xn"])
                    pgt, pk = next_pg()
                    pgb = pgt[:].bitcast(BF16)
                    for kc in range(8):
                        P.op("pe", (lambda kc=kc: nc.tensor.transpose(pgb[:, kc * 128:(kc + 1) * 128], xn[:, kc * 128:(kc + 1) * 128], identb[:])),
                             reads=["xn", "identb"], writes=[pk])
                    P.op("act", (lambda: nc.scalar.copy(out=xnT[:, n % 2, :, :].rearrange("p a b -> p (a b)"), in_=pgb)),
                         reads=[pk], writes=[f"xnT{n % 2}"])

                def b1_chunks(n):
                    b_ = n % 2
                    sl = n % 2
                    jobs = []
                    for j in range(8):
                        jobs.append(lambda j=j: proj_fm(n, (lambda kc, j=j: Win[:, kc, j * 128:(j + 1) * 128]), "act",
                                                        (lambda p_, j=j: nc.scalar.activation(out=qT[:, b_, j, :], in_=p_, func=AF.Copy, scale=0.125)),
                                                        "Win", writes=[f"qT{b_}"]))
                    jobs.append(lambda: proj_fm(n, (lambda kc: Win[:, kc, 1024:1152]), "act",
                                                (lambda p_: nc.scalar.copy(out=kTa[:, sl, :], in_=p_)), "Win", writes=[f"kTa{sl}"]))
                    jobs.append(lambda: proj_fm(n, (lambda kc: WkS[:, kc, :]), "act",
                                                (lambda p_: nc.scalar.copy(out=kTb[:, sl, :], in_=p_)), "WkS", writes=[f"kTb{sl}"]))
                    return jobs

                def stB1(n):
                    for jb in b1_chunks(n):
                        jb()

                def stB2(n, q_lo, q_hi):
                    b_ = n % 2
                    for qd in range(q_lo, q_hi):
                        pt, pk2 = next_pg()
                        for kc in range(8):
                            P.op("pe", (lambda kc=kc, pt=pt, qd=qd: nc.tensor.matmul(pt[:, 0:512], xnT[:, b_, kc, :], Win[:, kc, 1792 + qd * 512:1792 + (qd + 1) * 512], start=(kc == 0), stop=(kc == 7))),
                                 reads=[f"xnT{b_}", "Win"], writes=[pk2])
                        P.op("act", (lambda pt=pt, qd=qd: nc.scalar.activation(out=gT[:, b_, qd * 512:(qd + 1) * 512], in_=pt[:, 0:512], func=AF.Tanh, scale=0.5)),
                             reads=[pk2], writes=[f"gT{b_}"])

                def stB3(n):
                    b_ = n % 2
                    sl = n % 2
                    pv1, pk1 = next_pg()
                    pv2, pk2_ = next_pg()
                    for kc in range(8):
                        P.op("pe", (lambda kc=kc: nc.tensor.matmul(pv1[:, 0:512], xnT[:, b_, kc, :], Win[:, kc, 1152:1664], start=(kc == 0), stop=(kc == 7))),
                             reads=[f"xnT{b_}", "Win"], writes=[pk1])
                    for kc in range(8):
                        P.op("pe", (lambda kc=kc: nc.tensor.matmul(pv2[:, 0:128], xnT[:, b_, kc, :], Win[:, kc, 1664:1792], start=(kc == 0), stop=(kc == 7))),
                             reads=[f"xnT{b_}", "Win"], writes=[pk2_])
                    for kv in range(2):
                        P.op("act", (lambda kv=kv: nc.scalar.copy(out=vE[:, sl, kv, 0:64], in_=pv1[:, kv * 64:(kv + 1) * 64])),
                             reads=[pk1], writes=[f"vE{sl}"])
                    P.op("act", (lambda: nc.scalar.copy(out=u_sb[:, sl, 0:384], in_=pv1[:, 128:512])),
                         reads=[pk1], writes=[f"u{sl}"])
                    P.op("act", (lambda: nc.scalar.copy(out=u_sb[:, sl, 384:512], in_=pv2[:, 0:128])),
                         reads=[pk2_], writes=[f"u{sl}"])
                    if n == 0:
                        P.op("act", lambda: nc.scalar.copy(out=kTma[:], in_=kTa[:, 0, 112:128]), reads=["kTa0"], writes=["kTma"])
                        P.op("act", lambda: nc.scalar.copy(out=kTmb[:], in_=kTb[:, 0, 112:128]), reads=["kTb0"], writes=["kTmb"])
                        pm, pkm = next_pg()
                        for kc in range(8):
                            P.op("pe", (lambda kc=kc: nc.tensor.matmul(pm[0:16, 0:128], xnT[:, 0, kc, 112:128], Win[:, kc, 1152:1280], start=(kc == 0), stop=(kc == 7))),
                                 reads=["xnT0", "Win"], writes=[pkm])
                        for kv in range(2):
                            P.op("act", (lambda kv=kv: nc.scalar.copy(out=vEm[:, kv, 0:64], in_=pm[0:16, kv * 64:(kv + 1) * 64])),
                                 reads=[pkm], writes=["vEm"])

                def blocks_of(n):
                    blocks = []
                    if n >= 2:
                        blocks.append(("prev", (n - 1) % 2))
                    if n >= 1:
                        blocks.append(("cur", n % 2))
                    blocks.append(("meta", None))
                    return blocks

                def stC(n, fillers=()):
                    b_ = n % 2
                    fillers = list(fillers)
                    blocks = blocks_of(n)
                    nsteps = 2 * len(blocks)
                    step = 0
                    for (bname, bs) in blocks:
                        for g in range(2):
                            tb = step % 2
                            tk = f"t_sb{tb}"
                            for par in range(2):
                                base = par * 64
                                use_a = (g == par)
                                if bname == "meta":
                                    kt = (kTma if use_a else kTmb)[base:base + 64, :]
                                    kres = "kTma" if use_a else "kTmb"
                                    M = 16
                                else:
                                    kt = (kTa if use_a else kTb)[base:base + 64, bs, :]
                                    kres = (f"kTa{bs}" if use_a else f"kTb{bs}")
                                    M = 128
                                P.op("pe", (lambda kt=kt, M=M, par=par, g=g, base=base: nc.tensor.matmul(
                                    psc[0:M, par * 512:(par + 1) * 512], kt, qT[base:base + 64, b_, 4 * g:4 * g + 4, :], start=True, stop=True)),
                                    reads=[kres, f"qT{b_}"], writes=["psc"])
                            if bname == "meta":
                                P.op("dve", (lambda g=g, tb=tb: nc.vector.tensor_tensor(out=t_sb[0:16, tb, :], in0=psc[0:16, :], in1=bmeta[:, g * 1024:(g + 1) * 1024], op=ALU.add)),
                                     reads=["psc", "bmeta"], writes=[tk])
                                if n >= 2:
                                    P.op("dve", (lambda g=g, tb=tb: nc.vector.tensor_tensor(
                                        out=t_sb[0:16, tb, :].rearrange("p (c q) -> p c q", c=8),
                                        in0=t_sb[0:16, tb, :].rearrange("p (c q) -> p c q", c=8),
                                        in1=offt[:, n * 16 + g * 8:n * 16 + g * 8 + 8].unsqueeze(2).to_broadcast([16, 8, 128]), op=ALU.add)),
                                        reads=[tk, "offt"], writes=[tk])
                                P.op("act", (lambda g=g, tb=tb: nc.scalar.activation(out=pTm[:, g * 1024:(g + 1) * 1024], in_=t_sb[0:16, tb, :], func=AF.Exp)),
                                     reads=[tk], writes=["pTm"])
                            else:
                                btab = bprev if bname == "prev" else bcur
                                pdst = pTp if bname == "prev" else pTc
                                P.op("dve", (lambda g=g, btab=btab, tb=tb: nc.vector.tensor_tensor(out=t_sb[:, tb, :], in0=psc[:], in1=btab[:, g * 1024:(g + 1) * 1024], op=ALU.add)),
                                     reads=["psc", "bprev" if bname == "prev" else "bcur"], writes=[tk])
                                P.op("act", (lambda g=g, pdst=pdst, tb=tb: nc.scalar.activation(out=pdst[:, g * 1024:(g + 1) * 1024], in_=t_sb[:, tb, :], func=AF.Exp)),
                                     reads=[tk], writes=["pTp" if bname == "prev" else "pTc"])
                            step += 1
                            if fillers:
                                k = -(-len(fillers) // (nsteps - step + 1))
                                for _ in range(k):
                                    fillers.pop(0)()
                    while fillers:
                        fillers.pop(0)()
                    if n == 0:
                        P.op("sp", lambda: nc.sync.dma_start(out=bmeta[:], in_=c_bias_meta[:, :]), writes=["bmeta"], dma="ld")

                def stD(n):
                    blocks = blocks_of(n)
                    for h in range(16):
                        g = h // 8
                        par = h % 2
                        jj = (h % 8) // 2
                        col = g * 1024 + par * 512 + jj * 128
                        bank, hh = h // 7, h % 7
                        o = ppv[:, bank * 512 + hh * 65:bank * 512 + hh * 65 + 65]
                        seq = []
                        for (bname, bs) in blocks:
                            if bname == "meta":
                                seq.append((pTm[:, col:col + 128], vEm[:, g, 0:65], "pTm", "vEm"))
                            elif bname == "prev":
                                seq.append((pTp[:, col:col + 128], vE[:, bs, g, 0:65], "pTp", f"vE{bs}"))
                            else:
                                seq.append((pTc[:, col:col + 128], vE[:, bs, g, 0:65], "pTc", f"vE{bs}"))
                        for i, (lt, rt, r1, r2) in enumerate(seq):
                            P.op("pe", (lambda o=o, lt=lt, rt=rt, i=i, L=len(seq): nc.tensor.matmul(o, lt, rt, start=(i == 0), stop=(i == L - 1))),
                                 reads=[r1, r2], writes=["ppv"])
                    for bank, nh in ((0, 7), (1, 7), (2, 2)):
                        P.op("dve", (lambda bank=bank, nh=nh: nc.vector.tensor_tensor(
                            out=den[:, bank * 7:bank * 7 + nh],
                            in0=ppv[:, bank * 512:bank * 512 + nh * 65].rearrange("p (h e) -> p h e", e=65)[:, :, 64],
                            in1=sinkexp[:, bank * 7:bank * 7 + nh], op=ALU.add)),
                            reads=["ppv", "sinkexp"], writes=["den"])
                    P.op("dve", lambda: nc.vector.reciprocal(out=den[:], in_=den[:]), reads=["den"], writes=["den"])
                    for bank, nh in ((0, 7), (1, 7), (2, 2)):
                        P.op("dve", (lambda bank=bank, nh=nh: nc.vector.tensor_tensor(
                            out=attn[:, bank * 448:bank * 448 + nh * 64].rearrange("p (h e) -> p h e", e=64),
                            in0=ppv[:, bank * 512:bank * 512 + nh * 65].rearrange("p (h e) -> p h e", e=65)[:, :, 0:64],
                            in1=den[:, bank * 7:bank * 7 + nh].unsqueeze(2).to_broadcast([128, nh, 64]), op=ALU.mult)),
                            reads=["ppv", "den"], writes=["attn"])

                def stD2(n):
                    pgt, pk = next_pg()
                    pgb = pgt[:].bitcast(BF16)
                    for kc in range(8):
                        P.op("pe", (lambda kc=kc: nc.tensor.transpose(pgb[:, kc * 128:(kc + 1) * 128], attn[:, kc * 128:(kc + 1) * 128], identb[:])),
                             reads=["attn", "identb"], writes=[pk])
                    P.op("act", (lambda: nc.scalar.copy(out=attnT[:].rearrange("p a b -> p (a b)"), in_=pgb)),
                         reads=[pk], writes=["attnT"])

                def stE(n):
                    b_ = n % 2
                    for half in range(2):
                        pt, pk2 = next_pg()
                        for kc in range(8):
                            P.op("pe", (lambda kc=kc, pt=pt, half=half: nc.tensor.matmul(pt[:, 0:512], attnT[:, kc, :], Wbr[:, kc, half * 512:(half + 1) * 512], start=(kc == 0), stop=(kc == 7))),
                                 reads=["Wbr", "attnT"], writes=[pk2])
                        P.op("dve", (lambda pt=pt, half=half: nc.vector.scalar_tensor_tensor(out=m1[:, half * 512:(half + 1) * 512], in0=gT[:, b_, half * 512:(half + 1) * 512], scalar=1.0,
                                                                                            in1=pt[:, 0:512], op0=ALU.add, op1=ALU.mult)),
                             reads=[pk2, f"gT{b_}"], writes=["m1"])

                def stF1(n):
                    sl = n % 2
                    slp = (n - 1) % 2
                    pt, pk2 = next_pg()
                    for g in range(4):
                        pm_cur = (pcur0 if n == 0 else pcur)
                        two = (n >= 1)
                        P.op("pe", (lambda g=g, pm_cur=pm_cur, two=two: nc.tensor.matmul(
                            pt[:, g * 128:(g + 1) * 128], u_sb[:, sl, g * 128:(g + 1) * 128], pm_cur[:, g * 128:(g + 1) * 128], start=True, stop=(not two))),
                            reads=[f"u{sl}", "pcur0" if n == 0 else "pcur"], writes=[pk2])
                        if two:
                            P.op("pe", (lambda g=g: nc.tensor.matmul(
                                pt[:, g * 128:(g + 1) * 128], u_sb[:, slp, g * 128:(g + 1) * 128], pprev[:, g * 128:(g + 1) * 128], start=False, stop=True)),
                                reads=[f"u{slp}", "pprev"], writes=[pk2])
                    P.op("act", (lambda: nc.scalar.copy(out=pooledT[:].rearrange("p a b -> p (a b)"), in_=pt[:, 0:512])),
                         reads=[pk2], writes=["pooledT"])

                def stF2(n):
                    b_ = n % 2
                    for g in range(4):
                        pt, pk2 = next_pg()
                        P.op("pe", (lambda pt=pt, g=g: nc.tensor.matmul(pt[:, 0:256], pooledT[:, g, :], Wpool[:, g, :], start=True, stop=True)),
                             reads=["Wpool", "pooledT"], writes=[pk2])
                        P.op("dve", (lambda pt=pt, g=g: nc.vector.scalar_tensor_tensor(out=m2[:], in0=gT[:, b_, 1024 + g * 256:1024 + (g + 1) * 256], scalar=1.0, in1=pt[:, 0:256], op0=ALU.add, op1=ALU.mult)),
                             reads=[pk2, f"gT{b_}"], writes=["m2"])
                        P.op("dve", (lambda g=g: nc.vector.tensor_tensor(out=mergedTM[:, g * 256:(g + 1) * 256], in0=m2[:], in1=m1[:, g * 256:(g + 1) * 256], op=ALU.add)),
                             reads=["m2", "m1"], writes=["mergedTM"])

                def stF2b(n):
                    pgt, pk = next_pg()
                    pgb = pgt[:].bitcast(BF16)
                    for kc in range(8):
                        P.op("pe", (lambda kc=kc: nc.tensor.transpose(pgb[:, kc * 128:(kc + 1) * 128], mergedTM[:, kc * 128:(kc + 1) * 128], identb[:])),
                             reads=["mergedTM", "identb"], writes=[pk])
                    P.op("act", (lambda: nc.scalar.copy(out=mergedT[:].rearrange("p a b -> p (a b)"), in_=pgb)),
                         reads=[pk], writes=["mergedT"])

                def stG(n):
                    hb = n % 3
                    for half in range(2):
                        pt, pk2 = next_pg()
                        for kc in range(8):
                            P.op("pe", (lambda kc=kc, pt=pt, half=half: nc.tensor.matmul(pt[:, 0:512], mergedT[:, kc, :], Wout[:, kc, half * 512:(half + 1) * 512], start=(kc == 0), stop=(kc == 7))),
                                 reads=["mergedT", "Wout"], writes=[pk2])
                        P.op("dve", (lambda pt=pt, half=half: nc.vector.scalar_tensor_tensor(out=hnew[:, half * 512:(half + 1) * 512], in0=pt[:, 0:512], scalar=0.5,
                                                                                            in1=h_sb[:, hb, half * 512:(half + 1) * 512], op0=ALU.mult, op1=ALU.add)),
                             reads=[pk2, f"h{hb}"], writes=["hnew"])
                    P.op("act", (lambda: nc.scalar.activation(out=mergedTM[:], in_=hnew[:], func=AF.Square, accum_out=ssqtab[:, n:n + 1])),
                         reads=["hnew"], writes=["mergedTM", "ssqtab"])
                    P.op("sp", (lambda: nc.sync.dma_start(out=hbuf[n * 128:(n + 1) * 128, :], in_=hnew[:])),
                         reads=["hnew"], writes=[f"hbuf{n}"], dma="st")
                    if dbg is not None:
                        P.op("sp", (lambda: nc.sync.dma_start(out=dbg[l, n * 128:(n + 1) * 128, :], in_=hnew[:])),
                             reads=["hnew"], writes=[], dma="st")

                stA(0); stB1(0); stB2(0, 0, 4); stB3(0)
                if ntiles > 1:
                    stA(1)
                for n in range(ntiles):
                    nxt = n + 1 if n + 1 < ntiles else None
                    stC(n, b1_chunks(nxt) if nxt is not None else ())
                    stD(n)
                    stF1(n)
                    if nxt is not None:
                        stB2(nxt, 0, 2)
                    stD2(n)
                    stE(n)
                    stF2(n)
                    if nxt is not None:
                        stB2(nxt, 2, 4)
                        stB3(nxt)
                    if n + 2 < ntiles:
                        stA(n + 2)
                    stF2b(n)
                    stG(n)
                rstd_from_ssq(0, ntiles)
                P.emit_phase()

        def ffn_phase():
            NF = DFF // 128
            with ExitStack() as es:
                Wg = sbt(es, "Wg", [128, 8, DFF], BF16)
                Wu = sbt(es, "Wu", [128, 8, DFF], BF16)
                Wd = sbt(es, "Wd", [128, NF, D], BF16)
                gain = sbt(es, "fgain", [128, D], F32)
                h_sb = sbt(es, "fh", [128, 4, D], F32)
                xn = sbt(es, "fxn", [128, D], BF16)
                xnT = sbt(es, "fxnT", [128, 8, 512], BF16)
                hT = sbt(es, "fhT", [128, NF, 512], BF16)
                sg = sbt(es, "fsg", [128, 2, 512], F32)
                junk = sbt(es, "fjunk", [128, D], BF16)
                P.op("pool", lambda: nc.gpsimd.dma_start(out=Wg[:, :, :], in_=dwg[0, :, :].rearrange("(kc p) n -> p kc n", p=128)), writes=["Wg"], dma="w")
                P.op("pool", lambda: nc.gpsimd.dma_start(out=Wu[:, :, :], in_=dwu[0, :, :].rearrange("(kc p) n -> p kc n", p=128)), writes=["Wu"], dma="w")
                P.op("pool", lambda: nc.gpsimd.dma_start(out=Wd[:, :, :], in_=dwd[0, :, :].rearrange("(f p) n -> p f n", p=128)), writes=["Wd"], dma="w")
                P.op("sp", lambda: nc.sync.dma_start(out=gain[:], in_=norm_ffn[0:1, :].to_broadcast([128, D])), writes=["fgain"], dma="ld")
                groups = [[0]] + [list(range(s, min(s + 4, ntiles))) for s in range(1, ntiles, 4)]
                for tiles in groups:
                    nt = len(tiles)
                    N = nt * 128
                    for i, n in enumerate(tiles):
                        P.op("sp", (lambda n=n, i=i: nc.sync.dma_start(out=h_sb[:, i, :], in_=hbuf[n * 128:(n + 1) * 128, :])),
                             reads=[f"hbuf{n}"], writes=[f"fh{i}"], dma="ld")
                        P.op("dve", (lambda n=n, i=i: nc.vector.scalar_tensor_tensor(out=xn[:], in0=h_sb[:, i, :], scalar=rstdtab[:, n:n + 1], in1=gain[:], op0=ALU.mult, op1=ALU.mult)),
                             reads=[f"fh{i}", "rstdtab", "fgain"], writes=["fxn"])
                        pgt, pk = next_pg()
                        pgb = pgt[:].bitcast(BF16)
                        for kc in range(8):
                            P.op("pe", (lambda kc=kc, pgb=pgb: nc.tensor.transpose(pgb[:, kc * 128:(kc + 1) * 128], xn[:, kc * 128:(kc + 1) * 128], identb[:])),
                                 reads=["fxn", "identb"], writes=[pk])
                        P.op("act", (lambda pgb=pgb, i=i: nc.scalar.copy(out=xnT[:, :, i * 128:(i + 1) * 128], in_=pgb.rearrange("p (a b) -> p a b", a=8))),
                             reads=[pk], writes=["fxnT"])
                    for f in range(NF):
                        sb_ = f % 2
                        pgu = psc if sb_ == 0 else ppv
                        ra, rb = (("pscA", "pscB") if sb_ == 0 else ("ppvA", "ppvB"))
                        for kc in range(8):
                            P.op("pe", (lambda kc=kc, f=f, N=N, pgu=pgu: nc.tensor.matmul(pgu[:, 0:N], Wg[:, kc, f * 128:(f + 1) * 128], xnT[:, kc, 0:N], start=(kc == 0), stop=(kc == 7))),
                                 reads=["Wg", "fxnT"], writes=[ra])
                        for kc in range(8):
                            P.op("pe", (lambda kc=kc, f=f, N=N, pgu=pgu: nc.tensor.matmul(pgu[:, 512:512 + N], Wu[:, kc, f * 128:(f + 1) * 128], xnT[:, kc, 0:N], start=(kc == 0), stop=(kc == 7))),
                                 reads=["Wu", "fxnT"], writes=[rb])
                        P.op("act", (lambda sb_=sb_, N=N, pgu=pgu: nc.scalar.activation(out=sg[:, sb_, 0:N], in_=pgu[:, 0:N], func=AF.Silu)),
                             reads=[ra], writes=[f"fsg{sb_}"])
                        P.op("dve", (lambda sb_=sb_, f=f, N=N, pgu=pgu: nc.vector.tensor_tensor(out=hT[:, f, 0:N], in0=pgu[:, 512:512 + N], in1=sg[:, sb_, 0:N], op=ALU.mult)),
                             reads=[rb, f"fsg{sb_}"], writes=["fhT"])
                    for i, n in enumerate(tiles):
                        for half in range(2):
                            pt, pk2 = next_pg()
                            for f in range(NF):
                                P.op("pe", (lambda f=f, pt=pt, half=half, i=i: nc.tensor.matmul(pt[:, 0:512], hT[:, f, i * 128:(i + 1) * 128], Wd[:, f, half * 512:(half + 1) * 512], start=(f == 0), stop=(f == NF - 1))),
                                     reads=["fhT", "Wd"], writes=[pk2])
                            P.op("dve", (lambda pt=pt, half=half, i=i: nc.vector.tensor_tensor(out=h_sb[:, i, half * 512:(half + 1) * 512], in0=pt[:, 0:512], in1=h_sb[:, i, half * 512:(half + 1) * 512], op=ALU.add)),
                                 reads=[pk2, f"fh{i}"], writes=[f"fh{i}"])
                        P.op("act", (lambda n=n, i=i: nc.scalar.activation(out=junk[:], in_=h_sb[:, i, :], func=AF.Square, accum_out=ssqtab[:, n:n + 1])),
                             reads=[f"fh{i}"], writes=["fjunk", "ssqtab"])
                        P.op("sp", (lambda n=n, i=i: nc.sync.dma_start(out=hbuf[n * 128:(n + 1) * 128, :], in_=h_sb[:, i, :])),
                             reads=[f"fh{i}"], writes=[f"hbuf{n}"], dma="st")
                        if dbg is not None:
                            P.op("sp", (lambda n=n, i=i: nc.sync.dma_start(out=dbg[2, n * 128:(n + 1) * 128, :], in_=h_sb[:, i, :])),
                                 reads=[f"fh{i}"], writes=[], dma="st")
                rstd_from_ssq(0, ntiles)
                P.emit_phase()

        def moe_phase():
            NG = 7
            PT = moe_pass_tiles
            with ExitStack() as es:
                Wg = sbt(es, "eWg", [128, 2, 8, 512], BF16)
                Wu = sbt(es, "eWu", [128, 2, 8, 512], BF16)
                Wd = sbt(es, "eWd", [128, 2, 4, D], BF16)
                gain = sbt(es, "egain", [128, D], F32)
                gainf = sbt(es, "egainf", [128, D], F32)
                Rt = sbt(es, "eR", [128, 8, NEXP], F32)
                h_sb = sbt(es, "eh", [128, 2, D], F32)
                xnf = sbt(es, "exnf", [128, D], F32)
                xnTf = sbt(es, "exnTf", [128, 8, 128], F32)
                xnT = sbt(es, "exnT", [128, 8, PT * 128], BF16)
                yacc = sbt(es, "eyacc", [128, PT, D], F32)
                hT = sbt(es, "ehT", [128, 2, 4, 512], BF16)
                sg = sbt(es, "esg", [128, 2, 512], F32)
                lg = sbt(es, "elg", [128, PT, NEXP], F32)
                comb = sbt(es, "ecomb", [128, PT, NEXP], F32)
                tmp8 = sbt(es, "etmp8", [128, PT, NEXP], F32)
                v1 = sbt(es, "ev1", [128, PT], F32)
                v2 = sbt(es, "ev2", [128, PT], F32)
                fssq = sbt(es, "efssq", [128, PT], F32)
                frstd = sbt(es, "efrstd", [128, PT], F32)
                junk = sbt(es, "ejunk", [128, D], BF16)
                ob = sbt(es, "eob", [128, 2, D], F32)
                P.op("sp", lambda: nc.sync.dma_start(out=gain[:], in_=norm_ffn[1:2, :].to_broadcast([128, D])), writes=["egain"], dma="ld")
                P.op("sp", lambda: nc.sync.dma_start(out=gainf[:], in_=norm_final[0:1, :].to_broadcast([128, D])), writes=["egainf"], dma="ld")
                with nc.allow_non_contiguous_dma(reason="tiny router"):
                    P.op("sp", lambda: nc.sync.dma_start(out=Rt[:], in_=router[0, :, :].rearrange("(c p) e -> p c e", p=128)), writes=["eR"], dma="ld")
                npass = (nreal + PT - 1) // PT
                wctr = [0]
                for ps_i in range(npass):
                    tiles = list(range(1 + ps_i * PT, min(1 + (ps_i + 1) * PT, ntiles)))
                    ntl = len(tiles)
                    for i, n in enumerate(tiles):
                        hb = i % 2
                        P.op("sp", (lambda n=n, hb=hb: nc.sync.dma_start(out=h_sb[:, hb, :], in_=hbuf[n * 128:(n + 1) * 128, :])),
                             reads=[f"hbuf{n}"], writes=[f"eh{hb}"], dma="ld")
                        P.op("dve", (lambda n=n, hb=hb: nc.vector.scalar_tensor_tensor(out=xnf[:], in0=h_sb[:, hb, :], scalar=rstdtab[:, n:n + 1], in1=gain[:], op0=ALU.mult, op1=ALU.mult)),
                             reads=[f"eh{hb}", "rstdtab", "egain"], writes=["exnf"])
                        for kc in range(8):
                            P.op("pe", (lambda kc=kc: nc.tensor.transpose(psc[:, kc * 128:(kc + 1) * 128], xnf[:, kc * 128:(kc + 1) * 128], identf[:])),
                                 reads=["exnf", "identf"], writes=["pscA", "pscB"])
                        P.op("act", (lambda i=i: nc.scalar.copy(out=xnT[:, :, i * 128:(i + 1) * 128], in_=psc[:].rearrange("p (a b) -> p a b", a=8))),
                             reads=["pscA", "pscB"], writes=["exnT"])
                        P.op("act", lambda: nc.scalar.copy(out=xnTf[:].rearrange("p a b -> p (a b)"), in_=psc[:]),
                             reads=["pscA", "pscB"], writes=["exnTf"])
                        pt, pk2 = next_pg()
                        for kc in range(8):
                            P.op("pe", (lambda kc=kc, pt=pt: nc.tensor.matmul(pt[:, 0:NEXP], xnTf[:, kc, :], Rt[:, kc, :], start=(kc == 0), stop=(kc == 7))),
                                 reads=["exnTf", "eR"], writes=[pk2])
                        P.op("act", (lambda pt=pt, i=i: nc.scalar.copy(out=lg[:, i, :], in_=pt[:, 0:NEXP])), reads=[pk2], writes=["elg"])
                    L = lg[:, 0:ntl, :]
                    C = comb[:, 0:ntl, :]
                    T8 = tmp8[:, 0:ntl, :]
                    V1 = v1[:, 0:ntl]
                    V2 = v2[:, 0:ntl]
                    bc = (lambda v, ntl=ntl: v.unsqueeze(2).to_broadcast([128, ntl, NEXP]))
                    P.op("dve", lambda L=L, C=C, T8=T8, V1=V1, V2=V2, bc=bc: nc.vector.tensor_reduce(out=V1, in_=L, axis=AX.X, op=ALU.max), reads=["elg"], writes=["ev1"])
                    P.op("dve", lambda L=L, C=C, T8=T8, V1=V1, V2=V2, bc=bc: nc.vector.tensor_tensor(out=T8, in0=L, in1=bc(V1), op=ALU.is_equal), reads=["elg", "ev1"], writes=["etmp8"])
                    P.op("dve", lambda L=L, C=C, T8=T8, V1=V1, V2=V2, bc=bc: nc.vector.scalar_tensor_tensor(out=T8, in0=T8, scalar=-1e30, in1=L, op0=ALU.mult, op1=ALU.add), reads=["etmp8", "elg"], writes=["etmp8"])
                    P.op("dve", lambda L=L, C=C, T8=T8, V1=V1, V2=V2, bc=bc: nc.vector.tensor_reduce(out=V2, in_=T8, axis=AX.X, op=ALU.max), reads=["etmp8"], writes=["ev2"])
                    P.op("dve", lambda L=L, C=C, T8=T8, V1=V1, V2=V2, bc=bc: nc.vector.tensor_tensor(out=T8, in0=L, in1=bc(V2), op=ALU.is_ge), reads=["elg", "ev2", "etmp8"], writes=["etmp8"])
                    P.op("dve", lambda L=L, C=C, T8=T8, V1=V1, V2=V2, bc=bc: nc.vector.tensor_tensor(out=C, in0=L, in1=bc(V1), op=ALU.subtract), reads=["elg", "ev1"], writes=["ecomb"])
                    P.op("act", lambda L=L, C=C, T8=T8, V1=V1, V2=V2, bc=bc: nc.scalar.activation(out=C, in_=C, func=AF.Exp), reads=["ecomb"], writes=["ecomb"])
                    P.op("dve", lambda L=L, C=C, T8=T8, V1=V1, V2=V2, bc=bc: nc.vector.tensor_tensor(out=C, in0=C, in1=T8, op=ALU.mult), reads=["ecomb", "etmp8"], writes=["ecomb"])
                    P.op("dve", lambda L=L, C=C, T8=T8, V1=V1, V2=V2, bc=bc: nc.vector.tensor_reduce(out=V1, in_=C, axis=AX.X, op=ALU.add), reads=["ecomb"], writes=["ev1"])
                    P.op("dve", lambda L=L, C=C, T8=T8, V1=V1, V2=V2, bc=bc: nc.vector.reciprocal(out=V1, in_=V1), reads=["ev1"], writes=["ev1"])
                    P.op("dve", lambda L=L, C=C, T8=T8, V1=V1, V2=V2, bc=bc: nc.vector.tensor_tensor(out=C, in0=C, in1=bc(V1), op=ALU.mult), reads=["ecomb", "ev1"], writes=["ecomb"])
                    first = True
                    for e in range(NEXP):
                        for gq in range(NG):
                            wb = wctr[0] % 2
                            wctr[0] += 1
                            c0 = gq * 512
                            P.op("pool", (lambda wb=wb, e=e, c0=c0: nc.gpsimd.dma_start(out=Wg[:, wb, :, :], in_=mwg[0, e, :, c0:c0 + 512].rearrange("(kc p) n -> p kc n", p=128))),
                                 writes=[f"eWg{wb}"], dma="w")
                            P.op("pool", (lambda wb=wb, e=e, c0=c0: nc.gpsimd.dma_start(out=Wu[:, wb, :, :], in_=mwu[0, e, :, c0:c0 + 512].rearrange("(kc p) n -> p kc n", p=128))),
                                 writes=[f"eWu{wb}"], dma="w")
                            P.op("pool", (lambda wb=wb, e=e, c0=c0: nc.gpsimd.dma_start(out=Wd[:, wb, :, :], in_=mwd[0, e, c0:c0 + 512, :].rearrange("(f p) n -> p f n", p=128))),
                                 writes=[f"eWd{wb}"], dma="w")
                            for s0 in range(0, ntl, 4):
                                nt = min(4, ntl - s0)
                                N = nt * 128
                                hb2 = (s0 // 4) % 2
                                for f in range(4):
                                    sb_ = f % 2
                                    pgu = psc if sb_ == 0 else ppv
                                    ra, rb = (("pscA", "pscB") if sb_ == 0 else ("ppvA", "ppvB"))
                                    for kc in range(8):
                                        P.op("pe", (lambda kc=kc, f=f, wb=wb, s0=s0, N=N, pgu=pgu: nc.tensor.matmul(pgu[:, 0:N], Wg[:, wb, kc, f * 128:(f + 1) * 128], xnT[:, kc, s0 * 128:s0 * 128 + N], start=(kc == 0), stop=(kc == 7))),
                                             reads=[f"eWg{wb}", "exnT"], writes=[ra])
                                    for kc in range(8):
                                        P.op("pe", (lambda kc=kc, f=f, wb=wb, s0=s0, N=N, pgu=pgu: nc.tensor.matmul(pgu[:, 512:512 + N], Wu[:, wb, kc, f * 128:(f + 1) * 128], xnT[:, kc, s0 * 128:s0 * 128 + N], start=(kc == 0), stop=(kc == 7))),
                                             reads=[f"eWu{wb}", "exnT"], writes=[rb])
                                    P.op("act", (lambda sb_=sb_, N=N, pgu=pgu: nc.scalar.activation(out=sg[:, sb_, 0:N], in_=pgu[:, 0:N], func=AF.Silu)),
                                         reads=[ra], writes=[f"esg{sb_}"])
                                    P.op("dve", (lambda sb_=sb_, f=f, N=N, hb2=hb2, pgu=pgu: nc.vector.tensor_tensor(out=hT[:, hb2, f, 0:N], in0=pgu[:, 512:512 + N], in1=sg[:, sb_, 0:N], op=ALU.mult)),
                                         reads=[rb, f"esg{sb_}"], writes=[f"ehT{hb2}"])
                                for i in range(nt):
                                    ti = s0 + i
                                    for half in range(2):
                                        pt, pk2 = next_pg()
                                        for f in range(4):
                                            P.op("pe", (lambda f=f, pt=pt, half=half, i=i, wb=wb, hb2=hb2: nc.tensor.matmul(pt[:, 0:512], hT[:, hb2, f, i * 128:(i + 1) * 128], Wd[:, wb, f, half * 512:(half + 1) * 512], start=(f == 0), stop=(f == 3))),
                                                 reads=[f"ehT{hb2}", f"eWd{wb}"], writes=[pk2])
                                        if first:
                                            P.op("dve", (lambda pt=pt, half=half, ti=ti, e=e: nc.vector.tensor_scalar(yacc[:, ti, half * 512:(half + 1) * 512], pt[:, 0:512], comb[:, ti, e:e + 1], None, ALU.mult)),
                                                 reads=[pk2, "ecomb"], writes=[f"ey{ti}"])
                                        else:
                                            P.op("dve", (lambda pt=pt, half=half, ti=ti, e=e: nc.vector.scalar_tensor_tensor(out=yacc[:, ti, half * 512:(half + 1) * 512], in0=pt[:, 0:512], scalar=comb[:, ti, e:e + 1],
                                                                                                                            in1=yacc[:, ti, half * 512:(half + 1) * 512], op0=ALU.mult, op1=ALU.add)),
                                                 reads=[pk2, "ecomb", f"ey{ti}"], writes=[f"ey{ti}"])
                            first = False
                    for i, n in enumerate(tiles):
                        hb = i % 2
                        P.op("sp", (lambda n=n, hb=hb: nc.sync.dma_start(out=h_sb[:, hb, :], in_=hbuf[n * 128:(n + 1) * 128, :])),
                             reads=[f"hbuf{n}"], writes=[f"eh{hb}"], dma="ld")
                        P.op("dve", (lambda i=i, hb=hb: nc.vector.tensor_tensor(out=yacc[:, i, :], in0=yacc[:, i, :], in1=h_sb[:, hb, :], op=ALU.add)),
                             reads=[f"ey{i}", f"eh{hb}"], writes=[f"ey{i}"])
                        P.op("act", (lambda i=i: nc.scalar.activation(out=junk[:], in_=yacc[:, i, :], func=AF.Square, accum_out=fssq[:, i:i + 1])),
                             reads=[f"ey{i}"], writes=["ejunk", "efssq"])
                    P.op("act", lambda ntl=ntl: nc.scalar.activation(out=frstd[:, 0:ntl], in_=fssq[:, 0:ntl], func=AF.Sqrt, bias=epsb[:, 0:1], scale=1.0 / D),
                         reads=["efssq", "epsb"], writes=["efrstd"])
                    P.op("dve", lambda ntl=ntl: nc.vector.reciprocal(out=frstd[:, 0:ntl], in_=frstd[:, 0:ntl]), reads=["efrstd"], writes=["efrstd"])
                    for i, n in enumerate(tiles):
                        ob_i = i % 2
                        P.op("dve", (lambda i=i, ob_i=ob_i: nc.vector.scalar_tensor_tensor(out=ob[:, ob_i, :], in0=yacc[:, i, :], scalar=frstd[:, i:i + 1], in1=gainf[:], op0=ALU.mult, op1=ALU.mult)),
                             reads=[f"ey{i}", "efrstd", "egainf"], writes=[f"eob{ob_i}"])
                        P.op("sp", (lambda n=n, ob_i=ob_i: nc.sync.dma_start(out=out[(n - 1) * 128:n * 128, :], in_=ob[:, ob_i, :])),
                             reads=[f"eob{ob_i}"], writes=[], dma="st")
                P.emit_phase()

        if stop_after >= 1:
            mixer_phase(0)
        if stop_after >= 2:
            ffn_phase()
        if stop_after >= 3:
            mixer_phase(1)
        if stop_after >= 4:
            moe_phase()
    return nc


_CONSTS = None


def kernel(**inputs):
    global _CONSTS
    if _CONSTS is None:
        _CONSTS = make_consts()
    nc = build_program()
    shared = {k: np.ascontiguousarray(np.asarray(v, dtype=np.float32)) for k, v in inputs.items() if k != "x"}
    shared["norm_final"] = shared["norm_final"].reshape(1, D)
    shared.update(_CONSTS)
    x = np.asarray(inputs["x"], dtype=np.float32)
    in_maps = []
    for b in range(8):
        m = dict(shared)
        m["x"] = np.ascontiguousarray(x[b])
        in_maps.append(m)
    res = run_bass_kernel_spmd(nc, in_maps, core_ids=list(range(8)))
    return np.stack([np.asarray(r["out"]) for r in res.results], axis=0).astype(np.float32)
```

```python
import numpy as np
from contextlib import ExitStack
import ml_dtypes
import concourse.bass as bass
import concourse.mybir as mybir
from concourse.bass_utils import run_bass_kernel_spmd

F32 = mybir.dt.float32
BF16 = mybir.dt.bfloat16
AF = mybir.ActivationFunctionType
ALU = mybir.AluOpType
AX = mybir.AxisListType
bf = ml_dtypes.bfloat16

D = 1024
NTILES = 65
NEXP = 8
DFF = 2816
DFE = 3584
IN_W = 3840
EPS = 1e-5
NEG = -30000.0


class Prog:
    def __init__(self, nc, es):
        self.nc = nc
        self.es = es
        self.eng = {"pe": nc.tensor, "act": nc.scalar, "dve": nc.vector,
                    "pool": nc.gpsimd, "sp": nc.sync}
        self.ops = []
        self.last_w = {}
        self.readers = {}
        self.sems = {}
        self.cnt = {}
        self.waited = {}
        self.ticket = []
        self.emitted = 0
        self.dma_issued = {}
        self.RING = {"w": 16, "ld": 24, "st": 8}

    def _sem(self, name):
        if name not in self.sems:
            self.sems[name] = self.es.enter_context(self.nc.semaphore(name))
        return self.sems[name]

    def op(self, eng, fn, reads=(), writes=(), dma=None):
        idx = len(self.ops)
        deps = set()
        for r in reads:
            if r in self.last_w:
                deps.add(self.last_w[r])
        for w in writes:
            if w in self.last_w:
                deps.add(self.last_w[w])
            for rd in self.readers.get(w, {}).values():
                deps.add(rd)
        deps.discard(idx)
        self.ops.append(dict(eng=eng, fn=fn, deps=deps, dma=dma, reads=tuple(reads), writes=tuple(writes)))
        rkey = ("d", dma) if dma is not None else ("e", eng)
        for r in reads:
            self.readers.setdefault(r, {})[rkey] = idx
        for w in writes:
            self.last_w[w] = idx
            self.readers[w] = {}
        return idx

    def emit_phase(self):
        ops = self.ops
        lo, hi = self.emitted, len(ops)
        need = [False] * (hi - lo)
        last_of_eng = {}
        for i in range(lo, hi):
            o = ops[i]
            if o["dma"] is None:
                last_of_eng[o["eng"]] = i
            for d in o["deps"]:
                if d < lo:
                    continue
                p = ops[d]
                if p["dma"] is not None:
                    continue
                if p["eng"] == o["eng"] and o["dma"] is None:
                    if p["eng"] == "pe":
                        continue
                    if not (set(p["writes"]) & set(o["reads"])):
                        continue
                need[d - lo] = True
        for e, i in last_of_eng.items():
            need[i - lo] = True
        self.ticket.extend([None] * (hi - lo))
        for i in range(lo, hi):
            o = ops[i]
            if o["dma"] is not None:
                ring = self.RING.get(o["dma"], 8)
                k = self.dma_issued.get(o["dma"], 0)
                self.dma_issued[o["dma"]] = k + 1
                key = "d_%s_%d" % (o["dma"], k % ring)
                self.cnt[key] = self.cnt.get(key, 0) + 16
                self.ticket[i] = (key, self.cnt[key])
            elif need[i - lo]:
                key = "e_" + o["eng"]
                self.cnt[key] = self.cnt.get(key, 0) + 1
                self.ticket[i] = (key, self.cnt[key])
        for i in range(lo, hi):
            o = ops[i]
            e = self.eng[o["eng"]]
            req = {}
            for d in o["deps"]:
                if d < lo:
                    continue
                t = self.ticket[d]
                if t is None:
                    continue
                key, val = t
                if req.get(key, 0) < val:
                    req[key] = val
            for key, val in req.items():
                if self.waited.get((o["eng"], key), 0) >= val:
                    continue
                e.wait_ge(self._sem(key), val)
                self.waited[(o["eng"], key)] = val
            ins = o["fn"]()
            if self.ticket[i] is not None:
                key, val = self.ticket[i]
                ins.then_inc(self._sem(key), 16 if o["dma"] is not None else 1)
        for en, e in self.eng.items():
            for key, val in self.cnt.items():
                if self.waited.get((en, key), 0) >= val:
                    continue
                e.wait_ge(self._sem(key), val)
                self.waited[(en, key)] = val
        self.emitted = hi


def _slopes():
    return np.array([2.0 ** (-8.0 * (h + 1) / 16) for h in range(16)], dtype=np.float64)


def _head_of(g, col):
    par, jj = col // 4, col % 4
    return 8 * g + 2 * jj + par


def make_consts():
    sl = _slopes()
    key = np.arange(128)[:, None]
    q = np.arange(128)[None, :]
    bias_prev = np.zeros((128, 2, 8, 128), np.float32)
    bias_cur = np.zeros((128, 2, 8, 128), np.float32)
    for g in range(2):
        for c in range(8):
            s = sl[_head_of(g, c)]
            d_cur = q - key
            bias_cur[:, g, c, :] = np.where(d_cur >= 0, -s * d_cur, NEG)
            d_prev = q + 128 - key
            bias_prev[:, g, c, :] = np.where(d_prev < 128, -s * d_prev, NEG)
    m = np.arange(16)[:, None]
    bias_meta = np.zeros((16, 2, 8, 128), np.float32)
    bias_meta0 = np.zeros((16, 2, 8, 128), np.float32)
    off = np.zeros((16, NTILES, 2, 8), np.float32)
    for g in range(2):
        for c in range(8):
            s = sl[_head_of(g, c)]
            bias_meta[:, g, c, :] = -s * (16 + q - m)
            d0 = q - 112 - m
            bias_meta0[:, g, c, :] = np.where(d0 >= 0, -s * d0, NEG)
            for n in range(1, NTILES):
                off[:, n, g, c] = -s * 128.0 * (n - 1)
    W = (2, 4, 8, 16)
    pc = np.zeros((128, 4, 128), np.float32)
    pp = np.zeros((128, 4, 128), np.float32)
    pc0 = np.zeros((128, 4, 128), np.float32)
    for gi, w in enumerate(W):
        for t in range(128):
            for s_ in range(max(0, t - w + 1), t + 1):
                pc[s_, gi, t] += 1.0 / w
            pc[t, gi, t] -= 1.0
            for s_ in range(t + 128 - w + 1, 128):
                pp[s_, gi, t] += 1.0 / w
            if t >= 112:
                cnt = min(t - 111, w)
                for s_ in range(max(112, t - w + 1), t + 1):
                    pc0[s_, gi, t] += 1.0 / cnt
                pc0[t, gi, t] -= 1.0
    return {
        "c_identb": np.eye(128, dtype=np.float32).astype(bf),
        "c_identf": np.eye(128, dtype=np.float32),
        "c_bias_prev": bias_prev.reshape(128, 2048),
        "c_bias_cur": bias_cur.reshape(128, 2048),
        "c_bias_meta": bias_meta.reshape(16, 2048),
        "c_bias_meta0": bias_meta0.reshape(16, 2048),
        "c_off": off.reshape(16, NTILES * 16),
        "c_pool_cur": pc.reshape(128, 512).astype(bf),
        "c_pool_prev": pp.reshape(128, 512).astype(bf),
        "c_pool_cur0": pc0.reshape(128, 512).astype(bf),
    }


def build_program(ntiles=NTILES, moe_pass_tiles=16, debug=False, stop_after=4):
    nc = bass.Bass("TRN2", target_bir_lowering=False)
    nreal = ntiles - 1

    def din(name, shape, dt=F32):
        return nc.dram_tensor(name, list(shape), dt, kind="ExternalInput").ap()

    x = din("x", [8192, D])
    meta = din("meta_tokens", [16, D])
    norm_mix = din("norm_mix", [2, D])
    w_in = din("w_in", [2, D, IN_W])
    sinks = din("attn_sinks", [2, 16])
    w_br = din("w_attn_br", [2, D, D])
    w_pool = din("w_pool_grp", [2, 4, 128, 256])
    pool_scale = din("pool_scale", [2, D])
    w_out = din("w_out", [2, D, D])
    norm_ffn = din("norm_ffn", [2, D])
    dwg = din("dense_w_gate", [1, D, DFF])
    dwu = din("dense_w_up", [1, D, DFF])
    dwd = din("dense_w_down", [1, DFF, D])
    router = din("moe_router", [1, D, NEXP])
    mwg = din("moe_w_gate", [1, NEXP, D, DFE])
    mwu = din("moe_w_up", [1, NEXP, D, DFE])
    mwd = din("moe_w_down", [1, NEXP, DFE, D])
    norm_final = din("norm_final", [1, D])
    c_identb = din("c_identb", [128, 128], BF16)
    c_identf = din("c_identf", [128, 128])
    c_bias_prev = din("c_bias_prev", [128, 2048])
    c_bias_cur = din("c_bias_cur", [128, 2048])
    c_bias_meta = din("c_bias_meta", [16, 2048])
    c_bias_meta0 = din("c_bias_meta0", [16, 2048])
    c_off = din("c_off", [16, NTILES * 16])
    c_pool_cur = din("c_pool_cur", [128, 512], BF16)
    c_pool_prev = din("c_pool_prev", [128, 512], BF16)
    c_pool_cur0 = din("c_pool_cur0", [128, 512], BF16)
    out = nc.dram_tensor("out", [8192, D], F32, kind="ExternalOutput").ap()
    hbuf = nc.dram_tensor("hbuf", [NTILES * 128, D], F32, kind="Internal").ap()
    dbg = None
    if debug:
        dbg = nc.dram_tensor("dbg", [3, NTILES * 128, D], F32, kind="ExternalOutput").ap()

    with ExitStack() as top:
        P = Prog(nc, top)

        def sbt(es, name, shape, dt):
            return es.enter_context(nc.sbuf_tensor(name, list(shape), dt))

        ssqtab = sbt(top, "ssqtab", [128, NTILES], F32)
        rstdtab = sbt(top, "rstdtab", [128, NTILES], F32)
        identb = sbt(top, "identb", [128, 128], BF16)
        identf = sbt(top, "identf", [128, 128], F32)
        pg = [top.enter_context(nc.psum_tensor(f"pg{i}", [128, 512], F32)) for i in range(3)]
        psc = top.enter_context(nc.psum_tensor("psc", [128, 1024], F32))
        ppv = top.enter_context(nc.psum_tensor("ppv", [128, 1536], F32))
        pgi = [0]

        def next_pg():
            i = pgi[0] % 3
            pgi[0] += 1
            return pg[i], f"pg{i}"

        P.op("sp", lambda: nc.sync.dma_start(out=identb[:], in_=c_identb[:, :]), writes=["identb"], dma="ld")
        P.op("sp", lambda: nc.sync.dma_start(out=identf[:], in_=c_identf[:, :]), writes=["identf"], dma="ld")

        def rstd_from_ssq(n_lo, n_hi):
            P.op("act", lambda: nc.scalar.activation(out=rstdtab[:, n_lo:n_hi], in_=ssqtab[:, n_lo:n_hi], func=AF.Sqrt,
                                                     bias=epsb[:, 0:1], scale=1.0 / D),
                 reads=["ssqtab", "epsb"], writes=["rstdtab"])
            P.op("dve", lambda: nc.vector.reciprocal(out=rstdtab[:, n_lo:n_hi], in_=rstdtab[:, n_lo:n_hi]),
                 reads=["rstdtab"], writes=["rstdtab"])

        epsb = sbt(top, "epsb", [128, 1], F32)
        P.op("dve", lambda: nc.vector.memset(epsb[:], EPS), writes=["epsb"])

        def tile_src(layer, n):
            if layer == 0 and n >= 1:
                return x[(n - 1) * 128:n * 128, :]
            return hbuf[n * 128:(n + 1) * 128, :]

        with ExitStack() as es:
            hz = sbt(es, "p0_h", [128, 2, D], F32)
            junk = sbt(es, "p0_junk", [128, D], BF16)
            P.op("dve", lambda: nc.vector.memset(hz[:, 0, :], 0.0), writes=["p0h0"])
            P.op("sp", lambda: nc.sync.dma_start(out=hz[112:128, 0, :], in_=meta[:, :]), reads=[], writes=["p0h0"], dma="ld")
            P.op("sp", lambda: nc.sync.dma_start(out=hbuf[0:128, :], in_=hz[:, 0, :]), reads=["p0h0"], writes=["hbuf0"], dma="st")
            P.op("act", lambda: nc.scalar.activation(out=junk[:], in_=hz[:, 0, :], func=AF.Square, accum_out=ssqtab[:, 0:1]),
                 reads=["p0h0"], writes=["p0junk", "ssqtab"])
            for n in range(1, ntiles):
                b = n % 2
                P.op("sp", (lambda n=n, b=b: nc.sync.dma_start(out=hz[:, b, :], in_=x[(n - 1) * 128:n * 128, :])),
                     writes=[f"p0h{b}"], dma="ld")
                P.op("act", (lambda n=n, b=b: nc.scalar.activation(out=junk[:], in_=hz[:, b, :], func=AF.Square,
                                                                   accum_out=ssqtab[:, n:n + 1])),
                     reads=[f"p0h{b}"], writes=["p0junk", "ssqtab"])
            rstd_from_ssq(0, ntiles)
            P.emit_phase()

        def mixer_phase(l):
            with ExitStack() as es:
                Win = sbt(es, f"Win_{l}", [128, 8, IN_W], BF16)
                WkS = sbt(es, f"WkS_{l}", [128, 8, 128], BF16)
                Wbr = sbt(es, f"Wbr_{l}", [128, 8, D], BF16)
                Wout = sbt(es, f"Wout_{l}", [128, 8, D], BF16)
                Wpool = sbt(es, f"Wpool_{l}", [128, 4, 256], BF16)
                gain = sbt(es, f"gain_{l}", [128, D], F32)
                bprev = sbt(es, f"bprev_{l}", [128, 2048], F32)
                bcur = sbt(es, f"bcur_{l}", [128, 2048], F32)
                bmeta = sbt(es, f"bmeta_{l}", [16, 2048], F32)
                offt = sbt(es, f"offt_{l}", [16, NTILES * 16], F32)
                pcur = sbt(es, f"pcur_{l}", [128, 512], BF16)
                pprev = sbt(es, f"pprev_{l}", [128, 512], BF16)
                pcur0 = sbt(es, f"pcur0_{l}", [128, 512], BF16)
                sk_raw = sbt(es, f"sk_raw_{l}", [128, 16], F32)
                sinkexp = sbt(es, f"sinkexp_{l}", [128, 16], F32)
                h_sb = sbt(es, f"h_sb_{l}", [128, 3, D], F32)
                xn = sbt(es, f"xn_{l}", [128, D], BF16)
                xnT = sbt(es, f"xnT_{l}", [128, 2, 8, 128], BF16)
                qT = sbt(es, f"qT_{l}", [128, 2, 8, 128], BF16)
                kTa = sbt(es, f"kTa_{l}", [128, 2, 128], BF16)
                kTb = sbt(es, f"kTb_{l}", [128, 2, 128], BF16)
                kTma = sbt(es, f"kTma_{l}", [128, 16], BF16)
                kTmb = sbt(es, f"kTmb_{l}", [128, 16], BF16)
                vE = sbt(es, f"vE_{l}", [128, 2, 2, 66], BF16)
                vEm = sbt(es, f"vEm_{l}", [16, 2, 66], BF16)
                u_sb = sbt(es, f"u_sb_{l}", [128, 2, 512], BF16)
                gT = sbt(es, f"gT_{l}", [128, 2, 2048], BF16)
                t_sb = sbt(es, f"t_sb_{l}", [128, 2, 1024], F32)
                pTp = sbt(es, f"pTp_{l}", [128, 2048], BF16)
                pTc = sbt(es, f"pTc_{l}", [128, 2048], BF16)
                pTm = sbt(es, f"pTm_{l}", [16, 2048], BF16)
                den = sbt(es, f"den_{l}", [128, 16], F32)
                attn = sbt(es, f"attn_{l}", [128, D], BF16)
                attnT = sbt(es, f"attnT_{l}", [128, 8, 128], BF16)
                m1 = sbt(es, f"m1_{l}", [128, D], F32)
                m2 = sbt(es, f"m2_{l}", [128, 256], F32)
                mergedTM = sbt(es, f"mergedTM_{l}", [128, D], BF16)
                pooledT = sbt(es, f"pooledT_{l}", [128, 4, 128], BF16)
                mergedT = sbt(es, f"mergedT_{l}", [128, 8, 128], BF16)
                hnew = sbt(es, f"hnew_{l}", [128, D], F32)

                P.op("pool", lambda: nc.gpsimd.dma_start(out=Win[:, :, :], in_=w_in[l, :, :].rearrange("(kc p) n -> p kc n", p=128)),
                     writes=["Win"], dma="w")
                P.op("pool", lambda: nc.gpsimd.dma_start(out=WkS[:, :, 0:64], in_=w_in[l, :, 1088:1152].rearrange("(kc p) n -> p kc n", p=128)),
                     writes=["WkS"], dma="w")
                P.op("pool", lambda: nc.gpsimd.dma_start(out=WkS[:, :, 64:128], in_=w_in[l, :, 1024:1088].rearrange("(kc p) n -> p kc n", p=128)),
                     writes=["WkS"], dma="w")
                P.op("pool", lambda: nc.gpsimd.dma_start(out=Wbr[:, :, :], in_=w_br[l, :, :].rearrange("(kc p) n -> p kc n", p=128)),
                     writes=["Wbr"], dma="w")
                P.op("pool", lambda: nc.gpsimd.dma_start(out=Wout[:, :, :], in_=w_out[l, :, :].rearrange("(kc p) n -> p kc n", p=128)),
                     writes=["Wout"], dma="w")
                P.op("pool", lambda: nc.gpsimd.dma_start(out=Wpool[:, :, :], in_=w_pool[l, :, :, :].rearrange("g c d -> c g d")),
                     writes=["Wpool"], dma="w")
                P.op("sp", lambda: nc.sync.dma_start(out=gain[:], in_=norm_mix[l:l + 1, :].to_broadcast([128, D])), writes=["gain"], dma="ld")
                P.op("sp", lambda: nc.sync.dma_start(out=bprev[:], in_=c_bias_prev[:, :]), writes=["bprev"], dma="ld")
                P.op("sp", lambda: nc.sync.dma_start(out=bcur[:], in_=c_bias_cur[:, :]), writes=["bcur"], dma="ld")
                P.op("sp", lambda: nc.sync.dma_start(out=bmeta[:], in_=c_bias_meta0[:, :]), writes=["bmeta"], dma="ld")
                P.op("sp", lambda: nc.sync.dma_start(out=offt[:], in_=c_off[:, :]), writes=["offt"], dma="ld")
                P.op("sp", lambda: nc.sync.dma_start(out=pcur[:], in_=c_pool_cur[:, :]), writes=["pcur"], dma="ld")
                P.op("sp", lambda: nc.sync.dma_start(out=pprev[:], in_=c_pool_prev[:, :]), writes=["pprev"], dma="ld")
                P.op("sp", lambda: nc.sync.dma_start(out=pcur0[:], in_=c_pool_cur0[:, :]), writes=["pcur0"], dma="ld")
                P.op("sp", lambda: nc.sync.dma_start(out=sk_raw[:], in_=sinks[l:l + 1, :].to_broadcast([128, 16])), writes=["sk_raw"], dma="ld")
                P.op("sp", lambda: nc.sync.dma_start(out=hnew[:], in_=pool_scale[l:l + 1, :].to_broadcast([128, D])), writes=["hnew"], dma="ld")
                for g in range(4):
                    P.op("dve", (lambda g=g: nc.vector.tensor_tensor(out=Wpool[:, g, :], in0=Wpool[:, g, :], in1=hnew[:, g * 256:(g + 1) * 256], op=ALU.mult)),
                         reads=["Wpool", "hnew"], writes=["Wpool"])
                P.op("act", lambda: nc.scalar.activation(out=sinkexp[:], in_=sk_raw[:], func=AF.Exp), reads=["sk_raw"], writes=["sinkexp"])
                P.op("dve", lambda: nc.vector.memset(vE[:], 1.0), writes=["vE0", "vE1"])
                P.op("dve", lambda: nc.vector.memset(vEm[:], 1.0), writes=["vEm"])

                def proj_fm(n, wsel, evac_eng, evac_fn, wres, writes=()):
                    b_ = n % 2
                    pt, pk2 = next_pg()
                    for kc in range(8):
                        P.op("pe", (lambda kc=kc, pt=pt, b_=b_: nc.tensor.matmul(pt[:, 0:128], wsel(kc), xnT[:, b_, kc, :], start=(kc == 0), stop=(kc == 7))),
                             reads=[wres, f"xnT{b_}"], writes=[pk2])
                    P.op(evac_eng, (lambda pt=pt: evac_fn(pt[:, 0:128])), reads=[pk2], writes=list(writes))

                def stA(n):
                    hb = n % 3
                    P.op("sp", (lambda: nc.sync.dma_start(out=h_sb[:, hb, :], in_=tile_src(l, n))),
                         reads=[f"hbuf{n}"], writes=[f"h{hb}"], dma="ld")
                    P.op("dve", (lambda: nc.vector.scalar_tensor_tensor(out=xn[:], in0=h_sb[:, hb, :], scalar=rstdtab[:, n:n + 1],
                                                                        in1=gain[:], op0=ALU.mult, op1=ALU.mult)),
                         reads=[f"h{hb}", "rstdtab", "gain"], writes=["xn"])
                    pgt, pk = next_pg()
                    pgb = pgt[:].bitcast(BF16)
                    for kc in range(8):
                        P.op("pe", (lambda kc=kc: nc.tensor.transpose(pgb[:, kc * 128:(kc + 1) * 128], xn[:, kc * 128:(kc + 1) * 128], identb[:])),
                             reads=["xn", "identb"], writes=[pk])
                    P.op("act", (lambda: nc.scalar.copy(out=xnT[:, n % 2, :, :].rearrange("p a b -> p (a b)"), in_=pgb)),
                         reads=[pk], writes=[f"xnT{n % 2}"])

                def b1_chunks(n):
                    b_ = n % 2
                    sl = n % 2
                    jobs = []
                    for j in range(8):
                        jobs.append(lambda j=j: proj_fm(n, (lambda kc, j=j: Win[:, kc, j * 128:(j + 1) * 128]), "act",
                                                        (lambda p_, j=j: nc.scalar.activation(out=qT[:, b_, j, :], in_=p_, func=AF.Copy, scale=0.125)),
                                                        "Win", writes=[f"qT{b_}"]))
                    jobs.append(lambda: proj_fm(n, (lambda kc: Win[:, kc, 1024:1152]), "act",
                                                (lambda p_: nc.scalar.copy(out=kTa[:, sl, :], in_=p_)), "Win", writes=[f"kTa{sl}"]))
                    jobs.append(lambda: proj_fm(n, (lambda kc: WkS[:, kc, :]), "act",
                                                (lambda p_: nc.scalar.copy(out=kTb[:, sl, :], in_=p_)), "WkS", writes=[f"kTb{sl}"]))
                    return jobs

                def stB1(n):
                    for jb in b1_chunks(n):
                        jb()

                def stB2(n, q_lo, q_hi):
                    b_ = n % 2
                    for qd in range(q_lo, q_hi):
                        pt, pk2 = next_pg()
                        for kc in range(8):
                            P.op("pe", (lambda kc=kc, pt=pt, qd=qd: nc.tensor.matmul(pt[:, 0:512], xnT[:, b_, kc, :], Win[:, kc, 1792 + qd * 512:1792 + (qd + 1) * 512], start=(kc == 0), stop=(kc == 7))),
                                 reads=[f"xnT{b_}", "Win"], writes=[pk2])
                        P.op("act", (lambda pt=pt, qd=qd: nc.scalar.activation(out=gT[:, b_, qd * 512:(qd + 1) * 512], in_=pt[:, 0:512], func=AF.Tanh, scale=0.5)),
                             reads=[pk2], writes=[f"gT{b_}"])

                def stB3(n):
                    b_ = n % 2
                    sl = n % 2
                    pv1, pk1 = next_pg()
                    pv2, pk2_ = next_pg()
                    for kc in range(8):
                        P.op("pe", (lambda kc=kc: nc.tensor.matmul(pv1[:, 0:512], xnT[:, b_, kc, :], Win[:, kc, 1152:1664], start=(kc == 0), stop=(kc == 7))),
                             reads=[f"xnT{b_}", "Win"], writes=[pk1])
                    for kc in range(8):
                        P.op("pe", (lambda kc=kc: nc.tensor.matmul(pv2[:, 0:128], xnT[:, b_, kc, :], Win[:, kc, 1664:1792], start=(kc == 0), stop=(kc == 7))),
                             reads=[f"xnT{b_}", "Win"], writes=[pk2_])
                    for kv in range(2):
                        P.op("act", (lambda kv=kv: nc.scalar.copy(out=vE[:, sl, kv, 0:64], in_=pv1[:, kv * 64:(kv + 1) * 64])),
                             reads=[pk1], writes=[f"vE{sl}"])
                    P.op("act", (lambda: nc.scalar.copy(out=u_sb[:, sl, 0:384], in_=pv1[:, 128:512])),
                         reads=[pk1], writes=[f"u{sl}"])
                    P.op("act", (lambda: nc.scalar.copy(out=u_sb[:, sl, 384:512], in_=pv2[:, 0:128])),
                         reads=[pk2_], writes=[f"u{sl}"])
                    if n == 0:
                        P.op("act", lambda: nc.scalar.copy(out=kTma[:], in_=kTa[:, 0, 112:128]), reads=["kTa0"], writes=["kTma"])
                        P.op("act", lambda: nc.scalar.copy(out=kTmb[:], in_=kTb[:, 0, 112:128]), reads=["kTb0"], writes=["kTmb"])
                        pm, pkm = next_pg()
                        for kc in range(8):
                            P.op("pe", (lambda kc=kc: nc.tensor.matmul(pm[0:16, 0:128], xnT[:, 0, kc, 112:128], Win[:, kc, 1152:1280], start=(kc == 0), stop=(kc == 7))),
                                 reads=["xnT0", "Win"], writes=[pkm])
                        for kv in range(2):
                            P.op("act", (lambda kv=kv: nc.scalar.copy(out=vEm[:, kv, 0:64], in_=pm[0:16, kv * 64:(kv + 1) * 64])),
                                 reads=[pkm], writes=["vEm"])

                def blocks_of(n):
                    blocks = []
                    if n >= 2:
                        blocks.append(("prev", (n - 1) % 2))
                    if n >= 1:
                        blocks.append(("cur", n % 2))
                    blocks.append(("meta", None))
                    return blocks

                def stC(n, fillers=()):
                    b_ = n % 2
                    fillers = list(fillers)
                    blocks = blocks_of(n)
                    nsteps = 2 * len(blocks)
                    step = 0
                    for (bname, bs) in blocks:
                        for g in range(2):
                            tb = step % 2
                            tk = f"t_sb{tb}"
                            for par in range(2):
                                base = par * 64
                                use_a = (g == par)
                                if bname == "meta":
                                    kt = (kTma if use_a else kTmb)[base:base + 64, :]
                                    kres = "kTma" if use_a else "kTmb"
                                    M = 16
                                else:
                                    kt = (kTa if use_a else kTb)[base:base + 64, bs, :]
                                    kres = (f"kTa{bs}" if use_a else f"kTb{bs}")
                                    M = 128
                                P.op("pe", (lambda kt=kt, M=M, par=par, g=g, base=base: nc.tensor.matmul(
                                    psc[0:M, par * 512:(par + 1) * 512], kt, qT[base:base + 64, b_, 4 * g:4 * g + 4, :], start=True, stop=True)),
                                    reads=[kres, f"qT{b_}"], writes=["psc"])
                            if bname == "meta":
                                P.op("dve", (lambda g=g, tb=tb: nc.vector.tensor_tensor(out=t_sb[0:16, tb, :], in0=psc[0:16, :], in1=bmeta[:, g * 1024:(g + 1) * 1024], op=ALU.add)),
                                     reads=["psc", "bmeta"], writes=[tk])
                                P.op("act", (lambda g=g, tb=tb: nc.scalar.activation(out=pTm[:, g * 1024:(g + 1) * 1024], in_=t_sb[0:16, tb, :], func=AF.Exp)),
                                     reads=[tk], writes=["pTm"])
                            else:
                                btab = bprev if bname == "prev" else bcur
                                pdst = pTp if bname == "prev" else pTc
                                P.op("dve", (lambda g=g, btab=btab, tb=tb: nc.vector.tensor_tensor(out=t_sb[:, tb, :], in0=psc[:], in1=btab[:, g * 1024:(g + 1) * 1024], op=ALU.add)),
                                     reads=["psc", "bprev" if bname == "prev" else "bcur"], writes=[tk])
                                P.op("act", (lambda g=g, pdst=pdst, tb=tb: nc.scalar.activation(out=pdst[:, g * 1024:(g + 1) * 1024], in_=t_sb[:, tb, :], func=AF.Exp)),
                                     reads=[tk], writes=["pTp" if bname == "prev" else "pTc"])
                            step += 1
                            if fillers:
                                k = -(-len(fillers) // (nsteps - step + 1))
                                for _ in range(k):
                                    fillers.pop(0)()
                    while fillers:
                        fillers.pop(0)()
                    if n == 0:
                        P.op("sp", lambda: nc.sync.dma_start(out=bmeta[:], in_=c_bias_meta[:, :]), writes=["bmeta"], dma="ld")
                    elif n + 1 < ntiles:
                        P.op("pool", lambda: nc.gpsimd.tensor_tensor(
                            out=bmeta[:].rearrange("p (h q) -> p h q", h=16),
                            in0=bmeta[:].rearrange("p (h q) -> p h q", h=16),
                            in1=offt[:, 32:48].unsqueeze(2).to_broadcast([16, 16, 128]), op=ALU.add),
                            reads=["bmeta", "offt"], writes=["bmeta"])

                def stD(n):
                    blocks = blocks_of(n)
                    for h in range(16):
                        g = h // 8
                        par = h % 2
                        jj = (h % 8) // 2
                        col = g * 1024 + par * 512 + jj * 128
                        bank, hh = h // 7, h % 7
                        o = ppv[:, bank * 512 + hh * 65:bank * 512 + hh * 65 + 65]
                        seq = []
                        for (bname, bs) in blocks:
                            if bname == "meta":
                                seq.append((pTm[:, col:col + 128], vEm[:, g, 0:65], "pTm", "vEm"))
                            elif bname == "prev":
                                seq.append((pTp[:, col:col + 128], vE[:, bs, g, 0:65], "pTp", f"vE{bs}"))
                            else:
                                seq.append((pTc[:, col:col + 128], vE[:, bs, g, 0:65], "pTc", f"vE{bs}"))
                        for i, (lt, rt, r1, r2) in enumerate(seq):
                            P.op("pe", (lambda o=o, lt=lt, rt=rt, i=i, L=len(seq): nc.tensor.matmul(o, lt, rt, start=(i == 0), stop=(i == L - 1))),
                                 reads=[r1, r2], writes=["ppv"])
                    for bank, nh in ((0, 7), (1, 7), (2, 2)):
                        P.op("dve", (lambda bank=bank, nh=nh: nc.vector.tensor_tensor(
                            out=den[:, bank * 7:bank * 7 + nh],
                            in0=ppv[:, bank * 512:bank * 512 + nh * 65].rearrange("p (h e) -> p h e", e=65)[:, :, 64],
                            in1=sinkexp[:, bank * 7:bank * 7 + nh], op=ALU.add)),
                            reads=["ppv", "sinkexp"], writes=["den"])
                    P.op("dve", lambda: nc.vector.reciprocal(out=den[:], in_=den[:]), reads=["den"], writes=["den"])
                    for bank, nh in ((0, 7), (1, 7), (2, 2)):
                        P.op("dve", (lambda bank=bank, nh=nh: nc.vector.tensor_tensor(
                            out=attn[:, bank * 448:bank * 448 + nh * 64].rearrange("p (h e) -> p h e", e=64),
                            in0=ppv[:, bank * 512:bank * 512 + nh * 65].rearrange("p (h e) -> p h e", e=65)[:, :, 0:64],
                            in1=den[:, bank * 7:bank * 7 + nh].unsqueeze(2).to_broadcast([128, nh, 64]), op=ALU.mult)),
                            reads=["ppv", "den"], writes=["attn"])

                def stD2(n):
                    pgt, pk = next_pg()
                    pgb = pgt[:].bitcast(BF16)
                    for kc in range(8):
                        P.op("pe", (lambda kc=kc: nc.tensor.transpose(pgb[:, kc * 128:(kc + 1) * 128], attn[:, kc * 128:(kc + 1) * 128], identb[:])),
                             reads=["attn", "identb"], writes=[pk])
                    P.op("act", (lambda: nc.scalar.copy(out=attnT[:].rearrange("p a b -> p (a b)"), in_=pgb)),
                         reads=[pk], writes=["attnT"])

                def stE(n):
                    b_ = n % 2
                    for half in range(2):
                        pt, pk2 = next_pg()
                        for kc in range(8):
                            P.op("pe", (lambda kc=kc, pt=pt, half=half: nc.tensor.matmul(pt[:, 0:512], attnT[:, kc, :], Wbr[:, kc, half * 512:(half + 1) * 512], start=(kc == 0), stop=(kc == 7))),
                                 reads=["Wbr", "attnT"], writes=[pk2])
                        P.op("dve", (lambda pt=pt, half=half: nc.vector.scalar_tensor_tensor(out=m1[:, half * 512:(half + 1) * 512], in0=gT[:, b_, half * 512:(half + 1) * 512], scalar=1.0,
                                                                                            in1=pt[:, 0:512], op0=ALU.add, op1=ALU.mult)),
                             reads=[pk2, f"gT{b_}"], writes=["m1"])

                def stF1(n):
                    sl = n % 2
                    slp = (n - 1) % 2
                    pt, pk2 = next_pg()
                    for g in range(4):
                        pm_cur = (pcur0 if n == 0 else pcur)
                        two = (n >= 1)
                        P.op("pe", (lambda g=g, pm_cur=pm_cur, two=two: nc.tensor.matmul(
                            pt[:, g * 128:(g + 1) * 128], u_sb[:, sl, g * 128:(g + 1) * 128], pm_cur[:, g * 128:(g + 1) * 128], start=True, stop=(not two))),
                            reads=[f"u{sl}", "pcur0" if n == 0 else "pcur"], writes=[pk2])
                        if two:
                            P.op("pe", (lambda g=g: nc.tensor.matmul(
                                pt[:, g * 128:(g + 1) * 128], u_sb[:, slp, g * 128:(g + 1) * 128], pprev[:, g * 128:(g + 1) * 128], start=False, stop=True)),
                                reads=[f"u{slp}", "pprev"], writes=[pk2])
                    P.op("act", (lambda: nc.scalar.copy(out=pooledT[:].rearrange("p a b -> p (a b)"), in_=pt[:, 0:512])),
                         reads=[pk2], writes=["pooledT"])

                def stF2(n):
                    b_ = n % 2
                    for g in range(4):
                        pt, pk2 = next_pg()
                        P.op("pe", (lambda pt=pt, g=g: nc.tensor.matmul(pt[:, 0:256], pooledT[:, g, :], Wpool[:, g, :], start=True, stop=True)),
                             reads=["Wpool", "pooledT"], writes=[pk2])
                        P.op("dve", (lambda pt=pt, g=g: nc.vector.scalar_tensor_tensor(out=m2[:], in0=gT[:, b_, 1024 + g * 256:1024 + (g + 1) * 256], scalar=1.0, in1=pt[:, 0:256], op0=ALU.add, op1=ALU.mult)),
                             reads=[pk2, f"gT{b_}"], writes=["m2"])
                        P.op("dve", (lambda g=g: nc.vector.tensor_tensor(out=mergedTM[:, g * 256:(g + 1) * 256], in0=m2[:], in1=m1[:, g * 256:(g + 1) * 256], op=ALU.add)),
                             reads=["m2", "m1"], writes=["mergedTM"])

                def stF2b(n):
                    pgt, pk = next_pg()
                    pgb = pgt[:].bitcast(BF16)
                    for kc in range(8):
                        P.op("pe", (lambda kc=kc: nc.tensor.transpose(pgb[:, kc * 128:(kc + 1) * 128], mergedTM[:, kc * 128:(kc + 1) * 128], identb[:])),
                             reads=["mergedTM", "identb"], writes=[pk])
                    P.op("act", (lambda: nc.scalar.copy(out=mergedT[:].rearrange("p a b -> p (a b)"), in_=pgb)),
                         reads=[pk], writes=["mergedT"])

                def stG(n):
                    hb = n % 3
                    for half in range(2):
                        pt, pk2 = next_pg()
                        for kc in range(8):
                            P.op("pe", (lambda kc=kc, pt=pt, half=half: nc.tensor.matmul(pt[:, 0:512], mergedT[:, kc, :], Wout[:, kc, half * 512:(half + 1) * 512], start=(kc == 0), stop=(kc == 7))),
                                 reads=["mergedT", "Wout"], writes=[pk2])
                        P.op("dve", (lambda pt=pt, half=half: nc.vector.scalar_tensor_tensor(out=hnew[:, half * 512:(half + 1) * 512], in0=pt[:, 0:512], scalar=0.5,
                                                                                            in1=h_sb[:, hb, half * 512:(half + 1) * 512], op0=ALU.mult, op1=ALU.add)),
                             reads=[pk2, f"h{hb}"], writes=["hnew"])
                    P.op("act", (lambda: nc.scalar.activation(out=mergedTM[:], in_=hnew[:], func=AF.Square, accum_out=ssqtab[:, n:n + 1])),
                         reads=["hnew"], writes=["mergedTM", "ssqtab"])
                    P.op("sp", (lambda: nc.sync.dma_start(out=hbuf[n * 128:(n + 1) * 128, :], in_=hnew[:])),
                         reads=["hnew"], writes=[f"hbuf{n}"], dma="st")
                    if dbg is not None:
                        P.op("sp", (lambda: nc.sync.dma_start(out=dbg[l, n * 128:(n + 1) * 128, :], in_=hnew[:])),
                             reads=["hnew"], writes=[], dma="st")

                stA(0); stB1(0); stB2(0, 0, 4); stB3(0)
                if ntiles > 1:
                    stA(1)
                for n in range(ntiles):
                    nxt = n + 1 if n + 1 < ntiles else None
                    stC(n, b1_chunks(nxt) if nxt is not None else ())
                    stD(n)
                    stF1(n)
                    if nxt is not None:
                        stB2(nxt, 0, 2)
                    stD2(n)
                    stE(n)
                    stF2(n)
                    if nxt is not None:
                        stB2(nxt, 2, 4)
                        stB3(nxt)
                    if n + 2 < ntiles:
                        stA(n + 2)
                    stF2b(n)
                    stG(n)
                rstd_from_ssq(0, ntiles)
                P.emit_phase()

        def ffn_phase():
            NF = DFF // 128
            with ExitStack() as es:
                Wg = sbt(es, "Wg", [128, 8, DFF], BF16)
                Wu = sbt(es, "Wu", [128, 8, DFF], BF16)
                Wd = sbt(es, "Wd", [128, NF, D], BF16)
                gain = sbt(es, "fgain", [128, D], F32)
                h_sb = sbt(es, "fh", [128, 4, D], F32)
                xn = sbt(es, "fxn", [128, D], BF16)
                xnT = sbt(es, "fxnT", [128, 8, 512], BF16)
                hT = sbt(es, "fhT", [128, NF, 512], BF16)
                sg = sbt(es, "fsg", [128, 2, 512], F32)
                junk = sbt(es, "fjunk", [128, D], BF16)
                P.op("pool", lambda: nc.gpsimd.dma_start(out=Wg[:, :, :], in_=dwg[0, :, :].rearrange("(kc p) n -> p kc n", p=128)), writes=["Wg"], dma="w")
                P.op("pool", lambda: nc.gpsimd.dma_start(out=Wu[:, :, :], in_=dwu[0, :, :].rearrange("(kc p) n -> p kc n", p=128)), writes=["Wu"], dma="w")
                P.op("pool", lambda: nc.gpsimd.dma_start(out=Wd[:, :, :], in_=dwd[0, :, :].rearrange("(f p) n -> p f n", p=128)), writes=["Wd"], dma="w")
                P.op("sp", lambda: nc.sync.dma_start(out=gain[:], in_=norm_ffn[0:1, :].to_broadcast([128, D])), writes=["fgain"], dma="ld")
                groups = [[0]] + [list(range(s, min(s + 4, ntiles))) for s in range(1, ntiles, 4)]
                for tiles in groups:
                    nt = len(tiles)
                    N = nt * 128
                    for i, n in enumerate(tiles):
                        P.op("sp", (lambda n=n, i=i: nc.sync.dma_start(out=h_sb[:, i, :], in_=hbuf[n * 128:(n + 1) * 128, :])),
                             reads=[f"hbuf{n}"], writes=[f"fh{i}"], dma="ld")
                        P.op("dve", (lambda n=n, i=i: nc.vector.scalar_tensor_tensor(out=xn[:], in0=h_sb[:, i, :], scalar=rstdtab[:, n:n + 1], in1=gain[:], op0=ALU.mult, op1=ALU.mult)),
                             reads=[f"fh{i}", "rstdtab", "fgain"], writes=["fxn"])
                        pgt, pk = next_pg()
                        pgb = pgt[:].bitcast(BF16)
                        for kc in range(8):
                            P.op("pe", (lambda kc=kc, pgb=pgb: nc.tensor.transpose(pgb[:, kc * 128:(kc + 1) * 128], xn[:, kc * 128:(kc + 1) * 128], identb[:])),
                                 reads=["fxn", "identb"], writes=[pk])
                        P.op("act", (lambda pgb=pgb, i=i: nc.scalar.copy(out=xnT[:, :, i * 128:(i + 1) * 128], in_=pgb.rearrange("p (a b) -> p a b", a=8))),
                             reads=[pk], writes=["fxnT"])
                    for f in range(NF):
                        sb_ = f % 2
                        pgu = psc if sb_ == 0 else ppv
                        ra, rb = (("pscA", "pscB") if sb_ == 0 else ("ppvA", "ppvB"))
                        for kc in range(8):
                            P.op("pe", (lambda kc=kc, f=f, N=N, pgu=pgu: nc.tensor.matmul(pgu[:, 0:N], Wg[:, kc, f * 128:(f + 1) * 128], xnT[:, kc, 0:N], start=(kc == 0), stop=(kc == 7))),
                                 reads=["Wg", "fxnT"], writes=[ra])
                        for kc in range(8):
                            P.op("pe", (lambda kc=kc, f=f, N=N, pgu=pgu: nc.tensor.matmul(pgu[:, 512:512 + N], Wu[:, kc, f * 128:(f + 1) * 128], xnT[:, kc, 0:N], start=(kc == 0), stop=(kc == 7))),
                                 reads=["Wu", "fxnT"], writes=[rb])
                        P.op("act", (lambda sb_=sb_, N=N, pgu=pgu: nc.scalar.activation(out=sg[:, sb_, 0:N], in_=pgu[:, 0:N], func=AF.Silu)),
                             reads=[ra], writes=[f"fsg{sb_}"])
                        P.op("dve", (lambda sb_=sb_, f=f, N=N, pgu=pgu: nc.vector.tensor_tensor(out=hT[:, f, 0:N], in0=pgu[:, 512:512 + N], in1=sg[:, sb_, 0:N], op=ALU.mult)),
                             reads=[rb, f"fsg{sb_}"], writes=["fhT"])
                    for i, n in enumerate(tiles):
                        for half in range(2):
                            pt, pk2 = next_pg()
                            for f in range(NF):
                                P.op("pe", (lambda f=f, pt=pt, half=half, i=i: nc.tensor.matmul(pt[:, 0:512], hT[:, f, i * 128:(i + 1) * 128], Wd[:, f, half * 512:(half + 1) * 512], start=(f == 0), stop=(f == NF - 1))),
                                     reads=["fhT", "Wd"], writes=[pk2])
                            P.op("dve", (lambda pt=pt, half=half, i=i: nc.vector.tensor_tensor(out=h_sb[:, i, half * 512:(half + 1) * 512], in0=pt[:, 0:512], in1=h_sb[:, i, half * 512:(half + 1) * 512], op=ALU.add)),
                                 reads=[pk2, f"fh{i}"], writes=[f"fh{i}"])
                        P.op("act", (lambda n=n, i=i: nc.scalar.activation(out=junk[:], in_=h_sb[:, i, :], func=AF.Square, accum_out=ssqtab[:, n:n + 1])),
                             reads=[f"fh{i}"], writes=["fjunk", "ssqtab"])
                        P.op("sp", (lambda n=n, i=i: nc.sync.dma_start(out=hbuf[n * 128:(n + 1) * 128, :], in_=h_sb[:, i, :])),
                             reads=[f"fh{i}"], writes=[f"hbuf{n}"], dma="st")
                        if dbg is not None:
                            P.op("sp", (lambda n=n, i=i: nc.sync.dma_start(out=dbg[2, n * 128:(n + 1) * 128, :], in_=h_sb[:, i, :])),
                                 reads=[f"fh{i}"], writes=[], dma="st")
                rstd_from_ssq(0, ntiles)
                P.emit_phase()

        def moe_phase():
            NG = 7
            PT = moe_pass_tiles
            with ExitStack() as es:
                Wg = sbt(es, "eWg", [128, 2, 8, 512], BF16)
                Wu = sbt(es, "eWu", [128, 2, 8, 512], BF16)
                Wd = sbt(es, "eWd", [128, 2, 4, D], BF16)
                gain = sbt(es, "egain", [128, D], F32)
                gainf = sbt(es, "egainf", [128, D], F32)
                Rt = sbt(es, "eR", [128, 8, NEXP], F32)
                h_sb = sbt(es, "eh", [128, 2, D], F32)
                xnf = sbt(es, "exnf", [128, D], F32)
                xnTf = sbt(es, "exnTf", [128, 8, 128], F32)
                xnT = sbt(es, "exnT", [128, 8, PT * 128], BF16)
                yacc = sbt(es, "eyacc", [128, PT, D], F32)
                hT = sbt(es, "ehT", [128, 2, 4, 512], BF16)
                sg = sbt(es, "esg", [128, 2, 512], F32)
                lg = sbt(es, "elg", [128, PT, NEXP], F32)
                comb = sbt(es, "ecomb", [128, PT, NEXP], F32)
                tmp8 = sbt(es, "etmp8", [128, PT, NEXP], F32)
                v1 = sbt(es, "ev1", [128, PT], F32)
                v2 = sbt(es, "ev2", [128, PT], F32)
                fssq = sbt(es, "efssq", [128, PT], F32)
                frstd = sbt(es, "efrstd", [128, PT], F32)
                junk = sbt(es, "ejunk", [128, D], BF16)
                ob = sbt(es, "eob", [128, 2, D], F32)
                P.op("sp", lambda: nc.sync.dma_start(out=gain[:], in_=norm_ffn[1:2, :].to_broadcast([128, D])), writes=["egain"], dma="ld")
                P.op("sp", lambda: nc.sync.dma_start(out=gainf[:], in_=norm_final[0:1, :].to_broadcast([128, D])), writes=["egainf"], dma="ld")
                with nc.allow_non_contiguous_dma(reason="tiny router"):
                    P.op("sp", lambda: nc.sync.dma_start(out=Rt[:], in_=router[0, :, :].rearrange("(c p) e -> p c e", p=128)), writes=["eR"], dma="ld")
                npass = (nreal + PT - 1) // PT
                wctr = [0]
                for ps_i in range(npass):
                    tiles = list(range(1 + ps_i * PT, min(1 + (ps_i + 1) * PT, ntiles)))
                    ntl = len(tiles)
                    for i, n in enumerate(tiles):
                        hb = i % 2
                        P.op("sp", (lambda n=n, hb=hb: nc.sync.dma_start(out=h_sb[:, hb, :], in_=hbuf[n * 128:(n + 1) * 128, :])),
                             reads=[f"hbuf{n}"], writes=[f"eh{hb}"], dma="ld")
                        P.op("dve", (lambda n=n, hb=hb: nc.vector.scalar_tensor_tensor(out=xnf[:], in0=h_sb[:, hb, :], scalar=rstdtab[:, n:n + 1], in1=gain[:], op0=ALU.mult, op1=ALU.mult)),
                             reads=[f"eh{hb}", "rstdtab", "egain"], writes=["exnf"])
                        for kc in range(8):
                            P.op("pe", (lambda kc=kc: nc.tensor.transpose(psc[:, kc * 128:(kc + 1) * 128], xnf[:, kc * 128:(kc + 1) * 128], identf[:])),
                                 reads=["exnf", "identf"], writes=["pscA", "pscB"])
                        P.op("act", (lambda i=i: nc.scalar.copy(out=xnT[:, :, i * 128:(i + 1) * 128], in_=psc[:].rearrange("p (a b) -> p a b", a=8))),
                             reads=["pscA", "pscB"], writes=["exnT"])
                        P.op("act", lambda: nc.scalar.copy(out=xnTf[:].rearrange("p a b -> p (a b)"), in_=psc[:]),
                             reads=["pscA", "pscB"], writes=["exnTf"])
                        pt, pk2 = next_pg()
                        for kc in range(8):
                            P.op("pe", (lambda kc=kc, pt=pt: nc.tensor.matmul(pt[:, 0:NEXP], xnTf[:, kc, :], Rt[:, kc, :], start=(kc == 0), stop=(kc == 7))),
                                 reads=["exnTf", "eR"], writes=[pk2])
                        P.op("act", (lambda pt=pt, i=i: nc.scalar.copy(out=lg[:, i, :], in_=pt[:, 0:NEXP])), reads=[pk2], writes=["elg"])
                    L = lg[:, 0:ntl, :]
                    C = comb[:, 0:ntl, :]
                    T8 = tmp8[:, 0:ntl, :]
                    V1 = v1[:, 0:ntl]
                    V2 = v2[:, 0:ntl]
                    bc = (lambda v, ntl=ntl: v.unsqueeze(2).to_broadcast([128, ntl, NEXP]))
                    P.op("dve", lambda L=L, C=C, T8=T8, V1=V1, V2=V2, bc=bc: nc.vector.tensor_reduce(out=V1, in_=L, axis=AX.X, op=ALU.max), reads=["elg"], writes=["ev1"])
                    P.op("dve", lambda L=L, C=C, T8=T8, V1=V1, V2=V2, bc=bc: nc.vector.tensor_tensor(out=T8, in0=L, in1=bc(V1), op=ALU.is_equal), reads=["elg", "ev1"], writes=["etmp8"])
                    P.op("dve", lambda L=L, C=C, T8=T8, V1=V1, V2=V2, bc=bc: nc.vector.scalar_tensor_tensor(out=T8, in0=T8, scalar=-1e30, in1=L, op0=ALU.mult, op1=ALU.add), reads=["etmp8", "elg"], writes=["etmp8"])
                    P.op("dve", lambda L=L, C=C, T8=T8, V1=V1, V2=V2, bc=bc: nc.vector.tensor_reduce(out=V2, in_=T8, axis=AX.X, op=ALU.max), reads=["etmp8"], writes=["ev2"])
                    P.op("dve", lambda L=L, C=C, T8=T8, V1=V1, V2=V2, bc=bc: nc.vector.tensor_tensor(out=T8, in0=L, in1=bc(V2), op=ALU.is_ge), reads=["elg", "ev2", "etmp8"], writes=["etmp8"])
                    P.op("dve", lambda L=L, C=C, T8=T8, V1=V1, V2=V2, bc=bc: nc.vector.tensor_tensor(out=C, in0=L, in1=bc(V1), op=ALU.subtract), reads=["elg", "ev1"], writes=["ecomb"])
                    P.op("act", lambda L=L, C=C, T8=T8, V1=V1, V2=V2, bc=bc: nc.scalar.activation(out=C, in_=C, func=AF.Exp), reads=["ecomb"], writes=["ecomb"])
                    P.op("dve", lambda L=L, C=C, T8=T8, V1=V1, V2=V2, bc=bc: nc.vector.tensor_tensor(out=C, in0=C, in1=T8, op=ALU.mult), reads=["ecomb", "etmp8"], writes=["ecomb"])
                    P.op("dve", lambda L=L, C=C, T8=T8, V1=V1, V2=V2, bc=bc: nc.vector.tensor_reduce(out=V1, in_=C, axis=AX.X, op=ALU.add), reads=["ecomb"], writes=["ev1"])
                    P.op("dve", lambda L=L, C=C, T8=T8, V1=V1, V2=V2, bc=bc: nc.vector.reciprocal(out=V1, in_=V1), reads=["ev1"], writes=["ev1"])
                    P.op("dve", lambda L=L, C=C, T8=T8, V1=V1, V2=V2, bc=bc: nc.vector.tensor_tensor(out=C, in0=C, in1=bc(V1), op=ALU.mult), reads=["ecomb", "ev1"], writes=["ecomb"])
                    first = True
                    for e in range(NEXP):
                        for gq in range(NG):
                            wb = wctr[0] % 2
                            wctr[0] += 1
                            c0 = gq * 512
                            P.op("pool", (lambda wb=wb, e=e, c0=c0: nc.gpsimd.dma_start(out=Wg[:, wb, :, :], in_=mwg[0, e, :, c0:c0 + 512].rearrange("(kc p) n -> p kc n", p=128))),
                                 writes=[f"eWg{wb}"], dma="w")
                            P.op("pool", (lambda wb=wb, e=e, c0=c0: nc.gpsimd.dma_start(out=Wu[:, wb, :, :], in_=mwu[0, e, :, c0:c0 + 512].rearrange("(kc p) n -> p kc n", p=128))),
                                 writes=[f"eWu{wb}"], dma="w")
                            P.op("pool", (lambda wb=wb, e=e, c0=c0: nc.gpsimd.dma_start(out=Wd[:, wb, :, :], in_=mwd[0, e, c0:c0 + 512, :].rearrange("(f p) n -> p f n", p=128))),
                                 writes=[f"eWd{wb}"], dma="w")
                            for s0 in range(0, ntl, 4):
                                nt = min(4, ntl - s0)
                                N = nt * 128
                                hb2 = (s0 // 4) % 2
                                for f in range(4):
                                    sb_ = f % 2
                                    pgu = psc if sb_ == 0 else ppv
                                    ra, rb = (("pscA", "pscB") if sb_ == 0 else ("ppvA", "ppvB"))
                                    for kc in range(8):
                                        P.op("pe", (lambda kc=kc, f=f, wb=wb, s0=s0, N=N, pgu=pgu: nc.tensor.matmul(pgu[:, 0:N], Wg[:, wb, kc, f * 128:(f + 1) * 128], xnT[:, kc, s0 * 128:s0 * 128 + N], start=(kc == 0), stop=(kc == 7))),
                                             reads=[f"eWg{wb}", "exnT"], writes=[ra])
                                    for kc in range(8):
                                        P.op("pe", (lambda kc=kc, f=f, wb=wb, s0=s0, N=N, pgu=pgu: nc.tensor.matmul(pgu[:, 512:512 + N], Wu[:, wb, kc, f * 128:(f + 1) * 128], xnT[:, kc, s0 * 128:s0 * 128 + N], start=(kc == 0), stop=(kc == 7))),
                                             reads=[f"eWu{wb}", "exnT"], writes=[rb])
                                    P.op("act", (lambda sb_=sb_, N=N, pgu=pgu: nc.scalar.activation(out=sg[:, sb_, 0:N], in_=pgu[:, 0:N], func=AF.Silu)),
                                         reads=[ra], writes=[f"esg{sb_}"])
                                    P.op("dve", (lambda sb_=sb_, f=f, N=N, hb2=hb2, pgu=pgu: nc.vector.tensor_tensor(out=hT[:, hb2, f, 0:N], in0=pgu[:, 512:512 + N], in1=sg[:, sb_, 0:N], op=ALU.mult)),
                                         reads=[rb, f"esg{sb_}"], writes=[f"ehT{hb2}"])
                                for i in range(nt):
                                    ti = s0 + i
                                    for half in range(2):
                                        pt, pk2 = next_pg()
                                        for f in range(4):
                                            P.op("pe", (lambda f=f, pt=pt, half=half, i=i, wb=wb, hb2=hb2: nc.tensor.matmul(pt[:, 0:512], hT[:, hb2, f, i * 128:(i + 1) * 128], Wd[:, wb, f, half * 512:(half + 1) * 512], start=(f == 0), stop=(f == 3))),
                                                 reads=[f"ehT{hb2}", f"eWd{wb}"], writes=[pk2])
                                        if first:
                                            P.op("dve", (lambda pt=pt, half=half, ti=ti, e=e: nc.vector.tensor_scalar(yacc[:, ti, half * 512:(half + 1) * 512], pt[:, 0:512], comb[:, ti, e:e + 1], None, ALU.mult)),
                                                 reads=[pk2, "ecomb"], writes=[f"ey{ti}"])
                                        else:
                                            P.op("dve", (lambda pt=pt, half=half, ti=ti, e=e: nc.vector.scalar_tensor_tensor(out=yacc[:, ti, half * 512:(half + 1) * 512], in0=pt[:, 0:512], scalar=comb[:, ti, e:e + 1],
                                                                                                                            in1=yacc[:, ti, half * 512:(half + 1) * 512], op0=ALU.mult, op1=ALU.add)),
                                                 reads=[pk2, "ecomb", f"ey{ti}"], writes=[f"ey{ti}"])
                            first = False
                    for i, n in enumerate(tiles):
                        hb = i % 2
                        P.op("sp", (lambda n=n, hb=hb: nc.sync.dma_start(out=h_sb[:, hb, :], in_=hbuf[n * 128:(n + 1) * 128, :])),
                             reads=[f"hbuf{n}"], writes=[f"eh{hb}"], dma="ld")
                        P.op("dve", (lambda i=i, hb=hb: nc.vector.tensor_tensor(out=yacc[:, i, :], in0=yacc[:, i, :], in1=h_sb[:, hb, :], op=ALU.add)),
                             reads=[f"ey{i}", f"eh{hb}"], writes=[f"ey{i}"])
                        P.op("act", (lambda i=i: nc.scalar.activation(out=junk[:], in_=yacc[:, i, :], func=AF.Square, accum_out=fssq[:, i:i + 1])),
                             reads=[f"ey{i}"], writes=["ejunk", "efssq"])
                    P.op("act", lambda ntl=ntl: nc.scalar.activation(out=frstd[:, 0:ntl], in_=fssq[:, 0:ntl], func=AF.Sqrt, bias=epsb[:, 0:1], scale=1.0 / D),
                         reads=["efssq", "epsb"], writes=["efrstd"])
                    P.op("dve", lambda ntl=ntl: nc.vector.reciprocal(out=frstd[:, 0:ntl], in_=frstd[:, 0:ntl]), reads=["efrstd"], writes=["efrstd"])
                    for i, n in enumerate(tiles):
                        ob_i = i % 2
                        P.op("dve", (lambda i=i, ob_i=ob_i: nc.vector.scalar_tensor_tensor(out=ob[:, ob_i, :], in0=yacc[:, i, :], scalar=frstd[:, i:i + 1], in1=gainf[:], op0=ALU.mult, op1=ALU.mult)),
                             reads=[f"ey{i}", "efrstd", "egainf"], writes=[f"eob{ob_i}"])
                        P.op("sp", (lambda n=n, ob_i=ob_i: nc.sync.dma_start(out=out[(n - 1) * 128:n * 128, :], in_=ob[:, ob_i, :])),
                             reads=[f"eob{ob_i}"], writes=[], dma="st")
                P.emit_phase()

        if stop_after >= 1:
            mixer_phase(0)
        if stop_after >= 2:
            ffn_phase()
        if stop_after >= 3:
            mixer_phase(1)
        if stop_after >= 4:
            moe_phase()
    return nc


_CONSTS = None


def kernel(**inputs):
    global _CONSTS
    if _CONSTS is None:
        _CONSTS = make_consts()
    nc = build_program()
    shared = {k: np.ascontiguousarray(np.asarray(v, dtype=np.float32)) for k, v in inputs.items() if k != "x"}
    shared["norm_final"] = shared["norm_final"].reshape(1, D)
    shared.update(_CONSTS)
    x = np.asarray(inputs["x"], dtype=np.float32)
    in_maps = []
    for b in range(8):
        m = dict(shared)
        m["x"] = np.ascontiguousarray(x[b])
        in_maps.append(m)
    res = run_bass_kernel_spmd(nc, in_maps, core_ids=list(range(8)))
    return np.stack([np.asarray(r["out"]) for r in res.results], axis=0).astype(np.float32)
```

```python
import numpy as np
from contextlib import ExitStack
import ml_dtypes
import concourse.bass as bass
import concourse.mybir as mybir
from concourse.bass_utils import run_bass_kernel_spmd

F32 = mybir.dt.float32
BF16 = mybir.dt.bfloat16
AF = mybir.ActivationFunctionType
ALU = mybir.AluOpType
AX = mybir.AxisListType
bf = ml_dtypes.bfloat16

D = 1024
NTILES = 65
NEXP = 8
DFF = 2816
DFE = 3584
IN_W = 3840
EPS = 1e-5
NEG = -30000.0


class Prog:
    def __init__(self, nc, es):
        self.nc = nc
        self.es = es
        self.eng = {"pe": nc.tensor, "act": nc.scalar, "dve": nc.vector,
                    "pool": nc.gpsimd, "sp": nc.sync}
        self.ops = []
        self.last_w = {}
        self.readers = {}
        self.sems = {}
        self.cnt = {}
        self.waited = {}
        self.ticket = []
        self.emitted = 0
        self.dma_issued = {}
        self.RING = {"w": 16, "ld": 24, "st": 8}

    def _sem(self, name):
        if name not in self.sems:
            self.sems[name] = self.es.enter_context(self.nc.semaphore(name))
        return self.sems[name]

    def op(self, eng, fn, reads=(), writes=(), dma=None):
        idx = len(self.ops)
        deps = set()
        for r in reads:
            if r in self.last_w:
                deps.add(self.last_w[r])
        for w in writes:
            if w in self.last_w:
                deps.add(self.last_w[w])
            for rd in self.readers.get(w, {}).values():
                deps.add(rd)
        deps.discard(idx)
        self.ops.append(dict(eng=eng, fn=fn, deps=deps, dma=dma, reads=tuple(reads), writes=tuple(writes)))
        rkey = ("d", dma) if dma is not None else ("e", eng)
        for r in reads:
            self.readers.setdefault(r, {})[rkey] = idx
        for w in writes:
            self.last_w[w] = idx
            self.readers[w] = {}
        return idx

    def emit_phase(self):
        ops = self.ops
        lo, hi = self.emitted, len(ops)
        need = [False] * (hi - lo)
        last_of_eng = {}
        for i in range(lo, hi):
            o = ops[i]
            if o["dma"] is None:
                last_of_eng[o["eng"]] = i
            for d in o["deps"]:
                if d < lo:
                    continue
                p = ops[d]
                if p["dma"] is not None:
                    continue
                if p["eng"] == o["eng"] and o["dma"] is None:
                    if p["eng"] == "pe":
                        continue
                    if not (set(p["writes"]) & set(o["reads"])):
                        continue
                need[d - lo] = True
        for e, i in last_of_eng.items():
            need[i - lo] = True
        self.ticket.extend([None] * (hi - lo))
        for i in range(lo, hi):
            o = ops[i]
            if o["dma"] is not None:
                ring = self.RING.get(o["dma"], 8)
                k = self.dma_issued.get(o["dma"], 0)
                self.dma_issued[o["dma"]] = k + 1
                key = "d_%s_%d" % (o["dma"], k % ring)
                self.cnt[key] = self.cnt.get(key, 0) + 16
                self.ticket[i] = (key, self.cnt[key])
            elif need[i - lo]:
                key = "e_" + o["eng"]
                self.cnt[key] = self.cnt.get(key, 0) + 1
                self.ticket[i] = (key, self.cnt[key])
        for i in range(lo, hi):
            o = ops[i]
            e = self.eng[o["eng"]]
            req = {}
            for d in o["deps"]:
                if d < lo:
                    continue
                t = self.ticket[d]
                if t is None:
                    continue
                key, val = t
                if req.get(key, 0) < val:
                    req[key] = val
            for key, val in req.items():
                if self.waited.get((o["eng"], key), 0) >= val:
                    continue
                e.wait_ge(self._sem(key), val)
                self.waited[(o["eng"], key)] = val
            ins = o["fn"]()
            if self.ticket[i] is not None:
                key, val = self.ticket[i]
                ins.then_inc(self._sem(key), 16 if o["dma"] is not None else 1)
        for en, e in self.eng.items():
            for key, val in self.cnt.items():
                if self.waited.get((en, key), 0) >= val:
                    continue
                e.wait_ge(self._sem(key), val)
                self.waited[(en, key)] = val
        self.emitted = hi


def _slopes():
    return np.array([2.0 ** (-8.0 * (h + 1) / 16) for h in range(16)], dtype=np.float64)


def _head_of(g, col):
    par, jj = col // 4, col % 4
    return 8 * g + 2 * jj + par


def make_consts():
    sl = _slopes()
    key = np.arange(128)[:, None]
    q = np.arange(128)[None, :]
    bias_prev = np.zeros((128, 2, 8, 128), np.float32)
    bias_cur = np.zeros((128, 2, 8, 128), np.float32)
    for g in range(2):
        for c in range(8):
            s = sl[_head_of(g, c)]
            d_cur = q - key
            bias_cur[:, g, c, :] = np.where(d_cur >= 0, -s * d_cur, NEG)
            d_prev = q + 128 - key
            bias_prev[:, g, c, :] = np.where(d_prev < 128, -s * d_prev, NEG)
    m = np.arange(16)[:, None]
    bias_meta = np.zeros((16, 2, 8, 128), np.float32)
    bias_meta0 = np.zeros((16, 2, 8, 128), np.float32)
    off = np.zeros((16, NTILES, 2, 8), np.float32)
    for g in range(2):
        for c in range(8):
            s = sl[_head_of(g, c)]
            bias_meta[:, g, c, :] = -s * (16 + q - m)
            d0 = q - 112 - m
            bias_meta0[:, g, c, :] = np.where(d0 >= 0, -s * d0, NEG)
            for n in range(1, NTILES):
                off[:, n, g, c] = -s * 128.0 * (n - 1)
    W = (2, 4, 8, 16)
    pc = np.zeros((128, 4, 128), np.float32)
    pp = np.zeros((128, 4, 128), np.float32)
    pc0 = np.zeros((128, 4, 128), np.float32)
    for gi, w in enumerate(W):
        for t in range(128):
            for s_ in range(max(0, t - w + 1), t + 1):
                pc[s_, gi, t] += 1.0 / w
            pc[t, gi, t] -= 1.0
            for s_ in range(t + 128 - w + 1, 128):
                pp[s_, gi, t] += 1.0 / w
            if t >= 112:
                cnt = min(t - 111, w)
                for s_ in range(max(112, t - w + 1), t + 1):
                    pc0[s_, gi, t] += 1.0 / cnt
                pc0[t, gi, t] -= 1.0
    return {
        "c_identb": np.eye(128, dtype=np.float32).astype(bf),
        "c_identf": np.eye(128, dtype=np.float32),
        "c_bias_prev": bias_prev.reshape(128, 2048),
        "c_bias_cur": bias_cur.reshape(128, 2048),
        "c_bias_meta": bias_meta.reshape(16, 2048),
        "c_bias_meta0": bias_meta0.reshape(16, 2048),
        "c_off": off.reshape(16, NTILES * 16),
        "c_pool_cur": pc.reshape(128, 512).astype(bf),
        "c_pool_prev": pp.reshape(128, 512).astype(bf),
        "c_pool_cur0": pc0.reshape(128, 512).astype(bf),
    }


def build_program(ntiles=NTILES, moe_pass_tiles=16, debug=False, stop_after=4):
    nc = bass.Bass("TRN2", target_bir_lowering=False)
    nreal = ntiles - 1

    def din(name, shape, dt=F32):
        return nc.dram_tensor(name, list(shape), dt, kind="ExternalInput").ap()

    x = din("x", [8192, D])
    meta = din("meta_tokens", [16, D])
    norm_mix = din("norm_mix", [2, D])
    w_in = din("w_in", [2, D, IN_W])
    sinks = din("attn_sinks", [2, 16])
    w_br = din("w_attn_br", [2, D, D])
    w_pool = din("w_pool_grp", [2, 4, 128, 256])
    pool_scale = din("pool_scale", [2, D])
    w_out = din("w_out", [2, D, D])
    norm_ffn = din("norm_ffn", [2, D])
    dwg = din("dense_w_gate", [1, D, DFF])
    dwu = din("dense_w_up", [1, D, DFF])
    dwd = din("dense_w_down", [1, DFF, D])
    router = din("moe_router", [1, D, NEXP])
    mwg = din("moe_w_gate", [1, NEXP, D, DFE])
    mwu = din("moe_w_up", [1, NEXP, D, DFE])
    mwd = din("moe_w_down", [1, NEXP, DFE, D])
    norm_final = din("norm_final", [1, D])
    c_identb = din("c_identb", [128, 128], BF16)
    c_identf = din("c_identf", [128, 128])
    c_bias_prev = din("c_bias_prev", [128, 2048])
    c_bias_cur = din("c_bias_cur", [128, 2048])
    c_bias_meta = din("c_bias_meta", [16, 2048])
    c_bias_meta0 = din("c_bias_meta0", [16, 2048])
    c_off = din("c_off", [16, NTILES * 16])
    c_pool_cur = din("c_pool_cur", [128, 512], BF16)
    c_pool_prev = din("c_pool_prev", [128, 512], BF16)
    c_pool_cur0 = din("c_pool_cur0", [128, 512], BF16)
    out = nc.dram_tensor("out", [8192, D], F32, kind="ExternalOutput").ap()
    hbuf = nc.dram_tensor("hbuf", [NTILES * 128, D], F32, kind="Internal").ap()
    dbg = None
    if debug:
        dbg = nc.dram_tensor("dbg", [3, NTILES * 128, D], F32, kind="ExternalOutput").ap()

    with ExitStack() as top:
        P = Prog(nc, top)

        def sbt(es, name, shape, dt):
            return es.enter_context(nc.sbuf_tensor(name, list(shape), dt))

        ssqtab = sbt(top, "ssqtab", [128, NTILES], F32)
        rstdtab = sbt(top, "rstdtab", [128, NTILES], F32)
        identb = sbt(top, "identb", [128, 128], BF16)
        identf = sbt(top, "identf", [128, 128], F32)
        pg = [top.enter_context(nc.psum_tensor(f"pg{i}", [128, 512], F32)) for i in range(3)]
        psc = top.enter_context(nc.psum_tensor("psc", [128, 1024], F32))
        ppv = top.enter_context(nc.psum_tensor("ppv", [128, 1536], F32))
        pgi = [0]

        def next_pg():
            i = pgi[0] % 3
            pgi[0] += 1
            return pg[i], f"pg{i}"

        P.op("sp", lambda: nc.sync.dma_start(out=identb[:], in_=c_identb[:, :]), writes=["identb"], dma="ld")
        P.op("sp", lambda: nc.sync.dma_start(out=identf[:], in_=c_identf[:, :]), writes=["identf"], dma="ld")

        def rstd_from_ssq(n_lo, n_hi):
            P.op("act", lambda: nc.scalar.activation(out=rstdtab[:, n_lo:n_hi], in_=ssqtab[:, n_lo:n_hi], func=AF.Sqrt,
                                                     bias=epsb[:, 0:1], scale=1.0 / D),
                 reads=["ssqtab", "epsb"], writes=["rstdtab"])
            P.op("dve", lambda: nc.vector.reciprocal(out=rstdtab[:, n_lo:n_hi], in_=rstdtab[:, n_lo:n_hi]),
                 reads=["rstdtab"], writes=["rstdtab"])

        epsb = sbt(top, "epsb", [128, 1], F32)
        P.op("dve", lambda: nc.vector.memset(epsb[:], EPS), writes=["epsb"])

        def tile_src(layer, n):
            if layer == 0 and n >= 1:
                return x[(n - 1) * 128:n * 128, :]
            return hbuf[n * 128:(n + 1) * 128, :]

        with ExitStack() as es:
            hz = sbt(es, "p0_h", [128, 2, D], F32)
            junk = sbt(es, "p0_junk", [128, D], BF16)
            P.op("dve", lambda: nc.vector.memset(hz[:, 0, :], 0.0), writes=["p0h0"])
            P.op("sp", lambda: nc.sync.dma_start(out=hz[112:128, 0, :], in_=meta[:, :]), reads=[], writes=["p0h0"], dma="ld")
            P.op("sp", lambda: nc.sync.dma_start(out=hbuf[0:128, :], in_=hz[:, 0, :]), reads=["p0h0"], writes=["hbuf0"], dma="st")
            P.op("act", lambda: nc.scalar.activation(out=junk[:], in_=hz[:, 0, :], func=AF.Square, accum_out=ssqtab[:, 0:1]),
                 reads=["p0h0"], writes=["p0junk", "ssqtab"])
            for n in range(1, ntiles):
                b = n % 2
                P.op("sp", (lambda n=n, b=b: nc.sync.dma_start(out=hz[:, b, :], in_=x[(n - 1) * 128:n * 128, :])),
                     writes=[f"p0h{b}"], dma="ld")
                P.op("act", (lambda n=n, b=b: nc.scalar.activation(out=junk[:], in_=hz[:, b, :], func=AF.Square,
                                                                   accum_out=ssqtab[:, n:n + 1])),
                     reads=[f"p0h{b}"], writes=["p0junk", "ssqtab"])
            rstd_from_ssq(0, ntiles)
            P.emit_phase()

        def mixer_phase(l):
            with ExitStack() as es:
                Win = sbt(es, f"Win_{l}", [128, 8, IN_W], BF16)
                WkS = sbt(es, f"WkS_{l}", [128, 8, 128], BF16)
                Wbr = sbt(es, f"Wbr_{l}", [128, 8, D], BF16)
                Wout = sbt(es, f"Wout_{l}", [128, 8, D], BF16)
                Wpool = sbt(es, f"Wpool_{l}", [128, 4, 256], BF16)
                gain = sbt(es, f"gain_{l}", [128, D], F32)
                bprev = sbt(es, f"bprev_{l}", [128, 2048], F32)
                bcur = sbt(es, f"bcur_{l}", [128, 2048], F32)
                bmeta = sbt(es, f"bmeta_{l}", [16, 2048], F32)
                offt = sbt(es, f"offt_{l}", [16, NTILES * 16], F32)
                pcur = sbt(es, f"pcur_{l}", [128, 512], BF16)
                pprev = sbt(es, f"pprev_{l}", [128, 512], BF16)
                pcur0 = sbt(es, f"pcur0_{l}", [128, 512], BF16)
                sk_raw = sbt(es, f"sk_raw_{l}", [128, 16], F32)
                sinkexp = sbt(es, f"sinkexp_{l}", [128, 16], F32)
                h_sb = sbt(es, f"h_sb_{l}", [128, 3, D], F32)
                xn = sbt(es, f"xn_{l}", [128, D], BF16)
                xnT = sbt(es, f"xnT_{l}", [128, 2, 8, 128], BF16)
                qT = sbt(es, f"qT_{l}", [128, 2, 8, 128], BF16)
                kTa = sbt(es, f"kTa_{l}", [128, 2, 128], BF16)
                kTb = sbt(es, f"kTb_{l}", [128, 2, 128], BF16)
                kTma = sbt(es, f"kTma_{l}", [128, 16], BF16)
                kTmb = sbt(es, f"kTmb_{l}", [128, 16], BF16)
                vE = sbt(es, f"vE_{l}", [128, 2, 2, 66], BF16)
                vEm = sbt(es, f"vEm_{l}", [16, 2, 66], BF16)
                u_sb = sbt(es, f"u_sb_{l}", [128, 2, 512], BF16)
                gT = sbt(es, f"gT_{l}", [128, 2, 2048], BF16)
                t_sb = sbt(es, f"t_sb_{l}", [128, 2, 1024], F32)
                pTp = sbt(es, f"pTp_{l}", [128, 2048], BF16)
                pTc = sbt(es, f"pTc_{l}", [128, 2048], BF16)
                pTm = sbt(es, f"pTm_{l}", [16, 2048], BF16)
                den = sbt(es, f"den_{l}", [128, 16], F32)
                attn = sbt(es, f"attn_{l}", [128, D], BF16)
                attnT = sbt(es, f"attnT_{l}", [128, 8, 128], BF16)
                m1 = sbt(es, f"m1_{l}", [128, D], F32)
                m2 = sbt(es, f"m2_{l}", [128, 256], F32)
                mergedTM = sbt(es, f"mergedTM_{l}", [128, D], BF16)
                pooledT = sbt(es, f"pooledT_{l}", [128, 4, 128], BF16)
                mergedT = sbt(es, f"mergedT_{l}", [128, 8, 128], BF16)
                hnew = sbt(es, f"hnew_{l}", [128, D], F32)

                P.op("pool", lambda: nc.gpsimd.dma_start(out=Win[:, :, :], in_=w_in[l, :, :].rearrange("(kc p) n -> p kc n", p=128)),
                     writes=["Win"], dma="w")
                P.op("pool", lambda: nc.gpsimd.dma_start(out=WkS[:, :, 0:64], in_=w_in[l, :, 1088:1152].rearrange("(kc p) n -> p kc n", p=128)),
                     writes=["WkS"], dma="w")
                P.op("pool", lambda: nc.gpsimd.dma_start(out=WkS[:, :, 64:128], in_=w_in[l, :, 1024:1088].rearrange("(kc p) n -> p kc n", p=128)),
                     writes=["WkS"], dma="w")
                P.op("pool", lambda: nc.gpsimd.dma_start(out=Wbr[:, :, :], in_=w_br[l, :, :].rearrange("(kc p) n -> p kc n", p=128)),
                     writes=["Wbr"], dma="w")
                P.op("pool", lambda: nc.gpsimd.dma_start(out=Wout[:, :, :], in_=w_out[l, :, :].rearrange("(kc p) n -> p kc n", p=128)),
                     writes=["Wout"], dma="w")
                P.op("pool", lambda: nc.gpsimd.dma_start(out=Wpool[:, :, :], in_=w_pool[l, :, :, :].rearrange("g c d -> c g d")),
                     writes=["Wpool"], dma="w")
                P.op("sp", lambda: nc.sync.dma_start(out=gain[:], in_=norm_mix[l:l + 1, :].to_broadcast([128, D])), writes=["gain"], dma="ld")
                P.op("sp", lambda: nc.sync.dma_start(out=bprev[:], in_=c_bias_prev[:, :]), writes=["bprev"], dma="ld")
                P.op("sp", lambda: nc.sync.dma_start(out=bcur[:], in_=c_bias_cur[:, :]), writes=["bcur"], dma="ld")
                P.op("sp", lambda: nc.sync.dma_start(out=bmeta[:], in_=c_bias_meta0[:, :]), writes=["bmeta"], dma="ld")
                P.op("sp", lambda: nc.sync.dma_start(out=offt[:], in_=c_off[:, :]), writes=["offt"], dma="ld")
                P.op("sp", lambda: nc.sync.dma_start(out=pcur[:], in_=c_pool_cur[:, :]), writes=["pcur"], dma="ld")
                P.op("sp", lambda: nc.sync.dma_start(out=pprev[:], in_=c_pool_prev[:, :]), writes=["pprev"], dma="ld")
                P.op("sp", lambda: nc.sync.dma_start(out=pcur0[:], in_=c_pool_cur0[:, :]), writes=["pcur0"], dma="ld")
                P.op("sp", lambda: nc.sync.dma_start(out=sk_raw[:], in_=sinks[l:l + 1, :].to_broadcast([128, 16])), writes=["sk_raw"], dma="ld")
                P.op("sp", lambda: nc.sync.dma_start(out=hnew[:], in_=pool_scale[l:l + 1, :].to_broadcast([128, D])), writes=["hnew"], dma="ld")
                for g in range(4):
                    P.op("dve", (lambda g=g: nc.vector.tensor_tensor(out=Wpool[:, g, :], in0=Wpool[:, g, :], in1=hnew[:, g * 256:(g + 1) * 256], op=ALU.mult)),
                         reads=["Wpool", "hnew"], writes=["Wpool"])
                P.op("act", lambda: nc.scalar.activation(out=sinkexp[:], in_=sk_raw[:], func=AF.Exp), reads=["sk_raw"], writes=["sinkexp"])
                P.op("dve", lambda: nc.vector.memset(vE[:], 1.0), writes=["vE0", "vE1"])
                P.op("dve", lambda: nc.vector.memset(vEm[:], 1.0), writes=["vEm"])

                def proj_fm(n, wsel, evac_eng, evac_fn, wres, writes=()):
                    b_ = n % 2
                    pt, pk2 = next_pg()
                    for kc in range(8):
                        P.op("pe", (lambda kc=kc, pt=pt, b_=b_: nc.tensor.matmul(pt[:, 0:128], wsel(kc), xnT[:, b_, kc, :], start=(kc == 0), stop=(kc == 7))),
                             reads=[wres, f"xnT{b_}"], writes=[pk2])
                    P.op(evac_eng, (lambda pt=pt: evac_fn(pt[:, 0:128])), reads=[pk2], writes=list(writes))

                def stA(n):
                    hb = n % 3
                    P.op("sp", (lambda: nc.sync.dma_start(out=h_sb[:, hb, :], in_=tile_src(l, n))),
                         reads=[f"hbuf{n}"], writes=[f"h{hb}"], dma="ld")
                    P.op("dve", (lambda: nc.vector.scalar_tensor_tensor(out=xn[:], in0=h_sb[:, hb, :], scalar=rstdtab[:, n:n + 1],
                                                                        in1=gain[:], op0=ALU.mult, op1=ALU.mult)),
                         reads=[f"h{hb}", "rstdtab", "gain"], writes=["xn"])
                    pgt, pk = next_pg()
                    pgb = pgt[:].bitcast(BF16)
                    for kc in range(8):
                        P.op("pe", (lambda kc=kc: nc.tensor.transpose(pgb[:, kc * 128:(kc + 1) * 128], xn[:, kc * 128:(kc + 1) * 128], identb[:])),
                             reads=["xn", "identb"], writes=[pk])
                    P.op("act", (lambda: nc.scalar.copy(out=xnT[:, n % 2, :, :].rearrange("p a b -> p (a b)"), in_=pgb)),
                         reads=[pk], writes=[f"xnT{n % 2}"])

                def b1_chunks(n):
                    b_ = n % 2
                    sl = n % 2
                    jobs = []
                    for j in range(8):
                        jobs.append(lambda j=j: proj_fm(n, (lambda kc, j=j: Win[:, kc, j * 128:(j + 1) * 128]), "act",
                                                        (lambda p_, j=j: nc.scalar.activation(out=qT[:, b_, j, :], in_=p_, func=AF.Copy, scale=0.125)),
                                                        "Win", writes=[f"qT{b_}"]))
                    jobs.append(lambda: proj_fm(n, (lambda kc: Win[:, kc, 1024:1152]), "act",
                                                (lambda p_: nc.scalar.copy(out=kTa[:, sl, :], in_=p_)), "Win", writes=[f"kTa{sl}"]))
                    jobs.append(lambda: proj_fm(n, (lambda kc: WkS[:, kc, :]), "act",
                                                (lambda p_: nc.scalar.copy(out=kTb[:, sl, :], in_=p_)), "WkS", writes=[f"kTb{sl}"]))
                    return jobs

                def stB1(n):
                    for jb in b1_chunks(n):
                        jb()

                def stB2(n, q_lo, q_hi):
                    b_ = n % 2
                    for qd in range(q_lo, q_hi):
                        pt, pk2 = next_pg()
                        for kc in range(8):
                            P.op("pe", (lambda kc=kc, pt=pt, qd=qd: nc.tensor.matmul(pt[:, 0:512], xnT[:, b_, kc, :], Win[:, kc, 1792 + qd * 512:1792 + (qd + 1) * 512], start=(kc == 0), stop=(kc == 7))),
                                 reads=[f"xnT{b_}", "Win"], writes=[pk2])
                        P.op("act", (lambda pt=pt, qd=qd: nc.scalar.activation(out=gT[:, b_, qd * 512:(qd + 1) * 512], in_=pt[:, 0:512], func=AF.Tanh, scale=0.5)),
                             reads=[pk2], writes=[f"gT{b_}"])

                def stB3(n):
                    b_ = n % 2
                    sl = n % 2
                    pv1, pk1 = next_pg()
                    pv2, pk2_ = next_pg()
                    for kc in range(8):
                        P.op("pe", (lambda kc=kc: nc.tensor.matmul(pv1[:, 0:512], xnT[:, b_, kc, :], Win[:, kc, 1152:1664], start=(kc == 0), stop=(kc == 7))),
                             reads=[f"xnT{b_}", "Win"], writes=[pk1])
                    for kc in range(8):
                        P.op("pe", (lambda kc=kc: nc.tensor.matmul(pv2[:, 0:128], xnT[:, b_, kc, :], Win[:, kc, 1664:1792], start=(kc == 0), stop=(kc == 7))),
                             reads=[f"xnT{b_}", "Win"], writes=[pk2_])
                    for kv in range(2):
                        P.op("act", (lambda kv=kv: nc.scalar.copy(out=vE[:, sl, kv, 0:64], in_=pv1[:, kv * 64:(kv + 1) * 64])),
                             reads=[pk1], writes=[f"vE{sl}"])
                    P.op("act", (lambda: nc.scalar.copy(out=u_sb[:, sl, 0:384], in_=pv1[:, 128:512])),
                         reads=[pk1], writes=[f"u{sl}"])
                    P.op("act", (lambda: nc.scalar.copy(out=u_sb[:, sl, 384:512], in_=pv2[:, 0:128])),
                         reads=[pk2_], writes=[f"u{sl}"])
                    if n == 0:
                        P.op("act", lambda: nc.scalar.copy(out=kTma[:], in_=kTa[:, 0, 112:128]), reads=["kTa0"], writes=["kTma"])
                        P.op("act", lambda: nc.scalar.copy(out=kTmb[:], in_=kTb[:, 0, 112:128]), reads=["kTb0"], writes=["kTmb"])
                        pm, pkm = next_pg()
                        for kc in range(8):
                            P.op("pe", (lambda kc=kc: nc.tensor.matmul(pm[0:16, 0:128], xnT[:, 0, kc, 112:128], Win[:, kc, 1152:1280], start=(kc == 0), stop=(kc == 7))),
                                 reads=["xnT0", "Win"], writes=[pkm])
                        for kv in range(2):
                            P.op("act", (lambda kv=kv: nc.scalar.copy(out=vEm[:, kv, 0:64], in_=pm[0:16, kv * 64:(kv + 1) * 64])),
                                 reads=[pkm], writes=["vEm"])

                def blocks_of(n):
                    blocks = []
                    if n >= 2:
                        blocks.append(("prev", (n - 1) % 2))
                    if n >= 1:
                        blocks.append(("cur", n % 2))
                    blocks.append(("meta", None))
                    return blocks

                def stC(n, fillers=()):
                    b_ = n % 2
                    fillers = list(fillers)
                    blocks = blocks_of(n)
                    nsteps = 2 * len(blocks)
                    step = 0
                    for (bname, bs) in blocks:
                        for g in range(2):
                            tb = step % 2
                            tk = f"t_sb{tb}"
                            for par in range(2):
                                base = par * 64
                                use_a = (g == par)
                                if bname == "meta":
                                    kt = (kTma if use_a else kTmb)[base:base + 64, :]
                                    kres = "kTma" if use_a else "kTmb"
                                    M = 16
                                else:
                                    kt = (kTa if use_a else kTb)[base:base + 64, bs, :]
                                    kres = (f"kTa{bs}" if use_a else f"kTb{bs}")
                                    M = 128
                                P.op("pe", (lambda kt=kt, M=M, par=par, g=g, base=base: nc.tensor.matmul(
                                    psc[0:M, par * 512:(par + 1) * 512], kt, qT[base:base + 64, b_, 4 * g:4 * g + 4, :], start=True, stop=True)),
                                    reads=[kres, f"qT{b_}"], writes=[f"psc{par}"])
                            for par in range(2):
                                c0, c1 = par * 512, (par + 1) * 512
                                o0 = g * 1024 + c0
                                tkp = f"{tk}_{par}"
                                if bname == "meta":
                                    P.op("dve", (lambda tb=tb, c0=c0, c1=c1, o0=o0: nc.vector.tensor_tensor(out=t_sb[0:16, tb, c0:c1], in0=psc[0:16, c0:c1], in1=bmeta[:, o0:o0 + 512], op=ALU.add)),
                                         reads=[f"psc{par}", "bmeta"], writes=[tkp])
                                    P.op("act", (lambda tb=tb, c0=c0, c1=c1, o0=o0: nc.scalar.activation(out=pTm[:, o0:o0 + 512], in_=t_sb[0:16, tb, c0:c1], func=AF.Exp)),
                                         reads=[tkp], writes=["pTm"])
                                else:
                                    btab = bprev if bname == "prev" else bcur
                                    pdst = pTp if bname == "prev" else pTc
                                    P.op("dve", (lambda btab=btab, tb=tb, c0=c0, c1=c1, o0=o0: nc.vector.tensor_tensor(out=t_sb[:, tb, c0:c1], in0=psc[:, c0:c1], in1=btab[:, o0:o0 + 512], op=ALU.add)),
                                         reads=[f"psc{par}", "bprev" if bname == "prev" else "bcur"], writes=[tkp])
                                    P.op("act", (lambda pdst=pdst, tb=tb, c0=c0, c1=c1, o0=o0: nc.scalar.activation(out=pdst[:, o0:o0 + 512], in_=t_sb[:, tb, c0:c1], func=AF.Exp)),
                                         reads=[tkp], writes=["pTp" if bname == "prev" else "pTc"])
                            step += 1
                            if fillers:
                                k = -(-len(fillers) // (nsteps - step + 1))
                                for _ in range(k):
                                    fillers.pop(0)()
                    while fillers:
                        fillers.pop(0)()
                    if n == 0:
                        P.op("sp", lambda: nc.sync.dma_start(out=bmeta[:], in_=c_bias_meta[:, :]), writes=["bmeta"], dma="ld")
                    elif n + 1 < ntiles:
                        P.op("pool", lambda: nc.gpsimd.tensor_tensor(
                            out=bmeta[:].rearrange("p (h q) -> p h q", h=16),
                            in0=bmeta[:].rearrange("p (h q) -> p h q", h=16),
                            in1=offt[:, 32:48].unsqueeze(2).to_broadcast([16, 16, 128]), op=ALU.add),
                            reads=["bmeta", "offt"], writes=["bmeta"])

                def stD(n):
                    blocks = blocks_of(n)
                    for h in range(16):
                        g = h // 8
                        par = h % 2
                        jj = (h % 8) // 2
                        col = g * 1024 + par * 512 + jj * 128
                        bank, hh = h // 7, h % 7
                        o = ppv[:, bank * 512 + hh * 65:bank * 512 + hh * 65 + 65]
                        seq = []
                        for (bname, bs) in blocks:
                            if bname == "meta":
                                seq.append((pTm[:, col:col + 128], vEm[:, g, 0:65], "pTm", "vEm"))
                            elif bname == "prev":
                                seq.append((pTp[:, col:col + 128], vE[:, bs, g, 0:65], "pTp", f"vE{bs}"))
                            else:
                                seq.append((pTc[:, col:col + 128], vE[:, bs, g, 0:65], "pTc", f"vE{bs}"))
                        for i, (lt, rt, r1, r2) in enumerate(seq):
                            P.op("pe", (lambda o=o, lt=lt, rt=rt, i=i, L=len(seq): nc.tensor.matmul(o, lt, rt, start=(i == 0), stop=(i == L - 1))),
                                 reads=[r1, r2], writes=["ppv"])
                    for bank, nh in ((0, 7), (1, 7), (2, 2)):
                        P.op("dve", (lambda bank=bank, nh=nh: nc.vector.tensor_tensor(
                            out=den[:, bank * 7:bank * 7 + nh],
                            in0=ppv[:, bank * 512:bank * 512 + nh * 65].rearrange("p (h e) -> p h e", e=65)[:, :, 64],
                            in1=sinkexp[:, bank * 7:bank * 7 + nh], op=ALU.add)),
                            reads=["ppv", "sinkexp"], writes=["den"])
                    P.op("dve", lambda: nc.vector.reciprocal(out=den[:], in_=den[:]), reads=["den"], writes=["den"])
                    for bank, nh in ((0, 7), (1, 7), (2, 2)):
                        P.op("dve", (lambda bank=bank, nh=nh: nc.vector.tensor_tensor(
                            out=attn[:, bank * 448:bank * 448 + nh * 64].rearrange("p (h e) -> p h e", e=64),
                            in0=ppv[:, bank * 512:bank * 512 + nh * 65].rearrange("p (h e) -> p h e", e=65)[:, :, 0:64],
                            in1=den[:, bank * 7:bank * 7 + nh].unsqueeze(2).to_broadcast([128, nh, 64]), op=ALU.mult)),
                            reads=["ppv", "den"], writes=["attn"])

                def stD2(n):
                    pgt, pk = next_pg()
                    pgb = pgt[:].bitcast(BF16)
                    for kc in range(8):
                        P.op("pe", (lambda kc=kc: nc.tensor.transpose(pgb[:, kc * 128:(kc + 1) * 128], attn[:, kc * 128:(kc + 1) * 128], identb[:])),
                             reads=["attn", "identb"], writes=[pk])
                    P.op("act", (lambda: nc.scalar.copy(out=attnT[:].rearrange("p a b -> p (a b)"), in_=pgb)),
                         reads=[pk], writes=["attnT"])

                def stE(n):
                    b_ = n % 2
                    for half in range(2):
                        pt, pk2 = next_pg()
                        for kc in range(8):
                            P.op("pe", (lambda kc=kc, pt=pt, half=half: nc.tensor.matmul(pt[:, 0:512], attnT[:, kc, :], Wbr[:, kc, half * 512:(half + 1) * 512], start=(kc == 0), stop=(kc == 7))),
                                 reads=["Wbr", "attnT"], writes=[pk2])
                        P.op("dve", (lambda pt=pt, half=half: nc.vector.scalar_tensor_tensor(out=m1[:, half * 512:(half + 1) * 512], in0=gT[:, b_, half * 512:(half + 1) * 512], scalar=1.0,
                                                                                            in1=pt[:, 0:512], op0=ALU.add, op1=ALU.mult)),
                             reads=[pk2, f"gT{b_}"], writes=["m1"])

                def stF1(n):
                    sl = n % 2
                    slp = (n - 1) % 2
                    pt, pk2 = next_pg()
                    for g in range(4):
                        pm_cur = (pcur0 if n == 0 else pcur)
                        two = (n >= 1)
                        P.op("pe", (lambda g=g, pm_cur=pm_cur, two=two: nc.tensor.matmul(
                            pt[:, g * 128:(g + 1) * 128], u_sb[:, sl, g * 128:(g + 1) * 128], pm_cur[:, g * 128:(g + 1) * 128], start=True, stop=(not two))),
                            reads=[f"u{sl}", "pcur0" if n == 0 else "pcur"], writes=[pk2])
                        if two:
                            P.op("pe", (lambda g=g: nc.tensor.matmul(
                                pt[:, g * 128:(g + 1) * 128], u_sb[:, slp, g * 128:(g + 1) * 128], pprev[:, g * 128:(g + 1) * 128], start=False, stop=True)),
                                reads=[f"u{slp}", "pprev"], writes=[pk2])
                    P.op("act", (lambda: nc.scalar.copy(out=pooledT[:].rearrange("p a b -> p (a b)"), in_=pt[:, 0:512])),
                         reads=[pk2], writes=["pooledT"])

                def stF2(n):
                    b_ = n % 2
                    for g in range(4):
                        pt, pk2 = next_pg()
                        P.op("pe", (lambda pt=pt, g=g: nc.tensor.matmul(pt[:, 0:256], pooledT[:, g, :], Wpool[:, g, :], start=True, stop=True)),
                             reads=["Wpool", "pooledT"], writes=[pk2])
                        P.op("dve", (lambda pt=pt, g=g: nc.vector.scalar_tensor_tensor(out=m2[:], in0=gT[:, b_, 1024 + g * 256:1024 + (g + 1) * 256], scalar=1.0, in1=pt[:, 0:256], op0=ALU.add, op1=ALU.mult)),
                             reads=[pk2, f"gT{b_}"], writes=["m2"])
                        P.op("dve", (lambda g=g: nc.vector.tensor_tensor(out=mergedTM[:, g * 256:(g + 1) * 256], in0=m2[:], in1=m1[:, g * 256:(g + 1) * 256], op=ALU.add)),
                             reads=["m2", "m1"], writes=["mergedTM"])

                def stF2b(n):
                    pgt, pk = next_pg()
                    pgb = pgt[:].bitcast(BF16)
                    for kc in range(8):
                        P.op("pe", (lambda kc=kc: nc.tensor.transpose(pgb[:, kc * 128:(kc + 1) * 128], mergedTM[:, kc * 128:(kc + 1) * 128], identb[:])),
                             reads=["mergedTM", "identb"], writes=[pk])
                    P.op("act", (lambda: nc.scalar.copy(out=mergedT[:].rearrange("p a b -> p (a b)"), in_=pgb)),
                         reads=[pk], writes=["mergedT"])

                def stG(n):
                    hb = n % 3
                    for half in range(2):
                        pt, pk2 = next_pg()
                        for kc in range(8):
                            P.op("pe", (lambda kc=kc, pt=pt, half=half: nc.tensor.matmul(pt[:, 0:512], mergedT[:, kc, :], Wout[:, kc, half * 512:(half + 1) * 512], start=(kc == 0), stop=(kc == 7))),
                                 reads=["mergedT", "Wout"], writes=[pk2])
                        P.op("dve", (lambda pt=pt, half=half: nc.vector.scalar_tensor_tensor(out=hnew[:, half * 512:(half + 1) * 512], in0=pt[:, 0:512], scalar=0.5,
                                                                                            in1=h_sb[:, hb, half * 512:(half + 1) * 512], op0=ALU.mult, op1=ALU.add)),
                             reads=[pk2, f"h{hb}"], writes=["hnew"])
                    P.op("act", (lambda: nc.scalar.activation(out=mergedTM[:], in_=hnew[:], func=AF.Square, accum_out=ssqtab[:, n:n + 1])),
                         reads=["hnew"], writes=["mergedTM", "ssqtab"])
                    P.op("sp", (lambda: nc.sync.dma_start(out=hbuf[n * 128:(n + 1) * 128, :], in_=hnew[:])),
                         reads=["hnew"], writes=[f"hbuf{n}"], dma="st")
                    if dbg is not None:
                        P.op("sp", (lambda: nc.sync.dma_start(out=dbg[l, n * 128:(n + 1) * 128, :], in_=hnew[:])),
                             reads=["hnew"], writes=[], dma="st")

                stA(0); stB1(0); stB2(0, 0, 4); stB3(0)
                if ntiles > 1:
                    stA(1)
                for n in range(ntiles):
                    nxt = n + 1 if n + 1 < ntiles else None
                    stC(n, b1_chunks(nxt) if nxt is not None else ())
                    stD(n)
                    stF1(n)
                    if nxt is not None:
                        stB2(nxt, 0, 2)
                    stD2(n)
                    stE(n)
                    stF2(n)
                    if nxt is not None:
                        stB2(nxt, 2, 4)
                        stB3(nxt)
                    if n + 2 < ntiles:
                        stA(n + 2)
                    stF2b(n)
                    stG(n)
                rstd_from_ssq(0, ntiles)
                P.emit_phase()

        def ffn_phase():
            NF = DFF // 128
            with ExitStack() as es:
                Wg = sbt(es, "Wg", [128, 8, DFF], BF16)
                Wu = sbt(es, "Wu", [128, 8, DFF], BF16)
                Wd = sbt(es, "Wd", [128, NF, D], BF16)
                gain = sbt(es, "fgain", [128, D], F32)
                h_sb = sbt(es, "fh", [128, 4, D], F32)
                xn = sbt(es, "fxn", [128, D], BF16)
                xnT = sbt(es, "fxnT", [128, 8, 512], BF16)
                hT = sbt(es, "fhT", [128, NF, 512], BF16)
                sg = sbt(es, "fsg", [128, 2, 512], F32)
                junk = sbt(es, "fjunk", [128, D], BF16)
                P.op("pool", lambda: nc.gpsimd.dma_start(out=Wg[:, :, :], in_=dwg[0, :, :].rearrange("(kc p) n -> p kc n", p=128)), writes=["Wg"], dma="w")
                P.op("pool", lambda: nc.gpsimd.dma_start(out=Wu[:, :, :], in_=dwu[0, :, :].rearrange("(kc p) n -> p kc n", p=128)), writes=["Wu"], dma="w")
                P.op("pool", lambda: nc.gpsimd.dma_start(out=Wd[:, :, :], in_=dwd[0, :, :].rearrange("(f p) n -> p f n", p=128)), writes=["Wd"], dma="w")
                P.op("sp", lambda: nc.sync.dma_start(out=gain[:], in_=norm_ffn[0:1, :].to_broadcast([128, D])), writes=["fgain"], dma="ld")
                groups = [[0]] + [list(range(s, min(s + 4, ntiles))) for s in range(1, ntiles, 4)]
                for tiles in groups:
                    nt = len(tiles)
                    N = nt * 128
                    for i, n in enumerate(tiles):
                        P.op("sp", (lambda n=n, i=i: nc.sync.dma_start(out=h_sb[:, i, :], in_=hbuf[n * 128:(n + 1) * 128, :])),
                             reads=[f"hbuf{n}"], writes=[f"fh{i}"], dma="ld")
                        P.op("dve", (lambda n=n, i=i: nc.vector.scalar_tensor_tensor(out=xn[:], in0=h_sb[:, i, :], scalar=rstdtab[:, n:n + 1], in1=gain[:], op0=ALU.mult, op1=ALU.mult)),
                             reads=[f"fh{i}", "rstdtab", "fgain"], writes=["fxn"])
                        pgt, pk = next_pg()
                        pgb = pgt[:].bitcast(BF16)
                        for kc in range(8):
                            P.op("pe", (lambda kc=kc, pgb=pgb: nc.tensor.transpose(pgb[:, kc * 128:(kc + 1) * 128], xn[:, kc * 128:(kc + 1) * 128], identb[:])),
                                 reads=["fxn", "identb"], writes=[pk])
                        P.op("act", (lambda pgb=pgb, i=i: nc.scalar.copy(out=xnT[:, :, i * 128:(i + 1) * 128], in_=pgb.rearrange("p (a b) -> p a b", a=8))),
                             reads=[pk], writes=["fxnT"])
                    for f in range(NF):
                        sb_ = f % 2
                        pgu = psc if sb_ == 0 else ppv
                        ra, rb = (("pscA", "pscB") if sb_ == 0 else ("ppvA", "ppvB"))
                        for kc in range(8):
                            P.op("pe", (lambda kc=kc, f=f, N=N, pgu=pgu: nc.tensor.matmul(pgu[:, 0:N], Wg[:, kc, f * 128:(f + 1) * 128], xnT[:, kc, 0:N], start=(kc == 0), stop=(kc == 7))),
                                 reads=["Wg", "fxnT"], writes=[ra])
                        for kc in range(8):
                            P.op("pe", (lambda kc=kc, f=f, N=N, pgu=pgu: nc.tensor.matmul(pgu[:, 512:512 + N], Wu[:, kc, f * 128:(f + 1) * 128], xnT[:, kc, 0:N], start=(kc == 0), stop=(kc == 7))),
                                 reads=["Wu", "fxnT"], writes=[rb])
                        P.op("act", (lambda sb_=sb_, N=N, pgu=pgu: nc.scalar.activation(out=sg[:, sb_, 0:N], in_=pgu[:, 0:N], func=AF.Silu)),
                             reads=[ra], writes=[f"fsg{sb_}"])
                        P.op("dve", (lambda sb_=sb_, f=f, N=N, pgu=pgu: nc.vector.tensor_tensor(out=hT[:, f, 0:N], in0=pgu[:, 512:512 + N], in1=sg[:, sb_, 0:N], op=ALU.mult)),
                             reads=[rb, f"fsg{sb_}"], writes=["fhT"])
                    for i, n in enumerate(tiles):
                        for half in range(2):
                            pt, pk2 = next_pg()
                            for f in range(NF):
                                P.op("pe", (lambda f=f, pt=pt, half=half, i=i: nc.tensor.matmul(pt[:, 0:512], hT[:, f, i * 128:(i + 1) * 128], Wd[:, f, half * 512:(half + 1) * 512], start=(f == 0), stop=(f == NF - 1))),
                                     reads=["fhT", "Wd"], writes=[pk2])
                            P.op("dve", (lambda pt=pt, half=half, i=i: nc.vector.tensor_tensor(out=h_sb[:, i, half * 512:(half + 1) * 512], in0=pt[:, 0:512], in1=h_sb[:, i, half * 512:(half + 1) * 512], op=ALU.add)),
                                 reads=[pk2, f"fh{i}"], writes=[f"fh{i}"])
                        P.op("act", (lambda n=n, i=i: nc.scalar.activation(out=junk[:], in_=h_sb[:, i, :], func=AF.Square, accum_out=ssqtab[:, n:n + 1])),
                             reads=[f"fh{i}"], writes=["fjunk", "ssqtab"])
                        P.op("sp", (lambda n=n, i=i: nc.sync.dma_start(out=hbuf[n * 128:(n + 1) * 128, :], in_=h_sb[:, i, :])),
                             reads=[f"fh{i}"], writes=[f"hbuf{n}"], dma="st")
                        if dbg is not None:
                            P.op("sp", (lambda n=n, i=i: nc.sync.dma_start(out=dbg[2, n * 128:(n + 1) * 128, :], in_=h_sb[:, i, :])),
                                 reads=[f"fh{i}"], writes=[], dma="st")
                rstd_from_ssq(0, ntiles)
                P.emit_phase()

        def moe_phase():
            NG = 7
            PT = moe_pass_tiles
            with ExitStack() as es:
                Wg = sbt(es, "eWg", [128, 2, 8, 512], BF16)
                Wu = sbt(es, "eWu", [128, 2, 8, 512], BF16)
                Wd = sbt(es, "eWd", [128, 2, 4, D], BF16)
                gain = sbt(es, "egain", [128, D], F32)
                gainf = sbt(es, "egainf", [128, D], F32)
                Rt = sbt(es, "eR", [128, 8, NEXP], F32)
                h_sb = sbt(es, "eh", [128, 2, D], F32)
                xnf = sbt(es, "exnf", [128, D], F32)
                xnTf = sbt(es, "exnTf", [128, 8, 128], F32)
                xnT = sbt(es, "exnT", [128, 8, PT * 128], BF16)
                yacc = sbt(es, "eyacc", [128, PT, D], F32)
                hT = sbt(es, "ehT", [128, 2, 4, 512], BF16)
                sg = sbt(es, "esg", [128, 2, 512], F32)
                lg = sbt(es, "elg", [128, PT, NEXP], F32)
                comb = sbt(es, "ecomb", [128, PT, NEXP], F32)
                tmp8 = sbt(es, "etmp8", [128, PT, NEXP], F32)
                v1 = sbt(es, "ev1", [128, PT], F32)
                v2 = sbt(es, "ev2", [128, PT], F32)
                fssq = sbt(es, "efssq", [128, PT], F32)
                frstd = sbt(es, "efrstd", [128, PT], F32)
                junk = sbt(es, "ejunk", [128, D], BF16)
                ob = sbt(es, "eob", [128, 2, D], F32)
                P.op("sp", lambda: nc.sync.dma_start(out=gain[:], in_=norm_ffn[1:2, :].to_broadcast([128, D])), writes=["egain"], dma="ld")
                P.op("sp", lambda: nc.sync.dma_start(out=gainf[:], in_=norm_final[0:1, :].to_broadcast([128, D])), writes=["egainf"], dma="ld")
                with nc.allow_non_contiguous_dma(reason="tiny router"):
                    P.op("sp", lambda: nc.sync.dma_start(out=Rt[:], in_=router[0, :, :].rearrange("(c p) e -> p c e", p=128)), writes=["eR"], dma="ld")
                npass = (nreal + PT - 1) // PT
                wctr = [0]
                for ps_i in range(npass):
                    tiles = list(range(1 + ps_i * PT, min(1 + (ps_i + 1) * PT, ntiles)))
                    ntl = len(tiles)
                    for i, n in enumerate(tiles):
                        hb = i % 2
                        P.op("sp", (lambda n=n, hb=hb: nc.sync.dma_start(out=h_sb[:, hb, :], in_=hbuf[n * 128:(n + 1) * 128, :])),
                             reads=[f"hbuf{n}"], writes=[f"eh{hb}"], dma="ld")
                        P.op("dve", (lambda n=n, hb=hb: nc.vector.scalar_tensor_tensor(out=xnf[:], in0=h_sb[:, hb, :], scalar=rstdtab[:, n:n + 1], in1=gain[:], op0=ALU.mult, op1=ALU.mult)),
                             reads=[f"eh{hb}", "rstdtab", "egain"], writes=["exnf"])
                        for kc in range(8):
                            P.op("pe", (lambda kc=kc: nc.tensor.transpose(psc[:, kc * 128:(kc + 1) * 128], xnf[:, kc * 128:(kc + 1) * 128], identf[:])),
                                 reads=["exnf", "identf"], writes=["pscA", "pscB"])
                        P.op("act", (lambda i=i: nc.scalar.copy(out=xnT[:, :, i * 128:(i + 1) * 128], in_=psc[:].rearrange("p (a b) -> p a b", a=8))),
                             reads=["pscA", "pscB"], writes=["exnT"])
                        P.op("act", lambda: nc.scalar.copy(out=xnTf[:].rearrange("p a b -> p (a b)"), in_=psc[:]),
                             reads=["pscA", "pscB"], writes=["exnTf"])
                        pt, pk2 = next_pg()
                        for kc in range(8):
                            P.op("pe", (lambda kc=kc, pt=pt: nc.tensor.matmul(pt[:, 0:NEXP], xnTf[:, kc, :], Rt[:, kc, :], start=(kc == 0), stop=(kc == 7))),
                                 reads=["exnTf", "eR"], writes=[pk2])
                        P.op("act", (lambda pt=pt, i=i: nc.scalar.copy(out=lg[:, i, :], in_=pt[:, 0:NEXP])), reads=[pk2], writes=["elg"])
                    L = lg[:, 0:ntl, :]
                    C = comb[:, 0:ntl, :]
                    T8 = tmp8[:, 0:ntl, :]
                    V1 = v1[:, 0:ntl]
                    V2 = v2[:, 0:ntl]
                    bc = (lambda v, ntl=ntl: v.unsqueeze(2).to_broadcast([128, ntl, NEXP]))
                    P.op("dve", lambda L=L, C=C, T8=T8, V1=V1, V2=V2, bc=bc: nc.vector.tensor_reduce(out=V1, in_=L, axis=AX.X, op=ALU.max), reads=["elg"], writes=["ev1"])
                    P.op("dve", lambda L=L, C=C, T8=T8, V1=V1, V2=V2, bc=bc: nc.vector.tensor_tensor(out=T8, in0=L, in1=bc(V1), op=ALU.is_equal), reads=["elg", "ev1"], writes=["etmp8"])
                    P.op("dve", lambda L=L, C=C, T8=T8, V1=V1, V2=V2, bc=bc: nc.vector.scalar_tensor_tensor(out=T8, in0=T8, scalar=-1e30, in1=L, op0=ALU.mult, op1=ALU.add), reads=["etmp8", "elg"], writes=["etmp8"])
                    P.op("dve", lambda L=L, C=C, T8=T8, V1=V1, V2=V2, bc=bc: nc.vector.tensor_reduce(out=V2, in_=T8, axis=AX.X, op=ALU.max), reads=["etmp8"], writes=["ev2"])
                    P.op("dve", lambda L=L, C=C, T8=T8, V1=V1, V2=V2, bc=bc: nc.vector.tensor_tensor(out=T8, in0=L, in1=bc(V2), op=ALU.is_ge), reads=["elg", "ev2", "etmp8"], writes=["etmp8"])
                    P.op("dve", lambda L=L, C=C, T8=T8, V1=V1, V2=V2, bc=bc: nc.vector.tensor_tensor(out=C, in0=L, in1=bc(V1), op=ALU.subtract), reads=["elg", "ev1"], writes=["ecomb"])
                    P.op("act", lambda L=L, C=C, T8=T8, V1=V1, V2=V2, bc=bc: nc.scalar.activation(out=C, in_=C, func=AF.Exp), reads=["ecomb"], writes=["ecomb"])
                    P.op("dve", lambda L=L, C=C, T8=T8, V1=V1, V2=V2, bc=bc: nc.vector.tensor_tensor(out=C, in0=C, in1=T8, op=ALU.mult), reads=["ecomb", "etmp8"], writes=["ecomb"])
                    P.op("dve", lambda L=L, C=C, T8=T8, V1=V1, V2=V2, bc=bc: nc.vector.tensor_reduce(out=V1, in_=C, axis=AX.X, op=ALU.add), reads=["ecomb"], writes=["ev1"])
                    P.op("dve", lambda L=L, C=C, T8=T8, V1=V1, V2=V2, bc=bc: nc.vector.reciprocal(out=V1, in_=V1), reads=["ev1"], writes=["ev1"])
                    P.op("dve", lambda L=L, C=C, T8=T8, V1=V1, V2=V2, bc=bc: nc.vector.tensor_tensor(out=C, in0=C, in1=bc(V1), op=ALU.mult), reads=["ecomb", "ev1"], writes=["ecomb"])
                    first = True
                    for e in range(NEXP):
                        for gq in range(NG):
                            wb = wctr[0] % 2
                            wctr[0] += 1
                            c0 = gq * 512
                            P.op("pool", (lambda wb=wb, e=e, c0=c0: nc.gpsimd.dma_start(out=Wg[:, wb, :, :], in_=mwg[0, e, :, c0:c0 + 512].rearrange("(kc p) n -> p kc n", p=128))),
                                 writes=[f"eWg{wb}"], dma="w")
                            P.op("pool", (lambda wb=wb, e=e, c0=c0: nc.gpsimd.dma_start(out=Wu[:, wb, :, :], in_=mwu[0, e, :, c0:c0 + 512].rearrange("(kc p) n -> p kc n", p=128))),
                                 writes=[f"eWu{wb}"], dma="w")
                            P.op("pool", (lambda wb=wb, e=e, c0=c0: nc.gpsimd.dma_start(out=Wd[:, wb, :, :], in_=mwd[0, e, c0:c0 + 512, :].rearrange("(f p) n -> p f n", p=128))),
                                 writes=[f"eWd{wb}"], dma="w")
                            for s0 in range(0, ntl, 4):
                                nt = min(4, ntl - s0)
                                N = nt * 128
                                hb2 = (s0 // 4) % 2
                                for f in range(4):
                                    sb_ = f % 2
                                    pgu = psc if sb_ == 0 else ppv
                                    ra, rb = (("pscA", "pscB") if sb_ == 0 else ("ppvA", "ppvB"))
                                    for kc in range(8):
                                        P.op("pe", (lambda kc=kc, f=f, wb=wb, s0=s0, N=N, pgu=pgu: nc.tensor.matmul(pgu[:, 0:N], Wg[:, wb, kc, f * 128:(f + 1) * 128], xnT[:, kc, s0 * 128:s0 * 128 + N], start=(kc == 0), stop=(kc == 7))),
                                             reads=[f"eWg{wb}", "exnT"], writes=[ra])
                                    for kc in range(8):
                                        P.op("pe", (lambda kc=kc, f=f, wb=wb, s0=s0, N=N, pgu=pgu: nc.tensor.matmul(pgu[:, 512:512 + N], Wu[:, wb, kc, f * 128:(f + 1) * 128], xnT[:, kc, s0 * 128:s0 * 128 + N], start=(kc == 0), stop=(kc == 7))),
                                             reads=[f"eWu{wb}", "exnT"], writes=[rb])
                                    P.op("act", (lambda sb_=sb_, N=N, pgu=pgu: nc.scalar.activation(out=sg[:, sb_, 0:N], in_=pgu[:, 0:N], func=AF.Silu)),
                                         reads=[ra], writes=[f"esg{sb_}"])
                                    P.op("dve", (lambda sb_=sb_, f=f, N=N, hb2=hb2, pgu=pgu: nc.vector.tensor_tensor(out=hT[:, hb2, f, 0:N], in0=pgu[:, 512:512 + N], in1=sg[:, sb_, 0:N], op=ALU.mult)),
                                         reads=[rb, f"esg{sb_}"], writes=[f"ehT{hb2}"])
                                for i in range(nt):
                                    ti = s0 + i
                                    for half in range(2):
                                        pt, pk2 = next_pg()
                                        for f in range(4):
                                            P.op("pe", (lambda f=f, pt=pt, half=half, i=i, wb=wb, hb2=hb2: nc.tensor.matmul(pt[:, 0:512], hT[:, hb2, f, i * 128:(i + 1) * 128], Wd[:, wb, f, half * 512:(half + 1) * 512], start=(f == 0), stop=(f == 3))),
                                                 reads=[f"ehT{hb2}", f"eWd{wb}"], writes=[pk2])
                                        if first:
                                            P.op("dve", (lambda pt=pt, half=half, ti=ti, e=e: nc.vector.tensor_scalar(yacc[:, ti, half * 512:(half + 1) * 512], pt[:, 0:512], comb[:, ti, e:e + 1], None, ALU.mult)),
                                                 reads=[pk2, "ecomb"], writes=[f"ey{ti}"])
                                        else:
                                            P.op("dve", (lambda pt=pt, half=half, ti=ti, e=e: nc.vector.scalar_tensor_tensor(out=yacc[:, ti, half * 512:(half + 1) * 512], in0=pt[:, 0:512], scalar=comb[:, ti, e:e + 1],
                                                                                                                            in1=yacc[:, ti, half * 512:(half + 1) * 512], op0=ALU.mult, op1=ALU.add)),
                                                 reads=[pk2, "ecomb", f"ey{ti}"], writes=[f"ey{ti}"])
                            first = False
                    for i, n in enumerate(tiles):
                        hb = i % 2
                        P.op("sp", (lambda n=n, hb=hb: nc.sync.dma_start(out=h_sb[:, hb, :], in_=hbuf[n * 128:(n + 1) * 128, :])),
                             reads=[f"hbuf{n}"], writes=[f"eh{hb}"], dma="ld")
                        P.op("dve", (lambda i=i, hb=hb: nc.vector.tensor_tensor(out=yacc[:, i, :], in0=yacc[:, i, :], in1=h_sb[:, hb, :], op=ALU.add)),
                             reads=[f"ey{i}", f"eh{hb}"], writes=[f"ey{i}"])
                        P.op("act", (lambda i=i: nc.scalar.activation(out=junk[:], in_=yacc[:, i, :], func=AF.Square, accum_out=fssq[:, i:i + 1])),
                             reads=[f"ey{i}"], writes=["ejunk", "efssq"])
                    P.op("act", lambda ntl=ntl: nc.scalar.activation(out=frstd[:, 0:ntl], in_=fssq[:, 0:ntl], func=AF.Sqrt, bias=epsb[:, 0:1], scale=1.0 / D),
                         reads=["efssq", "epsb"], writes=["efrstd"])
                    P.op("dve", lambda ntl=ntl: nc.vector.reciprocal(out=frstd[:, 0:ntl], in_=frstd[:, 0:ntl]), reads=["efrstd"], writes=["efrstd"])
                    for i, n in enumerate(tiles):
                        ob_i = i % 2
                        P.op("dve", (lambda i=i, ob_i=ob_i: nc.vector.scalar_tensor_tensor(out=ob[:, ob_i, :], in0=yacc[:, i, :], scalar=frstd[:, i:i + 1], in1=gainf[:], op0=ALU.mult, op1=ALU.mult)),
                             reads=[f"ey{i}", "efrstd", "egainf"], writes=[f"eob{ob_i}"])
                        P.op("sp", (lambda n=n, ob_i=ob_i: nc.sync.dma_start(out=out[(n - 1) * 128:n * 128, :], in_=ob[:, ob_i, :])),
                             reads=[f"eob{ob_i}"], writes=[], dma="st")
                P.emit_phase()

        if stop_after >= 1:
            mixer_phase(0)
        if stop_after >= 2:
            ffn_phase()
        if stop_after >= 3:
            mixer_phase(1)
        if stop_after >= 4:
            moe_phase()
    return nc


_CONSTS = None


def kernel(**inputs):
    global _CONSTS
    if _CONSTS is None:
        _CONSTS = make_consts()
    nc = build_program()
    shared = {k: np.ascontiguousarray(np.asarray(v, dtype=np.float32)) for k, v in inputs.items() if k != "x"}
    shared["norm_final"] = shared["norm_final"].reshape(1, D)
    shared.update(_CONSTS)
    x = np.asarray(inputs["x"], dtype=np.float32)
    in_maps = []
    for b in range(8):
        m = dict(shared)
        m["x"] = np.ascontiguousarray(x[b])
        in_maps.append(m)
    res = run_bass_kernel_spmd(nc, in_maps, core_ids=list(range(8)))
    return np.stack([np.asarray(r["out"]) for r in res.results], axis=0).astype(np.float32)
```
